# Optimizing a Trainium2 kernel written in Bass

```python
import math
import jax, jax.numpy as jnp
from jax import lax
import numpy as np

D_MODEL = 1024
BATCH = 2
SEQ = 8192
DEPTH = 4

BRANCH_W = D_MODEL // 4
D_MIX = 4 * BRANCH_W
HEAD_DIM = 64
EPS = 1e-6
RET_HEADS = BRANCH_W // HEAD_DIM
RET_CHUNK = 64
ROPE_BASE = 10000.0
DN_HEADS = 4
DN_DK = 64
DN_DV = BRANCH_W // DN_HEADS
DN_CONV = 4
DN_CHUNK = 64
DN_QKV = 2 * DN_HEADS * DN_DK + DN_HEADS * DN_DV
S5_GROUP = 16
S5_GROUPS = BRANCH_W // S5_GROUP
S5_STATE = 64
S5_DT_MIN = 1e-3
S5_DT_MAX = 1e-1
GLA_HEADS = 4
GLA_DK = 32
GLA_DV = BRANCH_W // GLA_HEADS
GLA_QK = GLA_HEADS * GLA_DK
GLA_GATE_RANK = 16
GLA_GATE_TAU = 16.0
GLA_CHUNK = 16

IN_SPLITS = [
    BRANCH_W, BRANCH_W, BRANCH_W, BRANCH_W,
    DN_QKV, DN_HEADS, DN_HEADS, BRANCH_W,
    BRANCH_W, BRANCH_W,
    GLA_QK, GLA_QK, BRANCH_W, GLA_GATE_RANK, BRANCH_W,
]
IN_COLS = sum(IN_SPLITS)

kernel_name = "hymba_style_ret_deltanet_s5_gla"

F32 = jnp.float32


def _rmsnorm(x, g):
    x32 = x.astype(F32)
    y = x32 * lax.rsqrt(jnp.mean(x32 * x32, axis=-1, keepdims=True) + EPS)
    return (y * g.astype(F32)).astype(x.dtype)


def _head_rmsnorm(o, g):
    return o * lax.rsqrt(jnp.mean(o * o, axis=-1, keepdims=True) + EPS) * g.astype(F32)


def _l2norm(t):
    return t * lax.rsqrt(jnp.sum(t * t, axis=-1, keepdims=True) + EPS)


def _split_cols(t, sizes):
    offs = np.cumsum(sizes)[:-1].tolist()
    return jnp.split(t, offs, axis=-1)


def _heads(t, n_heads):
    b, l, _ = t.shape
    return t.reshape(b, l, n_heads, -1).transpose(0, 2, 1, 3)


def _merge(o):
    b, h, l, d = o.shape
    return o.transpose(0, 2, 1, 3).reshape(b, l, h * d)


def _chunk(t, c):
    return t.reshape(t.shape[0], t.shape[1], t.shape[2] // c, c, *t.shape[3:])


def _rotary(t, pos):
    d = t.shape[-1]
    inv = ROPE_BASE ** (-jnp.arange(0, d, 2, dtype=F32) / d)
    ang = pos[:, None] * inv[None, :]
    cos, sin = jnp.cos(ang), jnp.sin(ang)
    t1, t2 = t[..., 0::2], t[..., 1::2]
    return jnp.stack([t1 * cos - t2 * sin, t1 * sin + t2 * cos], axis=-1).reshape(t.shape)


def _causal_conv(x, w):
    k, ch = w.shape
    return lax.conv_general_dilated(x, w[:, None, :], window_strides=(1,), padding=((k - 1, 0),),
                                    dimension_numbers=('NWC', 'WIO', 'NWC'), feature_group_count=ch)


def _retention(q, k, v):
    b, h, l, dk = q.shape
    dv = v.shape[-1]
    c = RET_CHUNK
    log_gamma = jnp.log(1.0 - 2.0 ** (-5.0 - jnp.arange(h, dtype=F32)))
    pos = jnp.arange(l, dtype=F32)
    q = _rotary(q, pos) * (dk ** -0.5)
    k = _rotary(k, pos)
    qc, kc, vc = _chunk(q, c), _chunk(k, c), _chunk(v, c)
    idx = jnp.arange(c, dtype=F32)
    diff = idx[:, None] - idx[None, :]
    causal = diff >= 0
    dmat = jnp.where(causal, jnp.exp(log_gamma[:, None, None] * jnp.where(causal, diff, 0.0)), 0.0)
    scores = jnp.einsum('bhnid,bhnjd->bhnij', qc, kc) * dmat[None, :, None]
    o_intra = jnp.einsum('bhnij,bhnje->bhnie', scores, vc)
    q_decay = jnp.exp(log_gamma[:, None] * (idx + 1.0))
    k_decay = jnp.exp(log_gamma[:, None] * (c - 1.0 - idx))
    chunk_decay = jnp.exp(log_gamma * c)
    kv = jnp.einsum('bhncd,bhnce->bhnde', kc * k_decay[None, :, None, :, None], vc)

    def step(state, kv_n):
        return state * chunk_decay[None, :, None, None] + kv_n, state

    _, r_prev = lax.scan(step, jnp.zeros((b, h, dk, dv), F32), jnp.moveaxis(kv, 2, 0))
    r_prev = jnp.moveaxis(r_prev, 0, 2)
    o_inter = jnp.einsum('bhncd,bhnde->bhnce', qc, r_prev) * q_decay[None, :, None, :, None]
    return (o_intra + o_inter).reshape(b, h, l, dv)


def _retention_branch(rq, rk, rv, gate, norm_g):
    o = _retention(_heads(rq, RET_HEADS), _heads(rk, RET_HEADS), _heads(rv, RET_HEADS))
    return _merge(_head_rmsnorm(o, norm_g)) * jax.nn.silu(gate)


def _gated_delta_rule(q, k, v, beta, g):
    b, h, l, dk = q.shape
    dv = v.shape[-1]
    c = DN_CHUNK
    q = _l2norm(q) * (dk ** -0.5)
    k = _l2norm(k)
    qc, kc, vc = _chunk(q, c), _chunk(k, c), _chunk(v, c)
    bc, gc = _chunk(beta, c), _chunk(g, c)
    gcum = jnp.cumsum(gc, axis=-1)
    tri = jnp.tril(jnp.ones((c, c), dtype=bool))
    strict = jnp.tril(jnp.ones((c, c), dtype=bool), -1)
    decay = jnp.exp(jnp.where(tri, gcum[..., :, None] - gcum[..., None, :], -jnp.inf))
    kb = kc * bc[..., None]
    a_mat = jnp.where(strict, jnp.einsum('bhnid,bhnjd->bhnij', kb, kc) * decay, 0.0)
    rhs = jnp.concatenate([vc * bc[..., None], kb * jnp.exp(gcum)[..., None]], axis=-1)
    sol = lax.linalg.triangular_solve(jnp.eye(c, dtype=F32) + a_mat, rhs, left_side=True, lower=True)
    u, w = sol[..., :dv], sol[..., dv:]
    attn = jnp.einsum('bhnid,bhnjd->bhnij', qc, kc) * decay
    qg = qc * jnp.exp(gcum)[..., None]
    kg = kc * jnp.exp(gcum[..., -1:] - gcum)[..., None]
    glast = jnp.exp(gcum[..., -1])

    def step(state, inp):
        u_n, w_n, attn_n, qg_n, kg_n, gl_n = inp
        v_new = u_n - jnp.einsum('bhcd,bhde->bhce', w_n, state)
        o = jnp.einsum('bhcd,bhde->bhce', qg_n, state) + jnp.einsum('bhij,bhje->bhie', attn_n, v_new)
        state = state * gl_n[..., None, None] + jnp.einsum('bhcd,bhce->bhde', kg_n, v_new)
        return state, o

    xs = tuple(jnp.moveaxis(t, 2, 0) for t in (u, w, attn, qg, kg, glast))
    _, o = lax.scan(step, jnp.zeros((b, h, dk, dv), F32), xs)
    return jnp.moveaxis(o, 0, 2).reshape(b, h, l, dv)


def _deltanet_branch(qkv, beta_in, a_in, gate, conv_w, a_log, dt_bias, norm_g):
    qkv = jax.nn.silu(_causal_conv(qkv, conv_w.astype(F32)))
    q, k, v = jnp.split(qkv, [DN_HEADS * DN_DK, 2 * DN_HEADS * DN_DK], axis=-1)
    q, k, v = _heads(q, DN_HEADS), _heads(k, DN_HEADS), _heads(v, DN_HEADS)
    beta = jax.nn.sigmoid(beta_in).transpose(0, 2, 1)
    g = (-jnp.exp(a_log.astype(F32)) * jax.nn.softplus(a_in + dt_bias.astype(F32))).transpose(0, 2, 1)
    o = _gated_delta_rule(q, k, v, beta, g)
    return _merge(_head_rmsnorm(o, norm_g)) * jax.nn.silu(gate)


def _s5(u, lam_re, lam_im, b_re, b_im, c_re, c_im, d, log_dt):
    bsz, l, _ = u.shape
    ug = u.reshape(bsz, l, S5_GROUPS, S5_GROUP)
    lam_re, lam_im = lam_re.astype(F32), lam_im.astype(F32)
    dt = jnp.exp(log_dt.astype(F32))[:, None]
    mag = jnp.exp(lam_re * dt)
    ang = lam_im * dt
    a_re, a_im = mag * jnp.cos(ang), mag * jnp.sin(ang)
    den = lam_re * lam_re + lam_im * lam_im
    nr, ni = a_re - 1.0, a_im
    coef_re = (nr * lam_re + ni * lam_im) / den
    coef_im = (ni * lam_re - nr * lam_im) / den
    b_re, b_im = b_re.astype(F32), b_im.astype(F32)
    bb_re = coef_re[..., None] * b_re - coef_im[..., None] * b_im
    bb_im = coef_re[..., None] * b_im + coef_im[..., None] * b_re
    x_re = jnp.einsum('gph,blgh->blgp', bb_re, ug)
    x_im = jnp.einsum('gph,blgh->blgp', bb_im, ug)
    a_re_f = jnp.broadcast_to(a_re, x_re.shape)
    a_im_f = jnp.broadcast_to(a_im, x_im.shape)

    def combine(left, right):
        a1r, a1i, b1r, b1i = left
        a2r, a2i, b2r, b2i = right
        return (a2r * a1r - a2i * a1i, a2r * a1i + a2i * a1r,
                a2r * b1r - a2i * b1i + b2r, a2r * b1i + a2i * b1r + b2i)

    _, _, s_re, s_im = lax.associative_scan(combine, (a_re_f, a_im_f, x_re, x_im), axis=1)
    y = (jnp.einsum('ghp,blgp->blgh', c_re.astype(F32), s_re)
         - jnp.einsum('ghp,blgp->blgh', c_im.astype(F32), s_im)
         + d.astype(F32) * ug)
    return y.reshape(bsz, l, -1)


def _s5_branch(u, gate, lam_re, lam_im, b_re, b_im, c_re, c_im, d, log_dt, w_glu, b_glu):
    y = jax.nn.gelu(_s5(u, lam_re, lam_im, b_re, b_im, c_re, c_im, d, log_dt))
    y = y * jax.nn.sigmoid(y @ w_glu.astype(F32) + b_glu.astype(F32))
    return y * jax.nn.silu(gate)


def _gla(q, k, v, gk):
    b, h, l, dk = q.shape
    dv = v.shape[-1]
    c = GLA_CHUNK
    q = q * (dk ** -0.5)
    qc, kc, vc, gc = _chunk(q, c), _chunk(k, c), _chunk(v, c), _chunk(gk, c)
    cum = jnp.cumsum(gc, axis=3)
    causal = jnp.tril(jnp.ones((c, c), dtype=bool))
    rel = cum[:, :, :, :, None, :] - cum[:, :, :, None, :, :]
    rel = jnp.where(causal[:, :, None], rel, -jnp.inf)
    scores = jnp.sum(qc[:, :, :, :, None, :] * kc[:, :, :, None, :, :] * jnp.exp(rel), axis=-1)
    o_intra = jnp.einsum('bhnij,bhnje->bhnie', scores, vc)
    cum_last = cum[:, :, :, -1]
    kv = jnp.einsum('bhncd,bhnce->bhnde', kc * jnp.exp(cum_last[:, :, :, None] - cum), vc)

    def step(state, inp):
        kv_n, dec_n = inp
        return state * dec_n[..., None] + kv_n, state

    _, s_prev = lax.scan(step, jnp.zeros((b, h, dk, dv), F32),
                         (jnp.moveaxis(kv, 2, 0), jnp.moveaxis(jnp.exp(cum_last), 2, 0)))
    s_prev = jnp.moveaxis(s_prev, 0, 2)
    o_inter = jnp.einsum('bhncd,bhnde->bhnce', qc * jnp.exp(cum), s_prev)
    return (o_intra + o_inter).reshape(b, h, l, dv)


def _gla_branch(q, k, v, gate_code, gate, w_gk, b_gk, norm_g):
    gk = jax.nn.log_sigmoid(gate_code @ w_gk.astype(F32) + b_gk.astype(F32)) / GLA_GATE_TAU
    o = _gla(_heads(q, GLA_HEADS), _heads(k, GLA_HEADS), _heads(v, GLA_HEADS), _heads(gk, GLA_HEADS))
    return _merge(_head_rmsnorm(o, norm_g)) * jax.nn.silu(gate)


def setup_inputs(seed: int = 0) -> dict:
    key = jax.random.key(seed)
    ks = jax.random.split(key, 24)

    def nrm(k, shape, scale):
        return jax.random.normal(k, shape, F32) * scale

    x = nrm(ks[0], (BATCH, SEQ, D_MODEL), 1.0)
    norm_pre = 1.0 + nrm(ks[1], (DEPTH, D_MODEL), 0.02)
    norm_post = 1.0 + nrm(ks[2], (DEPTH, D_MODEL), 0.02)
    w_in = nrm(ks[3], (DEPTH, D_MODEL, IN_COLS), D_MODEL ** -0.5)
    w_out = nrm(ks[4], (DEPTH, D_MIX, D_MODEL), D_MIX ** -0.5)
    ret_norm = 1.0 + nrm(ks[5], (DEPTH, HEAD_DIM), 0.02)
    dn_conv = nrm(ks[6], (DEPTH, DN_CONV, DN_QKV), DN_CONV ** -0.5)
    dn_a_log = jnp.log(jax.random.uniform(ks[7], (DEPTH, DN_HEADS), F32, 1.0, 16.0))
    dt = jnp.exp(jax.random.uniform(ks[8], (DEPTH, DN_HEADS), F32, math.log(1e-3), math.log(1e-1)))
    dn_dt_bias = dt + jnp.log(-jnp.expm1(-dt))
    dn_norm = 1.0 + nrm(ks[9], (DEPTH, DN_DV), 0.02)
    n_idx = jnp.arange(S5_STATE, dtype=F32)
    s5_lam_re = -0.5 + nrm(ks[10], (DEPTH, S5_GROUPS, S5_STATE), 0.01)
    s5_lam_im = math.pi * n_idx + nrm(ks[11], (DEPTH, S5_GROUPS, S5_STATE), 0.01)
    s5_b_re = nrm(ks[12], (DEPTH, S5_GROUPS, S5_STATE, S5_GROUP), (2 * S5_GROUP) ** -0.5)
    s5_b_im = nrm(ks[13], (DEPTH, S5_GROUPS, S5_STATE, S5_GROUP), (2 * S5_GROUP) ** -0.5)
    s5_c_re = nrm(ks[14], (DEPTH, S5_GROUPS, S5_GROUP, S5_STATE), S5_STATE ** -0.5)
    s5_c_im = nrm(ks[15], (DEPTH, S5_GROUPS, S5_GROUP, S5_STATE), S5_STATE ** -0.5)
    s5_d = nrm(ks[16], (DEPTH, S5_GROUPS, S5_GROUP), 1.0)
    s5_log_dt = jax.random.uniform(ks[17], (DEPTH, S5_GROUPS), F32, math.log(S5_DT_MIN), math.log(S5_DT_MAX))
    s5_w_glu = nrm(ks[18], (DEPTH, BRANCH_W, BRANCH_W), BRANCH_W ** -0.5)
    s5_b_glu = nrm(ks[19], (DEPTH, BRANCH_W), 0.01)
    gla_w_gk = nrm(ks[20], (DEPTH, GLA_GATE_RANK, GLA_QK), GLA_GATE_RANK ** -0.5)
    gla_b_gk = nrm(ks[21], (DEPTH, GLA_QK), 0.01)
    gla_norm = 1.0 + nrm(ks[22], (DEPTH, GLA_DV), 0.02)
    return {"x": x, "norm_pre": norm_pre, "norm_post": norm_post, "w_in": w_in, "w_out": w_out,
            "ret_norm": ret_norm, "dn_conv": dn_conv, "dn_a_log": dn_a_log, "dn_dt_bias": dn_dt_bias,
            "dn_norm": dn_norm, "s5_lam_re": s5_lam_re, "s5_lam_im": s5_lam_im, "s5_b_re": s5_b_re,
            "s5_b_im": s5_b_im, "s5_c_re": s5_c_re, "s5_c_im": s5_c_im, "s5_d": s5_d,
            "s5_log_dt": s5_log_dt, "s5_w_glu": s5_w_glu, "s5_b_glu": s5_b_glu,
            "gla_w_gk": gla_w_gk, "gla_b_gk": gla_b_gk, "gla_norm": gla_norm}


def reference(x, norm_pre, norm_post, w_in, w_out, ret_norm, dn_conv, dn_a_log, dn_dt_bias, dn_norm,
              s5_lam_re, s5_lam_im, s5_b_re, s5_b_im, s5_c_re, s5_c_im, s5_d, s5_log_dt, s5_w_glu, s5_b_glu,
              gla_w_gk, gla_b_gk, gla_norm):
    for i in range(DEPTH):
        h = _rmsnorm(x, norm_pre[i])
        p = jnp.einsum('bld,dc->blc', h, w_in[i]).astype(F32)
        (rq, rk, rv, rg, dqkv, dbeta, da, dg, su, sg, gq, gkk, gv, gcode, gg) = _split_cols(p, IN_SPLITS)
        y_ret = _retention_branch(rq, rk, rv, rg, ret_norm[i])
        y_dn = _deltanet_branch(dqkv, dbeta, da, dg, dn_conv[i], dn_a_log[i], dn_dt_bias[i], dn_norm[i])
        y_s5 = _s5_branch(su, sg, s5_lam_re[i], s5_lam_im[i], s5_b_re[i], s5_b_im[i], s5_c_re[i], s5_c_im[i],
                          s5_d[i], s5_log_dt[i], s5_w_glu[i], s5_b_glu[i])
        y_gla = _gla_branch(gq, gkk, gv, gcode, gg, gla_w_gk[i], gla_b_gk[i], gla_norm[i])
        y = jnp.concatenate([y_ret, y_dn, y_s5, y_gla], axis=-1).astype(x.dtype)
        o = jnp.einsum('blm,md->bld', y, w_out[i])
        x = x + _rmsnorm(o, norm_post[i])
    return x
```

```python
import os
import numpy as np
import ml_dtypes
import concourse.bass as bass
import concourse.mybir as mybir
from concourse.bass_utils import run_bass_kernel_spmd

F32 = mybir.dt.float32
BF16 = mybir.dt.bfloat16
AF = mybir.ActivationFunctionType
ALU = mybir.AluOpType
AX = mybir.AxisListType


class Rec:
    def __init__(self, nc):
        self.nc = nc
        self.ops = {k: [] for k in ('pe', 'act', 'dve', 'pool', 'sp')}
        self.cnt = {k: 0 for k in self.ops}
        self.clock = {k: {} for k in self.ops}
        self.snap = {}
        self.last_w = {}
        self.readers = {}
        self.sems = {}
        self.dma_cnt = {}

    def sem(self, key):
        if key not in self.sems:
            self.sems[key] = self.nc.alloc_semaphore(name="s_" + key.replace(':', '_'))
        return self.sems[key]

    def sb(self, name, shape, dt):
        n = 1
        for d_ in shape[1:]:
            n *= d_
        self.sb_bytes = getattr(self, 'sb_bytes', 0) + n * (2 if dt == BF16 else 4)
        return self.nc.alloc_sbuf_tensor("s_" + name, list(shape), dt)

    def ps(self, name, shape, dt=F32):
        return self.nc.alloc_psum_tensor("q_" + name, list(shape), dt)

    def _deps(self, eng, r, w):
        deps = {}

        def add(kn):
            if kn is None:
                return
            k, n = kn
            if deps.get(k, 0) < n:
                deps[k] = n
        for b in r:
            add(self.last_w.get(b))
        for b in w:
            add(self.last_w.get(b))
            for kn in self.readers.get(b, ()):
                add(kn)
        ck = self.clock[eng]
        waits = []
        for k, n in deps.items():
            if ck.get(k, 0) < n:
                waits.append((k, n))
        for k, n in waits:
            sn = self.snap.get((k, n))
            if sn:
                for k2, n2 in sn.items():
                    if ck.get(k2, 0) < n2:
                        ck[k2] = n2
            if ck.get(k, 0) < n:
                ck[k] = n
        return waits

    def _mark(self, me, r, w):
        for b in r:
            self.readers.setdefault(b, []).append(me)
        for b in w:
            self.last_w[b] = me
            self.readers[b] = []

    def op(self, eng, fn, r=(), w=()):
        w = list(w) + [b_ for b_ in r if b_.startswith('PS') and b_ not in w]
        waits = self._deps(eng, r, w)
        self.cnt[eng] += 1
        me = (eng, self.cnt[eng])
        self.snap[me] = dict(self.clock[eng])
        self.ops[eng].append((waits, fn, eng, 1))
        self._mark(me, r, w)

    def dma(self, q, out, in_, key, r=(), w=()):
        waits = self._deps(q, r, w)
        dk = 'dma:' + key
        self.dma_cnt[dk] = self.dma_cnt.get(dk, 0) + 1
        me = (dk, self.dma_cnt[dk])
        self.snap[me] = dict(self.clock[q])
        self.ops[q].append((waits, lambda e: e.dma_start(out=out, in_=in_), dk, 16))
        self._mark(me, r, w)

    def finish(self, final_reads=()):
        waits = self._deps('sp', list(final_reads), [])
        nc = self.nc
        semval = lambda k, n: (self.sem(k), n * 16 if k.startswith('dma:') else n)
        for k in self.ops:
            self.sem(k)
        final = [semval(k, n) for k, n in waits]
        for k in ('pe', 'act', 'dve', 'pool'):
            if self.cnt[k] > 0:
                final.append((self.sem(k), self.cnt[k]))
        for dk, n in self.dma_cnt.items():
            final.append((self.sem(dk), 16 * n))
        emit_lists = {}
        for eng, lst in self.ops.items():
            el = []
            for waits_, fn, inck, incv in lst:
                el.append(([semval(k, n) for k, n in waits_], fn, self.sem(inck), incv))
            emit_lists[eng] = el
        with nc.Block() as block:
            def run(e, el, extra=()):
                for ws, fn, s, v in el:
                    for sm, val in ws:
                        e.wait_ge(sm, val)
                    fn(e).then_inc(s, v)
                for sm, val in extra:
                    e.wait_ge(sm, val)

            @block.sync
            def _(e):
                run(e, emit_lists['sp'], final)

            @block.tensor
            def _(e):
                run(e, emit_lists['pe'])

            @block.scalar
            def _(e):
                run(e, emit_lists['act'])

            @block.vector
            def _(e):
                run(e, emit_lists['dve'])

            @block.gpsimd
            def _(e):
                run(e, emit_lists['pool'])


D = 1024
NCOL = 3352
ORIG = dict(rq=(0, 256), rk=(256, 512), rv=(512, 768), rg=(768, 1024), dq=(1024, 1280), dk=(1280, 1536),
            dv=(1536, 1792), dbeta=(1792, 1796), da=(1796, 1800), dg=(1800, 2056), su=(2056, 2312),
            sg=(2312, 2568), gq=(2568, 2696), gk=(2696, 2824), gv=(2824, 3080), gcode=(3080, 3096),
            gg=(3096, 3352))
ORDER = ['rq', 'rk', 'rv', 'gv', 'dq', 'dk', 'dv', 'su', 'gq', 'gk', 'gcode', 'dbeta', 'da', 'rg', 'dg', 'sg', 'gg']
COL = {}
_o = 0
for _k in ORDER:
    _w = ORIG[_k][1] - ORIG[_k][0]
    COL[_k] = (_o, _o + _w)
    _o += _w
PERM = np.concatenate([np.arange(*ORIG[k]) for k in ORDER])
BANKS = [(0, 512), (512, 1024), (1024, 1536), (1536, 2048), (2048, 2328), (2328, 2840), (2840, 3352)]
C = 128
GAMMA = [1.0 - 2.0 ** (-5.0 - h) for h in range(4)]


def build_program(NT, NL, dbg=False, upto=99, do_setup=True):
    nc = bass.Bass("TRN2", target_bir_lowering=False)
    R = Rec(nc)
    din = lambda name, shape, dt=F32: nc.dram_tensor(name, list(shape), dt, kind="ExternalInput").ap()
    x_in = din("x", [NT * 128, D])
    x_out = nc.dram_tensor("out", [NT * 128, D], F32, kind="ExternalOutput").ap()
    xs_dram = [x_in]
    for l in range(NL - 1):
        xs_dram.append(nc.dram_tensor("xmid%d" % l, [NT * 128, D], F32).ap())
    xs_dram.append(x_out)
    d_dbg = nc.dram_tensor("dbg", [NT * 128, D], BF16, kind="ExternalOutput").ap() if dbg else None
    d_win = din("w_in", [NL, D, NCOL])
    d_wout = din("w_out", [NL, D, D])
    d_gpre = din("gpre", [NL, 128, 8])
    d_gpost = din("gpost", [NL, 1, D])
    d_ng = din("ng", [NL, 128, 8])
    d_cw = din("cw", [NL, 1, 4 * 768])
    d_dnp = din("dnp", [NL, 1, 8])
    d_s5p = din("s5p", [NL, 128, 24])
    d_bt = din("s5bt", [NL, 128, 2 * 8 * 128])
    d_ct = din("s5ct", [NL, 128, 2 * 8 * 32])
    d_dg = din("s5dg", [NL, 128, 8 * 32])
    d_wglu = din("wglu", [NL, 128, 2 * 256])
    d_bglu = din("bglu", [NL, 1, 256])
    d_wgk = din("wgk", [NL, 16, 128])
    d_bgk = din("bgk", [NL, 1, 128])
    d_ropec = din("ropec", [NT, 128, 512])
    d_ropes = din("ropes", [NT, 128, 512])
    d_cb = din("cb", [128, 128 * 8 + 512 * 3 + 14 * 128], BF16)
    d_cf = din("cf", [128, 128 * 3 + 512 + 256])

    sb, ps = R.sb, R.ps
    cb = sb("cb", [128, 128 * 8 + 1536 + 14 * 128], BF16)
    cf = sb("cf", [128, 128 * 3 + 768], F32)
    identb = cb[:, 0:128]
    Sh = [cb[:, 128 * (1 + s):128 * (2 + s)] for s in range(4)]
    ShP = [None] + [cb[:, 128 * (4 + s):128 * (5 + s)] for s in range(1, 4)]
    Ui4 = cb[:, 1024:1536]
    negLs4 = cb[:, 1536:2048]
    negUs4 = cb[:, 2048:2560]
    HM = [cb[:, 2560 + i * 128:2560 + (i + 1) * 128] for i in range(14)]
    identf = cf[:, 0:128]
    triU = cf[:, 128:256]
    ones = cf[:, 256:384]
    Esel = cf[0:4, 384:896]
    GC = cf[0:64, 896:1152]
    R.dma('sp', cb[:], d_cb[:, :], key='cb', w=['cb'])
    R.dma('sp', cf[:], d_cf[:, :], key='cf', w=['cf'])
    onesb = sb("onesb", [1, 128], BF16)
    R.op('dve', lambda e: e.tensor_copy(out=onesb[:], in_=cf[0:1, 256:384]), r=['cf'], w=['onesb'])

    Wb = sb("Wb", [128, 8, NCOL], BF16)
    Wo = sb("Wo", [128, 8, D], BF16)
    stage = sb("stage", [128, NCOL], F32)
    p = stage
    gpre = sb("gpre", [128, 8], F32)
    Gpost = sb("Gpost", [128, D], F32)
    NG = sb("NG", [128, 8], F32)
    cw = sb("cw", [128, 4, 768], BF16)
    dnp = sb("dnp", [128, 8], F32)
    s5p = sb("s5p", [128, 24], F32)
    BT = sb("BT", [128, 2, 8, 128], BF16)
    CT = sb("CT", [128, 2, 8, 32], BF16)
    Dg = sb("Dg", [128, 8, 32], BF16)
    Wglu = sb("Wglu", [128, 2, 256], BF16)
    bgluf = sb("bgluf", [1, 256], F32)
    bglu = sb("bglu", [1, 256], BF16)
    wgkf = sb("wgkf", [16, 128], F32)
    wgk = sb("wgk", [16, 128], BF16)
    bgkf = sb("bgkf", [1, 128], F32)
    bgk = sb("bgk", [1, 128], BF16)
    Tc = sb("Tc", [128, 8, 128], F32)
    Ts = sb("Ts", [128, 8, 128], F32)
    Oc = sb("Oc", [128, 8, 128], F32)
    Os = sb("Os", [128, 8, 128], F32)
    MAGz = sb("MAGz", [128, 8, 128], F32)
    s5t = sb("s5t", [128, 40, 8], F32)
    Rr32 = sb("Rr32", [64, 256], F32)
    Rrb = sb("Rrb", [64, 256], BF16)
    Sd32 = sb("Sd32", [64, 256], F32)
    Sdb = sb("Sdb", [64, 256], BF16)
    Gs32 = sb("Gs32", [32, 256], F32)
    Gsb = sb("Gsb", [32, 256], BF16)
    mc = sb("mc", [128, 2, 8], F32)
    xt1 = sb("xt", [128, D], F32)
    xt = [xt1, xt1]
    ropeC1 = sb("ropeC", [128, 512], F32)
    ropeS1 = sb("ropeS", [128, 512], F32)
    ropeC = [ropeC1, ropeC1]
    ropeS = [ropeS1, ropeS1]
    sm = sb("sm", [128, 72], F32)
    xs = sb("xs", [128, D], BF16)
    yb = xs
    hT = sb("hT", [128, 8, 128], BF16)
    ngg = sb("ngg", [128, D], BF16)
    qk = sb("qk", [128, 512], BF16)
    vb = sb("vb", [128, 512], BF16)
    sT = sb("sT", [128, 512], BF16)
    tmpR = sb("tmpR", [64, 256], F32)
    pk = [sb("pk%d" % i, [128, 4, 768], BF16) for i in range(2)]
    qn = sb("qn", [128, 256], BF16)
    kn = sb("kn", [128, 256], BF16)
    kbq = sb("kbq", [128, 512], BF16)
    dT = sb("dT", [64, 16, 128], BF16)
    qkT = dT[:, 0:8, :]
    gct = sb("gct", [4, 128], F32)
    PP = sb("PP", [128, 4, 512], BF16)
    Pm = [PP[:, 0, :], PP[:, 2, :]]
    PTm = [PP[:, 1, :], PP[:, 3, :]]
    attnT = sb("attnT", [128, 512], BF16)
    Xb = sb("Xb", [128, 4, 128], BF16)
    WT = sb("WT", [64, 4, 128], BF16)
    vnew = sb("vnew", [128, 256], BF16)
    kg = sb("kg", [128, 256], BF16)
    ub = sb("ub", [128, 256], BF16)
    uT = sb("uT", [128, 2, 128], BF16)
    A = [sb("A%d" % i, [128, 8, 128], F32) for i in range(4)]
    _fl = lambda a_: a_.rearrange("p a b -> p (a b)")
    tmpL, tmpU = _fl(A[0][:, 0:4, :]), _fl(A[0][:, 4:8, :])
    DLs, DUs = tmpL, tmpU
    DUi, X32 = _fl(A[1][:, 0:4, :]), A[1][:, 4:8, :]
    cs = _fl(A[2][:])[:, 0:768]
    sq = _fl(A[3][:])[:, 0:512]
    xo = [_fl(A[0][:]), _fl(A[0][:])]
    yT, gT, gsT = hT, qkT[0:32], sT
    sre = PP[:, 0:2, :].rearrange("p a (b c) -> p (a b) c", c=128)
    sim = PP[:, 2:4, :].rearrange("p a (b c) -> p (a b) c", c=128)
    ygb = sb("ygb", [128, 256], BF16)
    ygT = sb("ygT", [128, 2, 128], BF16)
    gcb = sb("gcb", [128, 16], BF16)
    gcT = sb("gcT", [16, 128], BF16)
    Gb = sb("Gb", [128, 768], F32)
    g128 = [Gb[:, i * 128:(i + 1) * 128] for i in range(6)]
    t256, u256, yg = Gb[:, 0:256], Gb[:, 256:512], Gb[:, 512:768]
    gq3 = sb("gq3", [128, 384], BF16)
    PS = [ps("PS%d" % i, [128, 1024], F32) for i in range(4)]
    bank_ctr = [0]

    def bank():
        i = bank_ctr[0] % 8
        bank_ctr[0] += 1
        return PS[i // 2][:, (i % 2) * 512:(i % 2) * 512 + 512], 'PS%d_%d' % (i // 2, i % 2)

    def dbank():
        if bank_ctr[0] % 2:
            bank_ctr[0] += 1
        i = bank_ctr[0] % 8
        bank_ctr[0] += 2
        return PS[i // 2], ['PS%d_0' % (i // 2), 'PS%d_1' % (i // 2)]

    op = R.op
    dumps = []

    def dump(tag, ap, names):
        if not dbg:
            return
        d = nc.dram_tensor("dump_" + tag, list(ap.shape), ap.dtype, kind="ExternalOutput").ap()
        R.dma('sp', d, ap, key='dump_' + tag, r=names, w=['dumpout_' + tag])
        dumps.append('dumpout_' + tag)
    TT = lambda out, a, b, o: (lambda e: e.tensor_tensor(out=out, in0=a, in1=b, op=o))
    TS = lambda out, a, s1, s2, o0, o1=None: (lambda e: e.tensor_scalar(out=out, in0=a, scalar1=s1, scalar2=s2, op0=o0, **({} if o1 is None else {'op1': o1})))
    STT = lambda out, a, s, b, o0, o1: (lambda e: e.scalar_tensor_tensor(out=out, in0=a, scalar=s, in1=b, op0=o0, op1=o1))
    ACT = lambda out, a, f, **kw: (lambda e: e.activation(out=out, in_=a, func=f, **kw))
    CP = lambda out, a: (lambda e: e.tensor_copy(out=out, in_=a))
    MM = lambda out, l, r_, st=True, sp=True: (lambda e: e.matmul(out, lhsT=l, rhs=r_, start=st, stop=sp))
    TR = lambda out, a, idn: (lambda e: e.transpose(out=out, in_=a, identity=idn))
    mult, add, sub = ALU.mult, ALU.add, ALU.subtract

    def rsqrt_small(ap, names, scale, eps):
        op('dve', TS(ap, ap, scale, eps, mult, add), r=names, w=names)
        op('act', ACT(ap, ap, AF.Sqrt), r=names, w=names)
        op('dve', lambda e: e.reciprocal(out=ap, in_=ap), r=names, w=names)

    def setup(l):
        R.dma('sp', gpre[:], d_gpre[l], key='gpre', w=['gpre'])
        R.dma('sp', NG[:], d_ng[l], key='ng', w=['NG'])
        for c in range(8):
            R.dma('sp', stage[:], d_win[l, c * 128:(c + 1) * 128, :], key='stage', w=['p%d' % i for i in range(7)])
            op('dve', TS(Wb[:, c, :], stage[:], gpre[:, c:c + 1], None, mult), r=['p%d' % i for i in range(7)] + ['gpre'], w=['Wb'])
        for c in range(8):
            R.dma('sp', stage[:, 0:D], d_wout[l, c * 128:(c + 1) * 128, :], key='stage', w=['p%d' % i for i in range(7)])
            op('act', ACT(Wo[:, c, :], stage[:, 0:D], AF.Copy, scale=NG[:, c:c + 1]), r=['p%d' % i for i in range(7)] + ['NG'], w=['Wo'])
        R.dma('sp', Gpost[:], d_gpost[l].partition_broadcast(128), key='gpost', w=['Gpost'])
        R.dma('sp', stage[:, 0:3072], d_cw[l].partition_broadcast(128), key='stage', w=['p%d' % i for i in range(7)])
        op('dve', CP(cw[:].rearrange("p a b -> p (a b)"), stage[:, 0:3072]), r=['p%d' % i for i in range(7)], w=['cw'])
        R.dma('sp', dnp[:], d_dnp[l].partition_broadcast(128), key='dnp', w=['dnp'])
        R.dma('sp', s5p[:], d_s5p[l], key='s5p', w=['s5p'])
        BTf, CTf, Dgf, Wgluf = stage[:, 0:2048], stage[:, 2048:2560], stage[:, 2560:2816], stage[:, 2816:3328]
        PN = ['p%d' % i for i in range(7)]
        R.dma('sp', BTf, d_bt[l], key='stage', w=PN)
        R.dma('sp', CTf, d_ct[l], key='stage2', w=PN)
        R.dma('sp', Dgf, d_dg[l], key='stage3', w=PN)
        R.dma('sp', Wgluf, d_wglu[l], key='stage4', w=PN)
        R.dma('sp', bgluf[:], d_bglu[l], key='bglu', w=['bgluf'])
        R.dma('sp', wgkf[:], d_wgk[l], key='wgk', w=['wgkf'])
        R.dma('sp', bgkf[:], d_bgk[l], key='bgk', w=['bgkf'])
        op('dve', CP(BT[:].rearrange("p a b c -> p (a b c)"), BTf), r=PN, w=['BT'])
        op('dve', CP(CT[:, 0].rearrange("p b c -> p (b c)"), CTf[:, 0:256]), r=PN, w=['CT'])
        op('dve', TS(CT[:, 1].rearrange("p b c -> p (b c)"), CTf[:, 256:512], -1.0, None, mult), r=PN, w=['CT'])
        op('dve', CP(Dg[:].rearrange("p b c -> p (b c)"), Dgf), r=PN, w=['Dg'])
        op('dve', CP(Wglu[:].rearrange("p b c -> p (b c)"), Wgluf), r=PN, w=['Wglu'])
        op('dve', CP(bglu[:], bgluf[:]), r=['bgluf'], w=['bglu'])
        op('dve', CP(wgk[:], wgkf[:]), r=['wgkf'], w=['wgk'])
        op('dve', CP(bgk[:], bgkf[:]), r=['bgkf'], w=['bgk'])
        op('act', ACT(dnp[:, 0:4], dnp[:, 0:4], AF.Exp), r=['dnp'], w=['dnp'])
        op('dve', TS(dnp[:, 0:4], dnp[:, 0:4], -1.0, None, mult), r=['dnp'], w=['dnp'])
        for nm, t_ in (('Rr32', Rr32), ('Sd32', Sd32), ('Gs32', Gs32), ('Rrb', Rrb), ('Sdb', Sdb), ('Gsb', Gsb), ('mc', mc)):
            ap_ = t_[:] if len(t_.shape) == 2 else t_[:].rearrange("p a b -> p (a b)")
            op('pool', lambda e, ap_=ap_: e.memset(ap_, 0.0), r=[], w=[nm])
        for i in range(2):
            op('pool', lambda e, i=i: e.memset(pk[i][:].rearrange("p a b -> p (a b)"), 0.0), r=[], w=['pk%d' % i])
        T = lambda i: s5t[:, i, :]
        S = ['s5t']
        lre, lim, ldt = s5p[:, 0:8], s5p[:, 8:16], s5p[:, 16:24]
        dt_, mag, th, xx, x2, sn, cs_, t0, t1 = T(0), T(1), T(2), T(3), T(4), T(5), T(6), T(7), T(8)
        op('act', ACT(dt_, ldt, AF.Exp), r=['s5p'], w=S)
        op('dve', TT(mag, lre, dt_, mult), r=['s5p'] + S, w=S)
        op('act', ACT(mag, mag, AF.Exp), r=S, w=S)
        op('dve', TT(th, lim, dt_, mult), r=['s5p'] + S, w=S)
        op('dve', TS(xx, th, 1.0 / 16, None, mult), r=S, w=S)
        op('dve', TT(x2, xx, xx, mult), r=S, w=S)
        sc = [1.0, -1.0 / 6, 1.0 / 120, -1.0 / 5040, 1.0 / 362880, -1.0 / 39916800, 1.0 / 6227020800]
        cc = [1.0, -1.0 / 2, 1.0 / 24, -1.0 / 720, 1.0 / 40320, -1.0 / 3628800, 1.0 / 479001600, -1.0 / 87178291200]
        op('dve', TS(sn, x2, sc[6], sc[5], mult, add), r=S, w=S)
        for k_ in (4, 3, 2, 1, 0):
            op('dve', TT(sn, sn, x2, mult), r=S, w=S)
            op('dve', TS(sn, sn, sc[k_], None, add), r=S, w=S)
        op('dve', TT(sn, sn, xx, mult), r=S, w=S)
        op('dve', TS(cs_, x2, cc[7], cc[6], mult, add), r=S, w=S)
        for k_ in (5, 4, 3, 2, 1, 0):
            op('dve', TT(cs_, cs_, x2, mult), r=S, w=S)
            op('dve', TS(cs_, cs_, cc[k_], None, add), r=S, w=S)
        for _ in range(4):
            op('dve', TT(t0, cs_, cs_, mult), r=S, w=S)
            op('dve', TT(t1, sn, sn, mult), r=S, w=S)
            op('dve', TT(sn, sn, cs_, mult), r=S, w=S)
            op('dve', TS(sn, sn, 2.0, None, mult), r=S, w=S)
            op('dve', TT(cs_, t0, t1, sub), r=S, w=S)
        are, aim, den, cre, cim = T(9), T(10), T(11), T(12), T(13)
        op('dve', TT(are, mag, cs_, mult), r=S, w=S)
        op('dve', TT(aim, mag, sn, mult), r=S, w=S)
        op('dve', TT(t0, lre, lre, mult), r=['s5p'] + S, w=S)
        op('dve', TT(t1, lim, lim, mult), r=['s5p'] + S, w=S)
        op('dve', TT(den, t0, t1, add), r=S, w=S)
        op('dve', lambda e: e.reciprocal(out=den, in_=den), r=S, w=S)
        nr = T(14)
        op('dve', TS(nr, are, -1.0, None, add), r=S, w=S)
        op('dve', TT(t0, nr, lre, mult), r=['s5p'] + S, w=S)
        op('dve', TT(t1, aim, lim, mult), r=['s5p'] + S, w=S)
        op('dve', TT(cre, t0, t1, add), r=S, w=S)
        op('dve', TT(cre, cre, den, mult), r=S, w=S)
        op('dve', TT(t0, aim, lre, mult), r=['s5p'] + S, w=S)
        op('dve', TT(t1, nr, lim, mult), r=['s5p'] + S, w=S)
        op('dve', TT(cim, t0, t1, sub), r=S, w=S)
        op('dve', TT(cim, cim, den, mult), r=S, w=S)
        op('pool', lambda e: e.memset(Oc[:, :, 0:1], 1.0), r=[], w=['Oc'])
        op('pool', lambda e: e.memset(Os[:, :, 0:1], 0.0), r=[], w=['Os'])
        op('dve', CP(Oc[:, :, 1], cs_), r=S, w=['Oc'])
        op('dve', CP(Os[:, :, 1], sn), r=S, w=['Os'])
        OO = ['Oc', 'Os']
        k_ = 1
        while k_ < 128:
            bc = lambda t_, k_=k_: t_[:, :, k_:k_ + 1].to_broadcast([128, 8, k_])
            hi = slice(k_ + 1, 2 * k_ + 1) if 2 * k_ + 1 <= 128 else slice(k_ + 1, 128)
            n_ = hi.stop - hi.start
            lo = slice(1, 1 + n_)
            bcn = lambda t_, k_=k_, n_=n_: t_[:, :, k_:k_ + 1].to_broadcast([128, 8, n_])
            a1, a2 = A[0][:, :, 0:n_], A[1][:, :, 0:n_]
            op('dve', TT(a1, Oc[:, :, lo], bcn(Oc), mult), r=OO, w=['A0'])
            op('dve', TT(a2, Os[:, :, lo], bcn(Os), mult), r=OO, w=['A1'])
            op('dve', TT(Oc[:, :, hi], a1, a2, sub), r=['A0', 'A1'] + OO, w=['Oc'])
            op('dve', TT(a1, Oc[:, :, lo], bcn(Os), mult), r=OO, w=['A0'])
            op('dve', TT(a2, Os[:, :, lo], bcn(Oc), mult), r=OO, w=['A1'])
            op('dve', TT(Os[:, :, hi], a1, a2, add), r=['A0', 'A1'] + OO, w=['Os'])
            k_ *= 2
        bc8 = lambda t_: t_.unsqueeze(2).to_broadcast([128, 8, 128])
        op('dve', TT(A[0][:], Oc[:], bc8(cre), mult), r=OO + S, w=['A0'])
        op('dve', TT(A[1][:], Os[:], bc8(cim), mult), r=OO + S, w=['A1'])
        op('dve', TT(Tc[:], A[0][:], A[1][:], add), r=['A0', 'A1'], w=['Tc'])
        op('dve', TT(A[0][:], Oc[:], bc8(cim), mult), r=OO + S, w=['A0'])
        op('dve', TT(A[1][:], Os[:], bc8(cre), mult), r=OO + S, w=['A1'])
        op('dve', TT(Ts[:], A[0][:], A[1][:], sub), r=['A0', 'A1'], w=['Ts'])
        op('dve', CP(MAGz[:], bc8(mag)), r=S, w=['MAGz'])
        op('pool', lambda e: e.memset(MAGz[:, :, 0:1], 0.0), r=[], w=['MAGz'])
        mec, mes = T(15), T(16)
        op('dve', TT(t0, Oc[:, :, 127], cs_, mult), r=OO + S, w=S)
        op('dve', TT(t1, Os[:, :, 127], sn, mult), r=OO + S, w=S)
        op('dve', TT(mec, t0, t1, sub), r=S, w=S)
        op('dve', TT(t0, Oc[:, :, 127], sn, mult), r=OO + S, w=S)
        op('dve', TT(t1, Os[:, :, 127], cs_, mult), r=OO + S, w=S)
        op('dve', TT(mes, t0, t1, add), r=S, w=S)
        op('dve', TT(mec, mec, mag, mult), r=S, w=S)
        op('dve', TT(mes, mes, mag, mult), r=S, w=S)

    def branch_out(obank, obn, br):
        op('act', ACT(sq[:, 0:256], obank, AF.Square), r=[obn], w=['A3'])
        ssq = sm[:, 8 + 4 * br: 12 + 4 * br]
        nm = ['sm_b%d' % br]
        op('dve', lambda e: e.tensor_reduce(out=ssq, in_=sq[:, 0:256].rearrange("p (h d) -> p h d", h=4), axis=AX.X, op=add), r=['A3'], w=nm)
        rsqrt_small(ssq, nm, 1.0 / 64, 1e-6)
        for h in range(4):
            c0 = br * 256 + h * 64
            op('dve', STT(yb[:, c0:c0 + 64], obank[:, h * 64:(h + 1) * 64], ssq[:, h:h + 1], ngg[:, c0:c0 + 64], mult, mult),
               r=[obn, 'ngg', 'hT'] + nm, w=['yb%d' % br])

    def early(l, n, dst, b):
        R.dma('sp', dst[n * 128:(n + 1) * 128, :], xt[b][:], key='xo', r=['xt'], w=['dst%d_%d' % (l, n)])

    def tile(l, n, src, dst):
        b = n % 2
        R.dma('sp', xt[b][:], src[n * 128:(n + 1) * 128, :], key='xt', r=(['dst%d_%d' % (l - 1, n)] if l > 0 else []), w=['xt'])
        R.dma('sp', ropeC[b][:], d_ropec[n], key='rc', w=['rc'])
        R.dma('sp', ropeS[b][:], d_ropes[n], key='rs', w=['rs'])
        X = 'xt'
        op('act', ACT(A[3][:].rearrange('p a b -> p (a b)'), xt[b][:], AF.Square, accum_out=sm[:, 0:1]), r=[X], w=['A3', 'sm0'])
        rsqrt_small(sm[:, 0:1], ['sm0'], 1.0 / D, 1e-6)
        op('act', ACT(xs[:], xt[b][:], AF.Copy, scale=sm[:, 0:1]), r=[X, 'sm0'], w=['xs', 'yb0', 'yb1', 'yb2', 'yb3'])
        bk, bn = bank()
        bkb = bk.bitcast(BF16)
        for c in range(8):
            op('pe', TR(bkb[:, c * 128:(c + 1) * 128], xs[:, c * 128:(c + 1) * 128], identb), r=['xs', 'cb'], w=[bn])
        op('dve', CP(hT[:].rearrange("p a b -> p (a b)"), bkb), r=[bn], w=['hT'])
        for i, (c0, c1) in enumerate(BANKS):
            bk, bn = bank()
            for c in range(8):
                op('pe', MM(bk[:, 0:c1 - c0], hT[:, c, :], Wb[:, c, c0:c1], c == 0, c == 7), r=['hT', 'Wb'], w=[bn])
            if i % 2 == 0:
                op('act', ACT(p[:, c0:c1], bk[:, 0:c1 - c0], AF.Copy), r=[bn], w=['p%d' % i])
            else:
                op('dve', CP(p[:, c0:c1], bk[:, 0:c1 - c0]), r=[bn], w=['p%d' % i])
        if upto < 1:
            return early(l, n, dst, b)
        g0 = COL['rg'][0]
        op('act', ACT(ngg[:], p[:, g0:g0 + D], AF.Silu), r=['p5', 'p6'], w=['ngg'])
        if upto < 2:
            return early(l, n, dst, b)
        RC, RS = ropeC[b], ropeS[b]
        A3f = A[3][:].rearrange('p a b -> p (a b)')
        m1, m2 = A3f[:, 0:512], A3f[:, 512:1024]
        op('dve', TT(m1, p[:, 0:512], RC[:], mult), r=['p0', 'rc'], w=['A3'])
        pv = p[:, 0:512].rearrange("p (a t) -> p a t", t=2)
        m2v = m2.rearrange("p (a t) -> p a t", t=2)
        sv = RS[:].rearrange("p (a t) -> p a t", t=2)
        op('dve', TT(m2v[:, :, 0], pv[:, :, 1], sv[:, :, 0], mult), r=['p0', 'rs'], w=['A3'])
        op('dve', TT(m2v[:, :, 1], pv[:, :, 0], sv[:, :, 1], mult), r=['p0', 'rs'], w=['A3'])
        op('dve', TT(qk[:], m1, m2, add), r=['A3'], w=['qk'])
        op('act', ACT(vb[:], p[:, 512:1024], AF.Copy), r=['p1'], w=['vb'])
        bk, bn = bank()
        bkb = bk.bitcast(BF16)
        for j in range(8):
            op('pe', TR(bkb[0:64, j * 128:(j + 1) * 128], qk[:, j * 64:(j + 1) * 64], identb), r=['qk', 'cb'], w=[bn])
        op('dve', CP(qkT[:].rearrange("p a b -> p (a b)"), bkb[0:64, :]), r=[bn], w=['dT0'])
        bk, bn = bank()
        for h in range(4):
            op('pe', MM(bk[:, h * 128:(h + 1) * 128], qkT[:, 4 + h, :], qkT[:, h, :]), r=['dT0'], w=[bn])
        op('dve', TT(sT[:], bk, Ui4, mult), r=[bn, 'cb'], w=['sT'])
        bo, bon = bank()
        for h in range(4):
            op('pe', MM(bo[:, h * 64:(h + 1) * 64], sT[:, h * 128:(h + 1) * 128], vb[:, h * 64:(h + 1) * 64], True, False), r=['sT', 'vb'], w=[bon])
            op('pe', MM(bo[:, h * 64:(h + 1) * 64], qkT[:, h, :], Rrb[:, h * 64:(h + 1) * 64], False, True), r=['dT0', 'Rrb'], w=[bon])
        bk, bn = bank()
        for h in range(4):
            op('pe', MM(bk[0:64, h * 64:(h + 1) * 64], qk[:, 256 + h * 64:256 + (h + 1) * 64], vb[:, h * 64:(h + 1) * 64]), r=['qk', 'vb'], w=[bn])
        op('dve', TT(tmpR[:], Rr32[:], bk[0:64, 0:256], add), r=['Rr32', bn], w=['tmpR'])
        op('pool', TT(Rr32[:], tmpR[:], GC, mult), r=['tmpR', 'cf'], w=['Rr32'])
        op('act', ACT(Rrb[:], Rr32[:], AF.Copy), r=['Rr32'], w=['Rrb'])
        branch_out(bo[:, 0:256], bon, 0)
        if upto < 3:
            return early(l, n, dst, b)
        c0 = COL['dq'][0]
        for k in range(4):
            op('pool', TT(pk[b][:, k, :], p[:, c0:c0 + 768], cw[:, k, :], mult), r=['p2', 'p3', 'cw'], w=['pk%d' % b])
        bq, bqn = bank()
        bv, bvn = bank()
        for (bk, bn, o0, w_) in ((bq, bqn, 0, 512), (bv, bvn, 512, 256)):
            taps = [(Sh[3 - k], pk[b][:, k, o0:o0 + w_], 'pk%d' % b) for k in range(4)]
            taps += [(ShP[3 - k], pk[1 - b][:, k, o0:o0 + w_], 'pk%d' % (1 - b)) for k in range(3)]
            for i, (lt, rh, nm) in enumerate(taps):
                op('pe', MM(bk[:, 0:w_], lt, rh, i == 0, i == len(taps) - 1), r=['cb', nm], w=[bn])
        if upto < 3.1:
            return early(l, n, dst, b)
        op('act', ACT(cs[:, 0:512], bq, AF.Silu), r=[bqn], w=['A2'])
        op('act', ACT(cs[:, 512:768], bv[:, 0:256], AF.Silu), r=[bvn], w=['A2'])
        op('act', ACT(sq[:], cs[:, 0:512], AF.Square), r=['A2'], w=['A3'])
        rn = sm[:, 24:32]
        op('dve', lambda e: e.tensor_reduce(out=rn, in_=sq[:].rearrange("p (h d) -> p h d", h=8), axis=AX.X, op=add), r=['A3'], w=['rn'])
        rsqrt_small(rn, ['rn'], 1.0, 1e-6)
        for h in range(4):
            op('dve', TS(qn[:, h * 64:(h + 1) * 64], cs[:, h * 64:(h + 1) * 64], rn[:, h:h + 1], 0.125, mult, mult), r=['A2', 'rn'], w=['qn'])
            op('dve', TS(kn[:, h * 64:(h + 1) * 64], cs[:, 256 + h * 64:256 + (h + 1) * 64], rn[:, 4 + h:5 + h], None, mult), r=['A2', 'rn'], w=['kn'])
        beta, gz, gg_, gc, eg, egl, egll, beg = (sm[:, 32:36], sm[:, 36:40], sm[:, 40:44], sm[:, 44:52], sm[:, 52:56],
                                                  sm[:, 56:60], sm[:, 60:64], sm[:, 4:8])
        DS = ['dsm']
        cb_, ca_ = COL['dbeta'][0], COL['da'][0]
        op('act', ACT(beta, p[:, cb_:cb_ + 4], AF.Sigmoid), r=['p4'], w=DS)
        op('dve', TT(gz, p[:, ca_:ca_ + 4], dnp[:, 4:8], add), r=['p4', 'dnp'], w=DS)
        op('act', ACT(gz, gz, AF.Exp), r=DS, w=DS)
        op('act', ACT(gz, gz, AF.Ln, bias=1.0), r=DS, w=DS)
        op('dve', TT(gg_, gz, dnp[:, 0:4], mult), r=DS + ['dnp'], w=DS)
        if upto < 3.2:
            return early(l, n, dst, b)
        bg, bgn = bank()
        op('pe', MM(bg[:, 0:4], triU, gg_), r=['cf'] + DS, w=[bgn])
        op('pe', MM(bg[:, 4:8], ones, gg_), r=['cf'] + DS, w=[bgn])
        op('pe', MM(bg[0:4, 128:256], gg_, triU), r=['cf'] + DS, w=[bgn])
        op('dve', CP(gc, bg[:, 0:8]), r=[bgn], w=DS)
        op('dve', CP(gct[:], bg[0:4, 128:256]), r=[bgn], w=['gct'])
        op('act', ACT(eg, gc[:, 0:4], AF.Exp), r=DS, w=DS)
        op('dve', TT(egl, gc[:, 4:8], gc[:, 0:4], sub), r=DS, w=DS)
        op('act', ACT(egl, egl, AF.Exp), r=DS, w=DS)
        op('act', ACT(egll, gc[:, 4:8], AF.Exp), r=DS, w=DS)
        op('dve', TT(beg, beta, eg, mult), r=DS, w=DS)
        if upto < 3.3:
            return early(l, n, dst, b)
        bR, bRn = bank()
        for h in range(4):
            op('pe', MM(bR[:, h * 128:(h + 1) * 128], Esel[:, h * 128:(h + 1) * 128], gct[:]), r=['cf', 'gct'], w=[bRn])
        for h in range(4):
            hs = slice(h * 128, (h + 1) * 128)
            op('dve', TS(tmpL[:, hs], bR[:, hs], gc[:, h:h + 1], 0.0, sub, ALU.max), r=[bRn] + DS, w=['A0'])
            op('dve', TS(tmpU[:, hs], bR[:, hs], gc[:, h:h + 1], 0.0, sub, ALU.min), r=[bRn] + DS, w=['A0'])
        if upto < 3.4:
            return early(l, n, dst, b)
        op('act', ACT(tmpL[:], tmpL[:], AF.Exp, scale=-1.0), r=['A0'], w=['A0'])
        op('act', ACT(tmpU[:], tmpU[:], AF.Exp), r=['A0'], w=['A0'])
        op('pool', TT(DLs[:], tmpL[:], negLs4, mult), r=['A0', 'cb'], w=['A0'])
        op('pool', TT(DUi[:], tmpU[:], Ui4, mult), r=['A0', 'cb'], w=['A1'])
        op('pool', TT(DUs[:], tmpU[:], negUs4, mult), r=['A0', 'cb'], w=['A0'])
        if l == 0 and n == 0:
            dump('cs', cs, ['A2']); dump('kn', kn[:], ['kn']); dump('qn', qn[:], ['qn']); dump('sm', sm[:, 24:64], DS + ['rn'])
            dump('DLs', DLs, ['A0']); dump('DUs', DUs, ['A0']); dump('DUi', DUi, ['A1'])
        for h in range(4):
            hs = slice(h * 64, (h + 1) * 64)
            op('dve', TS(kbq[:, hs], kn[:, hs], beta[:, h:h + 1], None, mult), r=['kn'] + DS, w=['kbq'])
            op('dve', TS(kbq[:, 256 + h * 64:256 + (h + 1) * 64], qn[:, hs], eg[:, h:h + 1], None, mult), r=['qn'] + DS, w=['kbq'])
        if upto < 3.5:
            return early(l, n, dst, b)
        srcs = [(kn, 0, 'kn'), (kbq, 0, 'kbq'), (qn, 0, 'qn'), (kbq, 256, 'kbq')]
        for half in range(2):
            bk, bn = bank()
            bkb = bk.bitcast(BF16)
            for jj in range(8):
                j = half * 8 + jj
                t_, off, nm = srcs[j // 4]
                h = j % 4
                op('pe', TR(bkb[0:64, jj * 128:(jj + 1) * 128], t_[:, off + h * 64:off + (h + 1) * 64], identb), r=[nm, 'cb'], w=[bn])
            op('dve' if half == 0 else 'act', CP(dT[:, half * 8:(half + 1) * 8, :].rearrange("p a b -> p (a b)"), bkb[0:64, :]) if half == 0 else
               ACT(dT[:, half * 8:(half + 1) * 8, :].rearrange("p a b -> p (a b)"), bkb[0:64, :], AF.Copy), r=[bn], w=['dT%d' % half])
        bA, bAn = bank()
        bAT, bATn = bank()
        bat, batn = bank()
        for h in range(4):
            hs = slice(h * 128, (h + 1) * 128)
            op('pe', MM(bA[:, hs], dT[:, 4 + h, :], dT[:, h, :]), r=['dT0'], w=[bAn])
            op('pe', MM(bAT[:, hs], dT[:, h, :], dT[:, 4 + h, :]), r=['dT0'], w=[bATn])
            op('pe', MM(bat[:, hs], dT[:, h, :], dT[:, 8 + h, :]), r=['dT0', 'dT1'], w=[batn])
        op('dve', TT(Pm[0][:], bA, DLs[:], mult), r=[bAn, 'A0'], w=['Pm0'])
        op('dve', TT(PTm[0][:], bAT, DUs[:], mult), r=[bATn, 'A0'], w=['PTm0'])
        op('dve', TT(attnT[:], bat, DUi[:], mult), r=[batn, 'A1'], w=['attnT'])
        for h in range(4):
            hs = slice(h * 64, (h + 1) * 64)
            op('dve', TS(X32[:, h, 0:64], cs[:, 512 + h * 64:512 + (h + 1) * 64], beta[:, h:h + 1], None, mult), r=['A2'] + DS, w=['A1'])
            op('dve', TS(X32[:, h, 64:128], kn[:, hs], beg[:, h:h + 1], None, mult), r=['kn'] + DS, w=['A1'])
        if l == 0 and n == 0:
            dump('N0', Pm[0], ['Pm0']); dump('NT0', PTm[0], ['PTm0']); dump('attnT', attnT[:], ['attnT']); dump('X0', X32, ['A1'])
        X32f = X32[:].rearrange("p a b -> p (a b)")
        Xbf = Xb[:].rearrange("p a b -> p (a b)")
        op('act', ACT(Xbf, X32f, AF.Copy), r=['A1'], w=['Xb'])
        if upto < 3.6:
            return early(l, n, dst, b)
        Lm, LmT, Yb, W1b = sT[:], qk[:], kbq[:], xs[:, 512:1024]
        Tm, TTm = Pm[1], PTm[1]
        v4 = lambda a_: a_.rearrange("p (h c) -> p h c", h=4)
        bm = lambda i: HM[i].unsqueeze(1).to_broadcast([128, 4, 128])
        idb4 = identb.unsqueeze(1).to_broadcast([128, 4, 128])
        if os.environ.get("HV") == "1":
            op('dve', CP(Tm, Pm[0]), r=['Pm0'], w=['Pm1'])
            op('dve', CP(TTm, PTm[0]), r=['PTm0'], w=['PTm1'])
        elif os.environ.get("HV") == "4":
            op('dve', CP(kbq[:, 0:256], kn[:]), r=['kn'], w=['kbq'])
            op('dve', CP(kbq[:, 256:512], kn[:]), r=['kn'], w=['kbq'])
        elif os.environ.get("HV") == "5":
            op('act', ACT(Tm, Pm[0], AF.Copy), r=['Pm0'], w=['Pm1'])
        elif os.environ.get("HV") == "2":
            op('dve', TT(Tm, Pm[0], Pm[0], mult), r=['Pm0'], w=['Pm1'])
            op('dve', TT(TTm, PTm[0], PTm[0], mult), r=['PTm0'], w=['PTm1'])
        elif os.environ.get("HV") == "3":
            op('dve', TT(Tm, Pm[0], Ui4, mult), r=['Pm0', 'cb'], w=['Pm1'])
            op('dve', TT(TTm, PTm[0], Ui4, mult), r=['PTm0', 'cb'], w=['PTm1'])
        else:
            for h_ in range(4):
                    op('pool', TT(Tm[:, h_ * 128:(h_ + 1) * 128], Pm[0][:, h_ * 128:(h_ + 1) * 128], HM[0], mult), r=['Pm0', 'cb'], w=['Pm1'])
            for h_ in range(4):
                    op('pool', TT(Tm[:, h_ * 128:(h_ + 1) * 128], Tm[:, h_ * 128:(h_ + 1) * 128], identb, add), r=['Pm1', 'cb'], w=['Pm1'])
            for h_ in range(4):
                    op('pool', TT(TTm[:, h_ * 128:(h_ + 1) * 128], PTm[0][:, h_ * 128:(h_ + 1) * 128], HM[7], mult), r=['PTm0', 'cb'], w=['PTm1'])
            for h_ in range(4):
                    op('pool', TT(TTm[:, h_ * 128:(h_ + 1) * 128], TTm[:, h_ * 128:(h_ + 1) * 128], identb, add), r=['PTm1', 'cb'], w=['PTm1'])
        for lev in range(1, int(os.environ.get("NLEV", "7"))):
            for h_ in range(4):
                op('pool', TT(Lm[:, h_ * 128:(h_ + 1) * 128], Pm[0][:, h_ * 128:(h_ + 1) * 128], HM[lev], mult), r=['Pm0', 'cb'], w=['sT'])
            for h_ in range(4):
                op('pool', TT(LmT[:, h_ * 128:(h_ + 1) * 128], PTm[0][:, h_ * 128:(h_ + 1) * 128], HM[7 + lev], mult), r=['PTm0', 'cb'], w=['qk'])
            bY, bYn = bank()
            bW, bWn = bank()
            for h in range(4):
                hs = slice(h * 128, (h + 1) * 128)
                op('pe', MM(bY[:, hs], LmT[:, hs], Tm[:, hs]), r=['qk', 'Pm1'], w=[bYn])
                op('pe', MM(bW[:, hs], Lm[:, hs], TTm[:, hs]), r=['sT', 'PTm1'], w=[bWn])
            op('act', ACT(Yb, bY, AF.Copy), r=[bYn], w=['kbq'])
            op('dve', CP(W1b, bW), r=[bWn], w=['yb2', 'yb3'])
            bZ, bZn = bank()
            bW2, bW2n = bank()
            for h in range(4):
                hs = slice(h * 128, (h + 1) * 128)
                op('pe', MM(bZ[:, hs], TTm[:, hs], Yb[:, hs]), r=['kbq', 'PTm1'], w=[bZn])
                op('pe', MM(bW2[:, hs], Tm[:, hs], W1b[:, hs]), r=['yb2', 'yb3', 'Pm1'], w=[bW2n])
            op('dve', TT(Tm, Tm, bZ, add), r=['Pm1', bZn], w=['Pm1'])
            op('dve', TT(TTm, TTm, bW2, add), r=['PTm1', bW2n], w=['PTm1'])
        if upto < 3.7:
            return early(l, n, dst, b)
        bX, bXn = bank()
        for h in range(4):
            hs = slice(h * 128, (h + 1) * 128)
            op('pe', MM(bX[:, hs], TTm[:, hs], Xb[:, h, :]), r=['PTm1', 'Xb'], w=[bXn])
        op('dve', CP(X32f, bX), r=[bXn], w=['A1'])
        op('act', ACT(Xbf, bX, AF.Copy), r=[bXn], w=['Xb'])
        if l == 0 and n == 0:
            dump('X7', X32, ['A1'])
        bk, bn = bank()
        bkb = bk.bitcast(BF16)
        for h in range(4):
            op('pe', TR(bkb[0:64, h * 128:(h + 1) * 128], Xb[:, h, 64:128], identb), r=['Xb', 'cb'], w=[bn])
        op('dve', CP(WT[:].rearrange("p a b -> p (a b)"), bkb[0:64, 0:512]), r=[bn], w=['WT'])
        if upto < 3.8:
            return early(l, n, dst, b)
        bw, bwn = bank()
        for h in range(4):
            hs = slice(h * 64, (h + 1) * 64)
            op('pe', MM(bw[:, hs], WT[:, h, :], Sdb[:, hs]), r=['WT', 'Sdb'], w=[bwn])
        op('dve', TT(vnew[:].rearrange("p (h d) -> p h d", h=4), X32[:, :, 0:64], bw[:, 0:256].rearrange("p (h d) -> p h d", h=4), sub),
           r=['A1', bwn], w=['vnew'])
        if upto < 3.85:
            return early(l, n, dst, b)
        bo, bon = bank()
        for h in range(4):
            hs = slice(h * 64, (h + 1) * 64)
            op('pe', MM(bo[:, hs], dT[:, 12 + h, :], Sdb[:, hs], True, False), r=['dT1', 'Sdb'], w=[bon])
            op('pe', MM(bo[:, hs], attnT[:, h * 128:(h + 1) * 128], vnew[:, hs], False, True), r=['attnT', 'vnew'], w=[bon])
        if upto < 3.9:
            return early(l, n, dst, b)
        for h in range(4):
            hs = slice(h * 64, (h + 1) * 64)
            op('dve', TS(kg[:, hs], kn[:, hs], egl[:, h:h + 1], None, mult), r=['kn'] + DS, w=['kg'])
        bk, bn = bank()
        for h in range(4):
            hs = slice(h * 64, (h + 1) * 64)
            op('pe', MM(bk[0:64, hs], kg[:, hs], vnew[:, hs]), r=['kg', 'vnew'], w=[bn])
        for h in range(4):
            hs = slice(h * 64, (h + 1) * 64)
            op('dve', STT(Sd32[:, hs], Sd32[:, hs], egll[0:64, h:h + 1], bk[0:64, hs], mult, add), r=['Sd32', bn] + DS, w=['Sd32'])
        if upto < 3.95:
            return early(l, n, dst, b)
        op('dve', CP(Sdb[:], Sd32[:]), r=['Sd32'], w=['Sdb'])
        if l == 0 and n == 0:
            dump('vnew', vnew[:], ['vnew'])
            op('dve', CP(Gb[:, 0:256], bo[:, 0:256]), r=[bon], w=['G'])
            dump('odn', Gb[:, 0:256], ['G'])
        branch_out(bo[:, 0:256], bon, 1)
        if l == 0 and n == 0:
            dump('ssqdn', sm[:, 12:16], ['sm_b1']); dump('ngg', ngg[:], ['ngg'])
        if upto < 4:
            return early(l, n, dst, b)
        cu = COL['su'][0]
        op('act', ACT(ub[:], p[:, cu:cu + 256], AF.Copy), r=['p3'], w=['ub'])
        bk, bn = bank()
        bkb = bk.bitcast(BF16)
        for c in range(2):
            op('pe', TR(bkb[:, c * 128:(c + 1) * 128], ub[:, c * 128:(c + 1) * 128], identb), r=['ub', 'cb'], w=[bn])
        op('dve', CP(uT[:].rearrange("p a b -> p (a b)"), bkb[:, 0:256]), r=[bn], w=['uT'])
        xr, xrn = dbank()
        xi, xin = dbank()
        for s_ in range(8):
            op('pe', MM(xr[:, s_ * 128:(s_ + 1) * 128], BT[:, 0, s_, :], uT[:, s_ // 4, :]), r=['BT', 'uT'], w=[xrn[s_ // 4]])
            op('pe', MM(xi[:, s_ * 128:(s_ + 1) * 128], BT[:, 1, s_, :], uT[:, s_ // 4, :]), r=['BT', 'uT'], w=[xin[s_ // 4]])
        Af = [a[:].rearrange("p a b -> p (a b)") for a in A]
        fl = lambda t_: t_[:].rearrange("p a b -> p (a b)")
        op('dve', TT(Af[0], xr[:], fl(Tc), mult), r=xrn + ['Tc'], w=['A0'])
        op('dve', TT(Af[1], xi[:], fl(Ts), mult), r=xin + ['Ts'], w=['A1'])
        op('dve', TT(Af[0], Af[0], Af[1], sub), r=['A0', 'A1'], w=['A0'])
        op('dve', TT(Af[1], xr[:], fl(Ts), mult), r=xrn + ['Ts'], w=['A1'])
        op('dve', TT(Af[2], xi[:], fl(Tc), mult), r=xin + ['Tc'], w=['A2'])
        op('dve', TT(Af[1], Af[1], Af[2], add), r=['A1', 'A2'], w=['A1'])
        op('dve', TT(A[0][:, :, 0], A[0][:, :, 0], mc[:, 0, :], add), r=['A0', 'mc'], w=['A0'])
        op('dve', TT(A[1][:, :, 0], A[1][:, :, 0], mc[:, 1, :], add), r=['A1', 'mc'], w=['A1'])
        op('dve', lambda e: e.tensor_tensor_scan(out=Af[2], data0=fl(MAGz), data1=Af[0], initial=0.0, op0=mult, op1=add), r=['MAGz', 'A0'], w=['A2'])
        op('dve', lambda e: e.tensor_tensor_scan(out=Af[3], data0=fl(MAGz), data1=Af[1], initial=0.0, op0=mult, op1=add), r=['MAGz', 'A1'], w=['A3'])
        T = lambda i: s5t[:, i, :]
        mec, mes, t0, t1 = T(15), T(16), T(17), T(18)
        S2 = ['s5t2']
        op('dve', TT(t0, A[2][:, :, 127], mec, mult), r=['A2', 's5t'], w=S2)
        op('dve', TT(t1, A[3][:, :, 127], mes, mult), r=['A3', 's5t'], w=S2)
        op('dve', TT(mc[:, 0, :], t0, t1, sub), r=S2, w=['mc'])
        op('dve', TT(t0, A[2][:, :, 127], mes, mult), r=['A2', 's5t'], w=S2)
        op('dve', TT(t1, A[3][:, :, 127], mec, mult), r=['A3', 's5t'], w=S2)
        op('dve', TT(mc[:, 1, :], t0, t1, add), r=S2, w=['mc'])
        op('pool', TT(Af[0], Af[2], fl(Oc), mult), r=['A2', 'Oc'], w=['A0'])
        op('pool', TT(Af[1], Af[3], fl(Os), mult), r=['A3', 'Os'], w=['A1'])
        op('pool', TT(fl(sre), Af[0], Af[1], sub), r=['A0', 'A1'], w=['Pm0', 'PTm0'])
        op('pool', TT(Af[0], Af[2], fl(Os), mult), r=['A2', 'Os'], w=['A0'])
        op('pool', TT(Af[1], Af[3], fl(Oc), mult), r=['A3', 'Oc'], w=['A1'])
        op('pool', TT(fl(sim), Af[0], Af[1], add), r=['A0', 'A1'], w=['Pm1', 'PTm1'])
        by, byn = bank()
        for s_ in range(8):
            cs_ = slice(s_ * 32, (s_ + 1) * 32)
            op('pe', MM(by[:, cs_], sre[:, s_, :], CT[:, 0, s_, :], True, False), r=['Pm0', 'PTm0', 'CT'], w=[byn])
            op('pe', MM(by[:, cs_], sim[:, s_, :], CT[:, 1, s_, :], False, False), r=['Pm1', 'PTm1', 'CT'], w=[byn])
            op('pe', MM(by[:, cs_], uT[:, s_ // 4, :], Dg[:, s_, :], False, True), r=['uT', 'Dg'], w=[byn])
        y_ = by[:, 0:256]
        op('act', ACT(t256[:], y_, AF.Square), r=[byn], w=['G'])
        op('dve', TS(t256[:], t256[:], 0.044715, 1.0, mult, add), r=['G'], w=['G'])
        op('dve', TT(t256[:], t256[:], y_, mult), r=['G', byn], w=['G'])
        op('act', ACT(t256[:], t256[:], AF.Sigmoid, scale=2.0 * 0.7978845608028654), r=['G'], w=['G'])
        op('dve', TT(yg[:], t256[:], y_, mult), r=['G', byn], w=['G'])
        op('act', ACT(ygb[:], yg[:], AF.Copy), r=['G'], w=['ygb'])
        bk, bn = bank()
        bkb = bk.bitcast(BF16)
        for c in range(2):
            op('pe', TR(bkb[:, c * 128:(c + 1) * 128], ygb[:, c * 128:(c + 1) * 128], identb), r=['ygb', 'cb'], w=[bn])
        op('dve', CP(ygT[:].rearrange("p a b -> p (a b)"), bkb[:, 0:256]), r=[bn], w=['ygT'])
        bgl, bgln = bank()
        op('pe', MM(bgl[:, 0:256], ygT[:, 0, :], Wglu[:, 0, :], True, False), r=['ygT', 'Wglu'], w=[bgln])
        op('pe', MM(bgl[:, 0:256], ygT[:, 1, :], Wglu[:, 1, :], False, False), r=['ygT', 'Wglu'], w=[bgln])
        op('pe', MM(bgl[:, 0:256], onesb[:], bglu[:], False, True), r=['onesb', 'bglu'], w=[bgln])
        op('act', ACT(u256[:], bgl[:, 0:256], AF.Sigmoid), r=[bgln], w=['G'])
        op('dve', TT(u256[:], u256[:], yg[:], mult), r=['G', 'G'], w=['G'])
        op('dve', TT(yb[:, 512:768], u256[:], ngg[:, 512:768], mult), r=['G', 'ngg', 'hT'], w=['yb2'])
        if upto < 5:
            return early(l, n, dst, b)
        cgc, cgq, cgk = COL['gcode'][0], COL['gq'][0], COL['gk'][0]
        op('act', ACT(gcb[:], p[:, cgc:cgc + 16], AF.Copy), r=['p4'], w=['gcb'])
        bk, bn = bank()
        bkb = bk.bitcast(BF16)
        op('pe', TR(bkb[0:16, 0:128], gcb[:], identb), r=['gcb', 'cb'], w=[bn])
        op('dve', CP(gcT[:], bkb[0:16, 0:128]), r=[bn], w=['gcT'])
        bz, bzn = bank()
        op('pe', MM(bz[:, 0:128], gcT[:], wgk[:], True, False), r=['gcT', 'wgk'], w=[bzn])
        op('pe', MM(bz[:, 0:128], onesb[:], bgk[:], False, True), r=['onesb', 'bgk'], w=[bzn])
        gkk, cum, clb, ec, enc, ecl = [g[:] for g in g128]
        op('act', ACT(gkk, bz[:, 0:128], AF.Exp, scale=-1.0), r=[bzn], w=['G'])
        op('act', ACT(gkk, gkk, AF.Ln, bias=1.0), r=['G'], w=['G'])
        op('dve', TS(gkk, gkk, -1.0 / 16, None, mult), r=['G'], w=['G'])
        bc_, bcn_ = bank()
        op('pe', MM(bc_[:, 0:128], triU, gkk), r=['cf', 'G'], w=[bcn_])
        op('pe', MM(bc_[:, 128:256], ones, gkk), r=['cf', 'G'], w=[bcn_])
        for h in range(4):
            op('pe', MM(bc_[0:32, 256 + h:257 + h], g128[0][:, h * 32:(h + 1) * 32], ones[:, 0:1]), r=['cf', 'G'], w=[bcn_])
        op('dve', CP(cum, bc_[:, 0:128]), r=[bcn_], w=['G'])
        op('dve', TT(clb, bc_[:, 128:256], cum, sub), r=[bcn_, 'G'], w=['G'])
        op('act', ACT(ec, cum, AF.Exp), r=['G'], w=['G'])
        op('act', ACT(enc, cum, AF.Exp, scale=-1.0), r=['G'], w=['G'])
        op('act', ACT(ecl, clb, AF.Exp), r=['G'], w=['G'])
        ecl32 = sm[0:32, 64:68]
        op('act', ACT(ecl32, bc_[0:32, 256:260], AF.Exp), r=[bcn_], w=['ecl32'])
        op('dve', STT(gq3[:, 0:128], p[:, cgq:cgq + 128], 32.0 ** -0.5, ec, mult, mult), r=['p4', 'G'], w=['gq3'])
        op('dve', TT(gq3[:, 128:256], p[:, cgk:cgk + 128], enc, mult), r=['p4', 'G'], w=['gq3'])
        op('dve', TT(gq3[:, 256:384], p[:, cgk:cgk + 128], ecl, mult), r=['p4', 'G'], w=['gq3'])
        bk, bn = bank()
        bkb = bk.bitcast(BF16)
        for j in range(8):
            op('pe', TR(bkb[0:32, j * 128:(j + 1) * 128], gq3[:, j * 32:(j + 1) * 32], identb), r=['gq3', 'cb'], w=[bn])
        op('dve', CP(gT[:].rearrange("p a b -> p (a b)"), bkb[0:32, :]), r=[bn], w=['dT0'])
        bk, bn = bank()
        for h in range(4):
            op('pe', MM(bk[:, h * 128:(h + 1) * 128], gT[:, 4 + h, :], gT[:, h, :]), r=['dT0'], w=[bn])
        op('dve', TT(gsT[:], bk, Ui4, mult), r=[bn, 'cb'], w=['sT'])
        bo, bon = bank()
        for h in range(4):
            hs = slice(h * 64, (h + 1) * 64)
            gv_ = vb[:, 256 + h * 64:256 + (h + 1) * 64]
            op('pe', MM(bo[:, hs], gsT[:, h * 128:(h + 1) * 128], gv_, True, False), r=['sT', 'vb'], w=[bon])
            op('pe', MM(bo[:, hs], gT[:, h, :], Gsb[:, hs], False, True), r=['dT0', 'Gsb'], w=[bon])
        bk, bn = bank()
        for h in range(4):
            hs = slice(h * 64, (h + 1) * 64)
            op('pe', MM(bk[0:32, hs], gq3[:, 256 + h * 32:256 + (h + 1) * 32], vb[:, 256 + h * 64:256 + (h + 1) * 64]), r=['gq3', 'vb'], w=[bn])
        for h in range(4):
            hs = slice(h * 64, (h + 1) * 64)
            op('dve', STT(Gs32[:, hs], Gs32[:, hs], ecl32[:, h:h + 1], bk[0:32, hs], mult, add), r=['Gs32', bn, 'ecl32'], w=['Gs32'])
        op('act', ACT(Gsb[:], Gs32[:], AF.Copy), r=['Gs32'], w=['Gsb'])
        branch_out(bo[:, 0:256], bon, 3)
        if upto < 6:
            return early(l, n, dst, b)
        bk, bn = bank()
        bkb = bk.bitcast(BF16)
        YB = ['yb0', 'yb1', 'yb2', 'yb3']
        if dbg and l == NL - 1:
            R.dma('sp', d_dbg[n * 128:(n + 1) * 128, :], yb[:], key='dbg', r=YB, w=['dbgout%d' % n])
        for c in range(8):
            op('pe', TR(bkb[:, c * 128:(c + 1) * 128], yb[:, c * 128:(c + 1) * 128], identb), r=YB + ['cb'], w=[bn])
        op('dve', CP(yT[:].rearrange("p a b -> p (a b)"), bkb), r=[bn], w=['hT'])
        po, pon = dbank()
        for half in range(2):
            for c in range(8):
                op('pe', MM(po[:, half * 512:(half + 1) * 512], yT[:, c, :], Wo[:, c, half * 512:(half + 1) * 512], c == 0, c == 7), r=['hT', 'Wo'], w=[pon[half]])
        op('act', ACT(A[3][:].rearrange('p a b -> p (a b)'), po[:], AF.Square, accum_out=sm[:, 1:2]), r=pon, w=['A3', 'sm1'])
        rsqrt_small(sm[:, 1:2], ['sm1'], 1.0 / D, 1e-6)
        op('dve', STT(Af[0], po[:], sm[:, 1:2], Gpost[:], mult, mult), r=pon + ['sm1', 'Gpost'], w=['A0'])
        op('pool', TT(Af[0], Af[0], xt[b][:], add), r=['A0', X], w=['A0'])
        R.dma('sp', dst[n * 128:(n + 1) * 128, :], Af[0], key='xo', r=['A0'], w=['dst%d_%d' % (l, n)])

    for l in range(NL):
        if do_setup:
            setup(l)
        for n in range(NT):
            tile(l, n, xs_dram[l], xs_dram[l + 1])
    print('SBUF bytes/partition', R.sb_bytes)
    R.finish(final_reads=['dst%d_%d' % (NL - 1, n) for n in range(max(0, NT - 2), NT)] + (['dbgout%d' % n for n in range(NT)] + dumps if dbg else []))
    return nc


def _bf(a):
    return np.ascontiguousarray(a).astype(ml_dtypes.bfloat16)


def make_consts(NT):
    idn = np.eye(128, dtype=np.float32)
    j = np.arange(128)[:, None]
    t = np.arange(128)[None, :]
    Sh = [(j == t - s).astype(np.float32) for s in range(4)]
    ShP = [(j == 128 + t - s).astype(np.float32) for s in range(1, 4)]
    Ui = (j <= t).astype(np.float32)
    negLs = -(t < j).astype(np.float32)
    negUs = -(t > j).astype(np.float32)
    hm = []
    ii_, jj_ = np.arange(128)[:, None], np.arange(128)[None, :]
    for lev in range(7):
        b_ = 1 << lev
        hm.append(((ii_ // (2 * b_) == jj_ // (2 * b_)) & (ii_ % (2 * b_) >= b_) & (jj_ % (2 * b_) < b_)).astype(np.float32))
    hm = hm + [m_.T.copy() for m_ in hm]
    cb = np.concatenate([idn] + Sh + ShP + [np.tile(Ui, (1, 4)), np.tile(negLs, (1, 4)), np.tile(negUs, (1, 4))] + hm, axis=1)
    Esel = np.zeros((128, 512), np.float32)
    for h in range(4):
        Esel[h, h * 128:(h + 1) * 128] = 1.0
    GCt = np.zeros((128, 256), np.float32)
    for h in range(4):
        GCt[:, h * 64:(h + 1) * 64] = np.float32(GAMMA[h]) ** 128
    cf = np.concatenate([idn, Ui, np.ones((128, 128), np.float32), Esel, GCt], axis=1).astype(np.float32)
    pos = np.arange(NT * 128, dtype=np.float64)
    inv = 10000.0 ** (-np.arange(0, 64, 2, dtype=np.float64) / 64)
    ang = pos[:, None] * inv[None, :]
    cos, sin = np.cos(ang), np.sin(ang)
    ii = (np.arange(NT * 128) % 128).astype(np.float64)
    Cq = np.zeros((NT * 128, 4, 32, 2)); Sq = np.zeros_like(Cq); Ck = np.zeros_like(Cq); Sk = np.zeros_like(Cq)
    for h in range(4):
        dq = (GAMMA[h] ** (ii + 1.0)) * (64 ** -0.5)
        dk = GAMMA[h] ** (-(ii + 1.0))
        for (Ct, St, dd) in ((Cq, Sq, dq), (Ck, Sk, dk)):
            Ct[:, h, :, 0] = cos * dd[:, None]
            Ct[:, h, :, 1] = cos * dd[:, None]
            St[:, h, :, 0] = -sin * dd[:, None]
            St[:, h, :, 1] = sin * dd[:, None]
    ropec = np.concatenate([Cq.reshape(-1, 256), Ck.reshape(-1, 256)], axis=1).reshape(NT, 128, 512).astype(np.float32)
    ropes = np.concatenate([Sq.reshape(-1, 256), Sk.reshape(-1, 256)], axis=1).reshape(NT, 128, 512).astype(np.float32)
    return dict(cb=_bf(cb), cf=cf, ropec=ropec, ropes=ropes)


def make_params(inp, layers):
    f = lambda k: np.asarray(inp[k], dtype=np.float32)
    L = list(layers)
    NL = len(L)
    w_in = f('w_in')[L][:, :, PERM]
    w_out = f('w_out')[L]
    gpre = f('norm_pre')[L].reshape(NL, 8, 128).transpose(0, 2, 1)
    gpost = f('norm_post')[L].reshape(NL, 1, D)
    ng = np.concatenate([np.tile(f('ret_norm')[L], (1, 4)), np.tile(f('dn_norm')[L], (1, 4)),
                         np.ones((NL, 256), np.float32), np.tile(f('gla_norm')[L], (1, 4))], axis=1).reshape(NL, 8, 128).transpose(0, 2, 1)
    cw = f('dn_conv')[L].reshape(NL, 1, 4 * 768)
    dnp = np.concatenate([f('dn_a_log')[L], f('dn_dt_bias')[L]], axis=1).reshape(NL, 1, 8)

    def st(a):
        return a.reshape(NL, 8, 2, 64).transpose(0, 2, 3, 1).reshape(NL, 128, 8)
    ldt = np.repeat(f('s5_log_dt')[L][:, :, None], 64, axis=2)
    s5p = np.concatenate([st(f('s5_lam_re')[L]), st(f('s5_lam_im')[L]), st(ldt)], axis=2)
    bt = np.zeros((NL, 2, 128, 8, 128), np.float32)
    ct = np.zeros((NL, 2, 128, 8, 32), np.float32)
    dg = np.zeros((NL, 128, 8, 32), np.float32)
    bre, bim, cre, cim, dd = f('s5_b_re')[L], f('s5_b_im')[L], f('s5_c_re')[L], f('s5_c_im')[L], f('s5_d')[L]
    for g in range(16):
        s_, gg = g // 2, g % 2
        r0 = 32 * (s_ % 4) + gg * 16
        for k_, (bb, cc) in enumerate(((bre, cre), (bim, cim))):
            bt[:, k_, r0:r0 + 16, s_, gg * 64:(gg + 1) * 64] = bb[:, g].transpose(0, 2, 1)
            ct[:, k_, gg * 64:(gg + 1) * 64, s_, gg * 16:(gg + 1) * 16] = cc[:, g].transpose(0, 2, 1)
        for h in range(16):
            dg[:, r0 + h, s_, gg * 16 + h] = dd[:, g, h]
    bt = bt.transpose(0, 2, 1, 3, 4).reshape(NL, 128, 2048)
    ct = ct.transpose(0, 2, 1, 3, 4).reshape(NL, 128, 512)
    dg = dg.reshape(NL, 128, 256)
    wglu = f('s5_w_glu')[L].reshape(NL, 2, 128, 256).transpose(0, 2, 1, 3).reshape(NL, 128, 512)
    bglu = f('s5_b_glu')[L].reshape(NL, 1, 256)
    wgk = f('gla_w_gk')[L]
    bgk = f('gla_b_gk')[L].reshape(NL, 1, 128)
    c = np.ascontiguousarray
    return dict(w_in=c(w_in), w_out=c(w_out), gpre=c(gpre), gpost=c(gpost), ng=c(ng), cw=c(cw), dnp=c(dnp), s5p=c(s5p),
                s5bt=c(bt), s5ct=c(ct), s5dg=c(dg), wglu=c(wglu), bglu=c(bglu), wgk=c(wgk), bgk=c(bgk))


_PROG = {}


def kernel(**inputs):
    x = np.asarray(inputs['x'], dtype=np.float32)
    B, L, _ = x.shape
    NT = L // 128
    NLAY = np.asarray(inputs['w_in']).shape[0]
    if NT not in _PROG:
        _PROG[NT] = (build_program(NT, 1), make_consts(NT))
    nc, consts = _PROG[NT]
    cur = [np.ascontiguousarray(x[b]) for b in range(B)]
    for l in range(NLAY):
        prm = make_params(inputs, [l])
        in_maps = []
        for b in range(B):
            m = dict(consts)
            m.update(prm)
            m['x'] = cur[b]
            in_maps.append(m)
        res = run_bass_kernel_spmd(nc, in_maps, core_ids=list(range(B)))
        cur = [np.ascontiguousarray(np.asarray(res.results[b]['out'], dtype=np.float32)) for b in range(B)]
    return np.stack(cur, axis=0).astype(np.float32)
```

```python
import os
import numpy as np
import ml_dtypes
import concourse.bass as bass
import concourse.mybir as mybir
from concourse.bass_utils import run_bass_kernel_spmd

F32 = mybir.dt.float32
BF16 = mybir.dt.bfloat16
AF = mybir.ActivationFunctionType
ALU = mybir.AluOpType
AX = mybir.AxisListType


class Rec:
    def __init__(self, nc):
        self.nc = nc
        self.ops = {k: [] for k in ('pe', 'act', 'dve', 'pool', 'sp')}
        self.cnt = {k: 0 for k in self.ops}
        self.clock = {k: {} for k in self.ops}
        self.snap = {}
        self.last_w = {}
        self.readers = {}
        self.sems = {}
        self.dma_cnt = {}

    def sem(self, key):
        if key not in self.sems:
            self.sems[key] = self.nc.alloc_semaphore(name="s_" + key.replace(':', '_'))
        return self.sems[key]

    def sb(self, name, shape, dt):
        n = 1
        for d_ in shape[1:]:
            n *= d_
        self.sb_bytes = getattr(self, 'sb_bytes', 0) + n * (2 if dt == BF16 else 4)
        return self.nc.alloc_sbuf_tensor("s_" + name, list(shape), dt)

    def ps(self, name, shape, dt=F32):
        return self.nc.alloc_psum_tensor("q_" + name, list(shape), dt)

    def _deps(self, eng, r, w):
        deps = {}

        def add(kn):
            if kn is None:
                return
            k, n = kn
            if deps.get(k, 0) < n:
                deps[k] = n
        for b in r:
            add(self.last_w.get(b))
        for b in w:
            add(self.last_w.get(b))
            for kn in self.readers.get(b, ()):
                add(kn)
        ck = self.clock[eng]
        waits = []
        if eng == 'pe':
            deps.pop('pe', None)
        for k, n in deps.items():
            if ck.get(k, 0) < n:
                waits.append((k, n))
        for k, n in waits:
            sn = self.snap.get((k, n))
            if sn:
                for k2, n2 in sn.items():
                    if ck.get(k2, 0) < n2:
                        ck[k2] = n2
            if ck.get(k, 0) < n:
                ck[k] = n
        return waits

    def _mark(self, me, r, w):
        for b in r:
            self.readers.setdefault(b, []).append(me)
        for b in w:
            self.last_w[b] = me
            self.readers[b] = []

    def op(self, eng, fn, r=(), w=()):
        w = list(w) + [b_ for b_ in r if b_.startswith('PS') and b_ not in w]
        waits = self._deps(eng, r, w)
        self.cnt[eng] += 1
        me = (eng, self.cnt[eng])
        self.snap[me] = dict(self.clock[eng])
        self.ops[eng].append((waits, fn, eng, 1))
        self._mark(me, r, w)

    def dma(self, q, out, in_, key, r=(), w=()):
        waits = self._deps(q, r, w)
        dk = 'dma:' + key
        self.dma_cnt[dk] = self.dma_cnt.get(dk, 0) + 1
        me = (dk, self.dma_cnt[dk])
        self.snap[me] = dict(self.clock[q])
        self.ops[q].append((waits, lambda e: e.dma_start(out=out, in_=in_), dk, 16))
        self._mark(me, r, w)

    def finish(self, final_reads=()):
        waits = self._deps('sp', list(final_reads), [])
        nc = self.nc
        semval = lambda k, n: (self.sem(k), n * 16 if k.startswith('dma:') else n)
        for k in self.ops:
            self.sem(k)
        final = [semval(k, n) for k, n in waits]
        for k in ('pe', 'act', 'dve', 'pool'):
            if self.cnt[k] > 0:
                final.append((self.sem(k), self.cnt[k]))
        for dk, n in self.dma_cnt.items():
            final.append((self.sem(dk), 16 * n))
        emit_lists = {}
        for eng, lst in self.ops.items():
            el = []
            for waits_, fn, inck, incv in lst:
                el.append(([semval(k, n) for k, n in waits_], fn, self.sem(inck), incv))
            emit_lists[eng] = el
        with nc.Block() as block:
            def run(e, el, extra=()):
                for ws, fn, s, v in el:
                    for sm, val in ws:
                        e.wait_ge(sm, val)
                    fn(e).then_inc(s, v)
                for sm, val in extra:
                    e.wait_ge(sm, val)

            @block.sync
            def _(e):
                run(e, emit_lists['sp'], final)

            @block.tensor
            def _(e):
                run(e, emit_lists['pe'])

            @block.scalar
            def _(e):
                run(e, emit_lists['act'])

            @block.vector
            def _(e):
                run(e, emit_lists['dve'])

            @block.gpsimd
            def _(e):
                run(e, emit_lists['pool'])


D = 1024
NCOL = 3352
ORIG = dict(rq=(0, 256), rk=(256, 512), rv=(512, 768), rg=(768, 1024), dq=(1024, 1280), dk=(1280, 1536),
            dv=(1536, 1792), dbeta=(1792, 1796), da=(1796, 1800), dg=(1800, 2056), su=(2056, 2312),
            sg=(2312, 2568), gq=(2568, 2696), gk=(2696, 2824), gv=(2824, 3080), gcode=(3080, 3096),
            gg=(3096, 3352))
ORDER = ['rq', 'rk', 'rv', 'gv', 'dq', 'dk', 'dv', 'su', 'gq', 'gk', 'gcode', 'dbeta', 'da', 'rg', 'dg', 'sg', 'gg']
COL = {}
_o = 0
for _k in ORDER:
    _w = ORIG[_k][1] - ORIG[_k][0]
    COL[_k] = (_o, _o + _w)
    _o += _w
PERM = np.concatenate([np.arange(*ORIG[k]) for k in ORDER])
BANKS = [(0, 512), (512, 1024), (1024, 1536), (1536, 2048), (2048, 2328), (2328, 2840), (2840, 3352)]
C = 128
GAMMA = [1.0 - 2.0 ** (-5.0 - h) for h in range(4)]


def build_program(NT, NL, dbg=False, upto=99, do_setup=True):
    nc = bass.Bass("TRN2", target_bir_lowering=False)
    R = Rec(nc)
    din = lambda name, shape, dt=F32: nc.dram_tensor(name, list(shape), dt, kind="ExternalInput").ap()
    x_in = din("x", [NT * 128, D])
    x_out = nc.dram_tensor("out", [NT * 128, D], F32, kind="ExternalOutput").ap()
    xs_dram = [x_in]
    for l in range(NL - 1):
        xs_dram.append(nc.dram_tensor("xmid%d" % l, [NT * 128, D], F32).ap())
    xs_dram.append(x_out)
    d_dbg = nc.dram_tensor("dbg", [NT * 128, D], BF16, kind="ExternalOutput").ap() if dbg else None
    d_win = din("w_in", [NL, D, NCOL])
    d_wout = din("w_out", [NL, D, D])
    d_gpre = din("gpre", [NL, 128, 8])
    d_gpost = din("gpost", [NL, 1, D])
    d_ng = din("ng", [NL, 128, 8])
    d_cw = din("cw", [NL, 1, 4 * 768])
    d_dnp = din("dnp", [NL, 1, 8])
    d_s5p = din("s5p", [NL, 128, 24])
    d_bt = din("s5bt", [NL, 128, 2 * 8 * 128])
    d_ct = din("s5ct", [NL, 128, 2 * 8 * 32])
    d_dg = din("s5dg", [NL, 128, 8 * 32])
    d_wglu = din("wglu", [NL, 128, 2 * 256])
    d_bglu = din("bglu", [NL, 1, 256])
    d_wgk = din("wgk", [NL, 16, 128])
    d_bgk = din("bgk", [NL, 1, 128])
    d_ropec = din("ropec", [NT, 128, 512])
    d_ropes = din("ropes", [NT, 128, 512])
    d_cb = din("cb", [128, 128 * 8 + 512 * 3 + 14 * 128], BF16)
    d_cf = din("cf", [128, 128 * 3 + 512 + 256])

    sb, ps = R.sb, R.ps
    cb = sb("cb", [128, 128 * 8 + 1536 + 14 * 128], BF16)
    cf = sb("cf", [128, 128 * 3 + 768], F32)
    identb = cb[:, 0:128]
    Sh = [cb[:, 128 * (1 + s):128 * (2 + s)] for s in range(4)]
    ShP = [None] + [cb[:, 128 * (4 + s):128 * (5 + s)] for s in range(1, 4)]
    Ui4 = cb[:, 1024:1536]
    negLs4 = cb[:, 1536:2048]
    negUs4 = cb[:, 2048:2560]
    HM = [cb[:, 2560 + i * 128:2560 + (i + 1) * 128] for i in range(14)]
    identf = cf[:, 0:128]
    triU = cf[:, 128:256]
    ones = cf[:, 256:384]
    Esel = cf[0:4, 384:896]
    GC = cf[0:64, 896:1152]
    R.dma('sp', cb[:], d_cb[:, :], key='cb', w=['cb'])
    R.dma('sp', cf[:], d_cf[:, :], key='cf', w=['cf'])
    onesb = sb("onesb", [1, 128], BF16)
    R.op('dve', lambda e: e.tensor_copy(out=onesb[:], in_=cf[0:1, 256:384]), r=['cf'], w=['onesb'])

    Wb = sb("Wb", [128, 8, NCOL], BF16)
    Wo = sb("Wo", [128, 8, D], BF16)
    stage = sb("stage", [128, NCOL], F32)
    p = stage
    gpre = sb("gpre", [128, 8], F32)
    Gpost = sb("Gpost", [128, D], F32)
    NG = sb("NG", [128, 8], F32)
    cw = sb("cw", [128, 4, 768], BF16)
    dnp = sb("dnp", [128, 8], F32)
    s5p = sb("s5p", [128, 24], F32)
    BT = sb("BT", [128, 2, 8, 128], BF16)
    CT = sb("CT", [128, 2, 8, 32], BF16)
    Dg = sb("Dg", [128, 8, 32], BF16)
    Wglu = sb("Wglu", [128, 2, 256], BF16)
    bgluf = sb("bgluf", [1, 256], F32)
    bglu = sb("bglu", [1, 256], BF16)
    wgkf = sb("wgkf", [16, 128], F32)
    wgk = sb("wgk", [16, 128], BF16)
    bgkf = sb("bgkf", [1, 128], F32)
    bgk = sb("bgk", [1, 128], BF16)
    Tc = sb("Tc", [128, 8, 128], F32)
    Ts = sb("Ts", [128, 8, 128], F32)
    Oc = sb("Oc", [128, 8, 128], F32)
    Os = sb("Os", [128, 8, 128], F32)
    MAGz = sb("MAGz", [128, 8, 128], F32)
    s5t = sb("s5t", [128, 40, 8], F32)
    Rr32 = sb("Rr32", [64, 256], F32)
    Rrb = sb("Rrb", [64, 256], BF16)
    Sd32 = sb("Sd32", [64, 256], F32)
    Sdb = sb("Sdb", [64, 256], BF16)
    Gs32 = sb("Gs32", [32, 256], F32)
    Gsb = sb("Gsb", [32, 256], BF16)
    mc = sb("mc", [128, 2, 8], F32)
    xt1 = sb("xt", [128, D], F32)
    xt = [xt1, xt1]
    ropeC1 = sb("ropeC", [128, 512], F32)
    ropeS1 = sb("ropeS", [128, 512], F32)
    ropeC = [ropeC1, ropeC1]
    ropeS = [ropeS1, ropeS1]
    sm = sb("sm", [128, 72], F32)
    xs = sb("xs", [128, D], BF16)
    yb = xs
    hT = sb("hT", [128, 8, 128], BF16)
    ngg = sb("ngg", [128, D], BF16)
    qk = sb("qk", [128, 512], BF16)
    vb = sb("vb", [128, 512], BF16)
    sT = sb("sT", [128, 512], BF16)
    tmpR = sb("tmpR", [64, 256], F32)
    pk = [sb("pk%d" % i, [128, 4, 768], BF16) for i in range(2)]
    qn = sb("qn", [128, 256], BF16)
    kn = sb("kn", [128, 256], BF16)
    kbq = sb("kbq", [128, 512], BF16)
    dT = sb("dT", [64, 16, 128], BF16)
    qkT = dT[:, 0:8, :]
    gct = sb("gct", [4, 128], F32)
    PP = sb("PP", [128, 4, 512], BF16)
    Pm = [PP[:, 0, :], PP[:, 2, :]]
    PTm = [PP[:, 1, :], PP[:, 3, :]]
    attnT = sb("attnT", [128, 512], BF16)
    Xb = sb("Xb", [128, 4, 128], BF16)
    WT = sb("WT", [64, 4, 128], BF16)
    vnew = sb("vnew", [128, 256], BF16)
    kg = sb("kg", [128, 256], BF16)
    ub = sb("ub", [128, 256], BF16)
    uT = sb("uT", [128, 2, 128], BF16)
    A = [sb("A%d" % i, [128, 8, 128], F32) for i in range(4)]
    _fl = lambda a_: a_.rearrange("p a b -> p (a b)")
    tmpL, tmpU = _fl(A[0][:, 0:4, :]), _fl(A[0][:, 4:8, :])
    DLs, DUs = tmpL, tmpU
    DUi, X32 = _fl(A[1][:, 0:4, :]), A[1][:, 4:8, :]
    cs = _fl(A[2][:])[:, 0:768]
    sq = _fl(A[3][:])[:, 0:512]
    xo = [_fl(A[0][:]), _fl(A[0][:])]
    yT, gT, gsT = hT, qkT[0:32], sT
    sre = PP[:, 0:2, :].rearrange("p a (b c) -> p (a b) c", c=128)
    sim = PP[:, 2:4, :].rearrange("p a (b c) -> p (a b) c", c=128)
    ygb = sb("ygb", [128, 256], BF16)
    ygT = sb("ygT", [128, 2, 128], BF16)
    gcb = sb("gcb", [128, 16], BF16)
    gcT = sb("gcT", [16, 128], BF16)
    Gb = sb("Gb", [128, 768], F32)
    g128 = [Gb[:, i * 128:(i + 1) * 128] for i in range(6)]
    t256, u256, yg = Gb[:, 0:256], Gb[:, 256:512], Gb[:, 512:768]
    gq3 = sb("gq3", [128, 384], BF16)
    PS = [ps("PS%d" % i, [128, 1024], F32) for i in range(4)]
    bank_ctr = [0]

    def bank():
        i = bank_ctr[0] % 8
        bank_ctr[0] += 1
        return PS[i // 2][:, (i % 2) * 512:(i % 2) * 512 + 512], 'PS%d_%d' % (i // 2, i % 2)

    def dbank():
        if bank_ctr[0] % 2:
            bank_ctr[0] += 1
        i = bank_ctr[0] % 8
        bank_ctr[0] += 2
        return PS[i // 2], ['PS%d_0' % (i // 2), 'PS%d_1' % (i // 2)]

    op = R.op
    dumps = []

    def dump(tag, ap, names):
        if not dbg:
            return
        d = nc.dram_tensor("dump_" + tag, list(ap.shape), ap.dtype, kind="ExternalOutput").ap()
        R.dma('sp', d, ap, key='dump_' + tag, r=names, w=['dumpout_' + tag])
        dumps.append('dumpout_' + tag)
    TT = lambda out, a, b, o: (lambda e: e.tensor_tensor(out=out, in0=a, in1=b, op=o))
    TS = lambda out, a, s1, s2, o0, o1=None: (lambda e: e.tensor_scalar(out=out, in0=a, scalar1=s1, scalar2=s2, op0=o0, **({} if o1 is None else {'op1': o1})))
    STT = lambda out, a, s, b, o0, o1: (lambda e: e.scalar_tensor_tensor(out=out, in0=a, scalar=s, in1=b, op0=o0, op1=o1))
    ACT = lambda out, a, f, **kw: (lambda e: e.activation(out=out, in_=a, func=f, **kw))
    CP = lambda out, a: (lambda e: e.tensor_copy(out=out, in_=a))
    MM = lambda out, l, r_, st=True, sp=True: (lambda e: e.matmul(out, lhsT=l, rhs=r_, start=st, stop=sp))
    TR = lambda out, a, idn: (lambda e: e.transpose(out=out, in_=a, identity=idn))
    mult, add, sub = ALU.mult, ALU.add, ALU.subtract

    def rsqrt_small(ap, names, scale, eps):
        op('dve', TS(ap, ap, scale, eps, mult, add), r=names, w=names)
        op('act', ACT(ap, ap, AF.Sqrt), r=names, w=names)
        op('dve', lambda e: e.reciprocal(out=ap, in_=ap), r=names, w=names)

    def setup(l):
        R.dma('sp', gpre[:], d_gpre[l], key='gpre', w=['gpre'])
        R.dma('sp', NG[:], d_ng[l], key='ng', w=['NG'])
        for c in range(8):
            R.dma('sp', stage[:], d_win[l, c * 128:(c + 1) * 128, :], key='stage', w=['p%d' % i for i in range(7)])
            op('dve', TS(Wb[:, c, :], stage[:], gpre[:, c:c + 1], None, mult), r=['p%d' % i for i in range(7)] + ['gpre'], w=['Wb'])
        for c in range(8):
            R.dma('sp', stage[:, 0:D], d_wout[l, c * 128:(c + 1) * 128, :], key='stage', w=['p%d' % i for i in range(7)])
            op('act', ACT(Wo[:, c, :], stage[:, 0:D], AF.Copy, scale=NG[:, c:c + 1]), r=['p%d' % i for i in range(7)] + ['NG'], w=['Wo'])
        R.dma('sp', Gpost[:], d_gpost[l].partition_broadcast(128), key='gpost', w=['Gpost'])
        R.dma('sp', stage[:, 0:3072], d_cw[l].partition_broadcast(128), key='stage', w=['p%d' % i for i in range(7)])
        op('dve', CP(cw[:].rearrange("p a b -> p (a b)"), stage[:, 0:3072]), r=['p%d' % i for i in range(7)], w=['cw'])
        R.dma('sp', dnp[:], d_dnp[l].partition_broadcast(128), key='dnp', w=['dnp'])
        R.dma('sp', s5p[:], d_s5p[l], key='s5p', w=['s5p'])
        BTf, CTf, Dgf, Wgluf = stage[:, 0:2048], stage[:, 2048:2560], stage[:, 2560:2816], stage[:, 2816:3328]
        PN = ['p%d' % i for i in range(7)]
        R.dma('sp', BTf, d_bt[l], key='stage', w=PN)
        R.dma('sp', CTf, d_ct[l], key='stage2', w=PN)
        R.dma('sp', Dgf, d_dg[l], key='stage3', w=PN)
        R.dma('sp', Wgluf, d_wglu[l], key='stage4', w=PN)
        R.dma('sp', bgluf[:], d_bglu[l], key='bglu', w=['bgluf'])
        R.dma('sp', wgkf[:], d_wgk[l], key='wgk', w=['wgkf'])
        R.dma('sp', bgkf[:], d_bgk[l], key='bgk', w=['bgkf'])
        op('dve', CP(BT[:].rearrange("p a b c -> p (a b c)"), BTf), r=PN, w=['BT'])
        op('dve', CP(CT[:, 0].rearrange("p b c -> p (b c)"), CTf[:, 0:256]), r=PN, w=['CT'])
        op('dve', TS(CT[:, 1].rearrange("p b c -> p (b c)"), CTf[:, 256:512], -1.0, None, mult), r=PN, w=['CT'])
        op('dve', CP(Dg[:].rearrange("p b c -> p (b c)"), Dgf), r=PN, w=['Dg'])
        op('dve', CP(Wglu[:].rearrange("p b c -> p (b c)"), Wgluf), r=PN, w=['Wglu'])
        op('dve', CP(bglu[:], bgluf[:]), r=['bgluf'], w=['bglu'])
        op('dve', CP(wgk[:], wgkf[:]), r=['wgkf'], w=['wgk'])
        op('dve', CP(bgk[:], bgkf[:]), r=['bgkf'], w=['bgk'])
        op('act', ACT(dnp[:, 0:4], dnp[:, 0:4], AF.Exp), r=['dnp'], w=['dnp'])
        op('dve', TS(dnp[:, 0:4], dnp[:, 0:4], -1.0, None, mult), r=['dnp'], w=['dnp'])
        for nm, t_ in (('Rr32', Rr32), ('Sd32', Sd32), ('Gs32', Gs32), ('Rrb', Rrb), ('Sdb', Sdb), ('Gsb', Gsb), ('mc', mc)):
            ap_ = t_[:] if len(t_.shape) == 2 else t_[:].rearrange("p a b -> p (a b)")
            op('pool', lambda e, ap_=ap_: e.memset(ap_, 0.0), r=[], w=[nm])
        for i in range(2):
            op('pool', lambda e, i=i: e.memset(pk[i][:].rearrange("p a b -> p (a b)"), 0.0), r=[], w=['pk%d' % i])
        T = lambda i: s5t[:, i, :]
        S = ['s5t']
        lre, lim, ldt = s5p[:, 0:8], s5p[:, 8:16], s5p[:, 16:24]
        dt_, mag, th, xx, x2, sn, cs_, t0, t1 = T(0), T(1), T(2), T(3), T(4), T(5), T(6), T(7), T(8)
        op('act', ACT(dt_, ldt, AF.Exp), r=['s5p'], w=S)
        op('dve', TT(mag, lre, dt_, mult), r=['s5p'] + S, w=S)
        op('act', ACT(mag, mag, AF.Exp), r=S, w=S)
        op('dve', TT(th, lim, dt_, mult), r=['s5p'] + S, w=S)
        op('dve', TS(xx, th, 1.0 / 16, None, mult), r=S, w=S)
        op('dve', TT(x2, xx, xx, mult), r=S, w=S)
        sc = [1.0, -1.0 / 6, 1.0 / 120, -1.0 / 5040, 1.0 / 362880, -1.0 / 39916800, 1.0 / 6227020800]
        cc = [1.0, -1.0 / 2, 1.0 / 24, -1.0 / 720, 1.0 / 40320, -1.0 / 3628800, 1.0 / 479001600, -1.0 / 87178291200]
        op('dve', TS(sn, x2, sc[6], sc[5], mult, add), r=S, w=S)
        for k_ in (4, 3, 2, 1, 0):
            op('dve', TT(sn, sn, x2, mult), r=S, w=S)
            op('dve', TS(sn, sn, sc[k_], None, add), r=S, w=S)
        op('dve', TT(sn, sn, xx, mult), r=S, w=S)
        op('dve', TS(cs_, x2, cc[7], cc[6], mult, add), r=S, w=S)
        for k_ in (5, 4, 3, 2, 1, 0):
            op('dve', TT(cs_, cs_, x2, mult), r=S, w=S)
            op('dve', TS(cs_, cs_, cc[k_], None, add), r=S, w=S)
        for _ in range(4):
            op('dve', TT(t0, cs_, cs_, mult), r=S, w=S)
            op('dve', TT(t1, sn, sn, mult), r=S, w=S)
            op('dve', TT(sn, sn, cs_, mult), r=S, w=S)
            op('dve', TS(sn, sn, 2.0, None, mult), r=S, w=S)
            op('dve', TT(cs_, t0, t1, sub), r=S, w=S)
        are, aim, den, cre, cim = T(9), T(10), T(11), T(12), T(13)
        op('dve', TT(are, mag, cs_, mult), r=S, w=S)
        op('dve', TT(aim, mag, sn, mult), r=S, w=S)
        op('dve', TT(t0, lre, lre, mult), r=['s5p'] + S, w=S)
        op('dve', TT(t1, lim, lim, mult), r=['s5p'] + S, w=S)
        op('dve', TT(den, t0, t1, add), r=S, w=S)
        op('dve', lambda e: e.reciprocal(out=den, in_=den), r=S, w=S)
        nr = T(14)
        op('dve', TS(nr, are, -1.0, None, add), r=S, w=S)
        op('dve', TT(t0, nr, lre, mult), r=['s5p'] + S, w=S)
        op('dve', TT(t1, aim, lim, mult), r=['s5p'] + S, w=S)
        op('dve', TT(cre, t0, t1, add), r=S, w=S)
        op('dve', TT(cre, cre, den, mult), r=S, w=S)
        op('dve', TT(t0, aim, lre, mult), r=['s5p'] + S, w=S)
        op('dve', TT(t1, nr, lim, mult), r=['s5p'] + S, w=S)
        op('dve', TT(cim, t0, t1, sub), r=S, w=S)
        op('dve', TT(cim, cim, den, mult), r=S, w=S)
        op('pool', lambda e: e.memset(Oc[:, :, 0:1], 1.0), r=[], w=['Oc'])
        op('pool', lambda e: e.memset(Os[:, :, 0:1], 0.0), r=[], w=['Os'])
        op('dve', CP(Oc[:, :, 1], cs_), r=S, w=['Oc'])
        op('dve', CP(Os[:, :, 1], sn), r=S, w=['Os'])
        OO = ['Oc', 'Os']
        k_ = 1
        while k_ < 128:
            bc = lambda t_, k_=k_: t_[:, :, k_:k_ + 1].to_broadcast([128, 8, k_])
            hi = slice(k_ + 1, 2 * k_ + 1) if 2 * k_ + 1 <= 128 else slice(k_ + 1, 128)
            n_ = hi.stop - hi.start
            lo = slice(1, 1 + n_)
            bcn = lambda t_, k_=k_, n_=n_: t_[:, :, k_:k_ + 1].to_broadcast([128, 8, n_])
            a1, a2 = A[0][:, :, 0:n_], A[1][:, :, 0:n_]
            op('dve', TT(a1, Oc[:, :, lo], bcn(Oc), mult), r=OO, w=['A0'])
            op('dve', TT(a2, Os[:, :, lo], bcn(Os), mult), r=OO, w=['A1'])
            op('dve', TT(Oc[:, :, hi], a1, a2, sub), r=['A0', 'A1'] + OO, w=['Oc'])
            op('dve', TT(a1, Oc[:, :, lo], bcn(Os), mult), r=OO, w=['A0'])
            op('dve', TT(a2, Os[:, :, lo], bcn(Oc), mult), r=OO, w=['A1'])
            op('dve', TT(Os[:, :, hi], a1, a2, add), r=['A0', 'A1'] + OO, w=['Os'])
            k_ *= 2
        bc8 = lambda t_: t_.unsqueeze(2).to_broadcast([128, 8, 128])
        op('dve', TT(A[0][:], Oc[:], bc8(cre), mult), r=OO + S, w=['A0'])
        op('dve', TT(A[1][:], Os[:], bc8(cim), mult), r=OO + S, w=['A1'])
        op('dve', TT(Tc[:], A[0][:], A[1][:], add), r=['A0', 'A1'], w=['Tc'])
        op('dve', TT(A[0][:], Oc[:], bc8(cim), mult), r=OO + S, w=['A0'])
        op('dve', TT(A[1][:], Os[:], bc8(cre), mult), r=OO + S, w=['A1'])
        op('dve', TT(Ts[:], A[0][:], A[1][:], sub), r=['A0', 'A1'], w=['Ts'])
        op('dve', CP(MAGz[:], bc8(mag)), r=S, w=['MAGz'])
        op('pool', lambda e: e.memset(MAGz[:, :, 0:1], 0.0), r=[], w=['MAGz'])
        mec, mes = T(15), T(16)
        op('dve', TT(t0, Oc[:, :, 127], cs_, mult), r=OO + S, w=S)
        op('dve', TT(t1, Os[:, :, 127], sn, mult), r=OO + S, w=S)
        op('dve', TT(mec, t0, t1, sub), r=S, w=S)
        op('dve', TT(t0, Oc[:, :, 127], sn, mult), r=OO + S, w=S)
        op('dve', TT(t1, Os[:, :, 127], cs_, mult), r=OO + S, w=S)
        op('dve', TT(mes, t0, t1, add), r=S, w=S)
        op('dve', TT(mec, mec, mag, mult), r=S, w=S)
        op('dve', TT(mes, mes, mag, mult), r=S, w=S)

    def branch_out(obank, obn, br):
        op('act', ACT(sq[:, 0:256], obank, AF.Square), r=[obn], w=['A3'])
        ssq = sm[:, 8 + 4 * br: 12 + 4 * br]
        nm = ['sm_b%d' % br]
        op('dve', lambda e: e.tensor_reduce(out=ssq, in_=sq[:, 0:256].rearrange("p (h d) -> p h d", h=4), axis=AX.X, op=add), r=['A3'], w=nm)
        rsqrt_small(ssq, nm, 1.0 / 64, 1e-6)
        for h in range(4):
            c0 = br * 256 + h * 64
            op('dve', STT(yb[:, c0:c0 + 64], obank[:, h * 64:(h + 1) * 64], ssq[:, h:h + 1], ngg[:, c0:c0 + 64], mult, mult),
               r=[obn, 'ngg', 'hT'] + nm, w=['yb%d' % br])

    def early(l, n, dst, b):
        R.dma('sp', dst[n * 128:(n + 1) * 128, :], xt[b][:], key='xo', r=['xt'], w=['dst%d_%d' % (l, n)])

    def tile(l, n, src, dst):
        b = n % 2
        R.dma('sp', xt[b][:], src[n * 128:(n + 1) * 128, :], key='xt', r=(['dst%d_%d' % (l - 1, n)] if l > 0 else []), w=['xt'])
        R.dma('sp', ropeC[b][:], d_ropec[n], key='rc', w=['rc'])
        R.dma('sp', ropeS[b][:], d_ropes[n], key='rs', w=['rs'])
        X = 'xt'
        op('act', ACT(A[3][:].rearrange('p a b -> p (a b)'), xt[b][:], AF.Square, accum_out=sm[:, 0:1]), r=[X], w=['A3', 'sm0'])
        rsqrt_small(sm[:, 0:1], ['sm0'], 1.0 / D, 1e-6)
        op('act', ACT(xs[:], xt[b][:], AF.Copy, scale=sm[:, 0:1]), r=[X, 'sm0'], w=['xs', 'yb0', 'yb1', 'yb2', 'yb3'])
        bk, bn = bank()
        bkb = bk.bitcast(BF16)
        for c in range(8):
            op('pe', TR(bkb[:, c * 128:(c + 1) * 128], xs[:, c * 128:(c + 1) * 128], identb), r=['xs', 'cb'], w=[bn])
        op('dve', CP(hT[:].rearrange("p a b -> p (a b)"), bkb), r=[bn], w=['hT'])
        for i, (c0, c1) in enumerate(BANKS):
            bk, bn = bank()
            for c in range(8):
                op('pe', MM(bk[:, 0:c1 - c0], hT[:, c, :], Wb[:, c, c0:c1], c == 0, c == 7), r=['hT', 'Wb'], w=[bn])
            if i % 2 == 0:
                op('act', ACT(p[:, c0:c1], bk[:, 0:c1 - c0], AF.Copy), r=[bn], w=['p%d' % i])
            else:
                op('dve', CP(p[:, c0:c1], bk[:, 0:c1 - c0]), r=[bn], w=['p%d' % i])
        if upto < 1:
            return early(l, n, dst, b)
        g0 = COL['rg'][0]
        op('act', ACT(ngg[:], p[:, g0:g0 + D], AF.Silu), r=['p5', 'p6'], w=['ngg'])
        if upto < 2:
            return early(l, n, dst, b)
        RC, RS = ropeC[b], ropeS[b]
        A3f = A[3][:].rearrange('p a b -> p (a b)')
        m1, m2 = A3f[:, 0:512], A3f[:, 512:1024]
        op('dve', TT(m1, p[:, 0:512], RC[:], mult), r=['p0', 'rc'], w=['A3'])
        pv = p[:, 0:512].rearrange("p (a t) -> p a t", t=2)
        m2v = m2.rearrange("p (a t) -> p a t", t=2)
        sv = RS[:].rearrange("p (a t) -> p a t", t=2)
        op('dve', TT(m2v[:, :, 0], pv[:, :, 1], sv[:, :, 0], mult), r=['p0', 'rs'], w=['A3'])
        op('dve', TT(m2v[:, :, 1], pv[:, :, 0], sv[:, :, 1], mult), r=['p0', 'rs'], w=['A3'])
        op('dve', TT(qk[:], m1, m2, add), r=['A3'], w=['qk'])
        op('act', ACT(vb[:], p[:, 512:1024], AF.Copy), r=['p1'], w=['vb'])
        bk, bn = bank()
        bkb = bk.bitcast(BF16)
        for j in range(8):
            op('pe', TR(bkb[0:64, j * 128:(j + 1) * 128], qk[:, j * 64:(j + 1) * 64], identb), r=['qk', 'cb'], w=[bn])
        op('dve', CP(qkT[:].rearrange("p a b -> p (a b)"), bkb[0:64, :]), r=[bn], w=['dT0'])
        bk, bn = bank()
        for h in range(4):
            op('pe', MM(bk[:, h * 128:(h + 1) * 128], qkT[:, 4 + h, :], qkT[:, h, :]), r=['dT0'], w=[bn])
        op('dve', TT(sT[:], bk, Ui4, mult), r=[bn, 'cb'], w=['sT'])
        bo, bon = bank()
        for h in range(4):
            op('pe', MM(bo[:, h * 64:(h + 1) * 64], sT[:, h * 128:(h + 1) * 128], vb[:, h * 64:(h + 1) * 64], True, False), r=['sT', 'vb'], w=[bon])
            op('pe', MM(bo[:, h * 64:(h + 1) * 64], qkT[:, h, :], Rrb[:, h * 64:(h + 1) * 64], False, True), r=['dT0', 'Rrb'], w=[bon])
        bk, bn = bank()
        for h in range(4):
            op('pe', MM(bk[0:64, h * 64:(h + 1) * 64], qk[:, 256 + h * 64:256 + (h + 1) * 64], vb[:, h * 64:(h + 1) * 64]), r=['qk', 'vb'], w=[bn])
        op('dve', TT(tmpR[:], Rr32[:], bk[0:64, 0:256], add), r=['Rr32', bn], w=['tmpR'])
        op('pool', TT(Rr32[:], tmpR[:], GC, mult), r=['tmpR', 'cf'], w=['Rr32'])
        op('act', ACT(Rrb[:], Rr32[:], AF.Copy), r=['Rr32'], w=['Rrb'])
        branch_out(bo[:, 0:256], bon, 0)
        if upto < 3:
            return early(l, n, dst, b)
        c0 = COL['dq'][0]
        for k in range(4):
            op('pool', TT(pk[b][:, k, :], p[:, c0:c0 + 768], cw[:, k, :], mult), r=['p2', 'p3', 'cw'], w=['pk%d' % b])
        bq, bqn = bank()
        bv, bvn = bank()
        for (bk, bn, o0, w_) in ((bq, bqn, 0, 512), (bv, bvn, 512, 256)):
            taps = [(Sh[3 - k], pk[b][:, k, o0:o0 + w_], 'pk%d' % b) for k in range(4)]
            taps += [(ShP[3 - k], pk[1 - b][:, k, o0:o0 + w_], 'pk%d' % (1 - b)) for k in range(3)]
            for i, (lt, rh, nm) in enumerate(taps):
                op('pe', MM(bk[:, 0:w_], lt, rh, i == 0, i == len(taps) - 1), r=['cb', nm], w=[bn])
        if upto < 3.1:
            return early(l, n, dst, b)
        op('act', ACT(cs[:, 0:512], bq, AF.Silu), r=[bqn], w=['A2'])
        op('act', ACT(cs[:, 512:768], bv[:, 0:256], AF.Silu), r=[bvn], w=['A2'])
        op('act', ACT(sq[:], cs[:, 0:512], AF.Square), r=['A2'], w=['A3'])
        rn = sm[:, 24:32]
        op('dve', lambda e: e.tensor_reduce(out=rn, in_=sq[:].rearrange("p (h d) -> p h d", h=8), axis=AX.X, op=add), r=['A3'], w=['rn'])
        rsqrt_small(rn, ['rn'], 1.0, 1e-6)
        for h in range(4):
            op('dve', TS(qn[:, h * 64:(h + 1) * 64], cs[:, h * 64:(h + 1) * 64], rn[:, h:h + 1], 0.125, mult, mult), r=['A2', 'rn'], w=['qn'])
            op('dve', TS(kn[:, h * 64:(h + 1) * 64], cs[:, 256 + h * 64:256 + (h + 1) * 64], rn[:, 4 + h:5 + h], None, mult), r=['A2', 'rn'], w=['kn'])
        beta, gz, gg_, gc, eg, egl, egll, beg = (sm[:, 32:36], sm[:, 36:40], sm[:, 40:44], sm[:, 44:52], sm[:, 52:56],
                                                  sm[:, 56:60], sm[:, 60:64], sm[:, 4:8])
        DS = ['dsm']
        cb_, ca_ = COL['dbeta'][0], COL['da'][0]
        op('act', ACT(beta, p[:, cb_:cb_ + 4], AF.Sigmoid), r=['p4'], w=DS)
        op('dve', TT(gz, p[:, ca_:ca_ + 4], dnp[:, 4:8], add), r=['p4', 'dnp'], w=DS)
        op('act', ACT(gz, gz, AF.Exp), r=DS, w=DS)
        op('act', ACT(gz, gz, AF.Ln, bias=1.0), r=DS, w=DS)
        op('dve', TT(gg_, gz, dnp[:, 0:4], mult), r=DS + ['dnp'], w=DS)
        if upto < 3.2:
            return early(l, n, dst, b)
        bg, bgn = bank()
        op('pe', MM(bg[:, 0:4], triU, gg_), r=['cf'] + DS, w=[bgn])
        op('pe', MM(bg[:, 4:8], ones, gg_), r=['cf'] + DS, w=[bgn])
        op('pe', MM(bg[0:4, 128:256], gg_, triU), r=['cf'] + DS, w=[bgn])
        op('dve', CP(gc, bg[:, 0:8]), r=[bgn], w=DS)
        op('dve', CP(gct[:], bg[0:4, 128:256]), r=[bgn], w=['gct'])
        op('act', ACT(eg, gc[:, 0:4], AF.Exp), r=DS, w=DS)
        op('dve', TT(egl, gc[:, 4:8], gc[:, 0:4], sub), r=DS, w=DS)
        op('act', ACT(egl, egl, AF.Exp), r=DS, w=DS)
        op('act', ACT(egll, gc[:, 4:8], AF.Exp), r=DS, w=DS)
        op('dve', TT(beg, beta, eg, mult), r=DS, w=DS)
        if upto < 3.3:
            return early(l, n, dst, b)
        bR, bRn = bank()
        for h in range(4):
            op('pe', MM(bR[:, h * 128:(h + 1) * 128], Esel[:, h * 128:(h + 1) * 128], gct[:]), r=['cf', 'gct'], w=[bRn])
        for h in range(4):
            hs = slice(h * 128, (h + 1) * 128)
            op('dve', TS(tmpL[:, hs], bR[:, hs], gc[:, h:h + 1], 0.0, sub, ALU.max), r=[bRn] + DS, w=['A0'])
            op('dve', TS(tmpU[:, hs], bR[:, hs], gc[:, h:h + 1], 0.0, sub, ALU.min), r=[bRn] + DS, w=['A0'])
        if upto < 3.4:
            return early(l, n, dst, b)
        op('act', ACT(tmpL[:], tmpL[:], AF.Exp, scale=-1.0), r=['A0'], w=['A0'])
        op('act', ACT(tmpU[:], tmpU[:], AF.Exp), r=['A0'], w=['A0'])
        op('pool', TT(DLs[:], tmpL[:], negLs4, mult), r=['A0', 'cb'], w=['A0'])
        op('pool', TT(DUi[:], tmpU[:], Ui4, mult), r=['A0', 'cb'], w=['A1'])
        op('pool', TT(DUs[:], tmpU[:], negUs4, mult), r=['A0', 'cb'], w=['A0'])
        if l == 0 and n == 0:
            dump('cs', cs, ['A2']); dump('kn', kn[:], ['kn']); dump('qn', qn[:], ['qn']); dump('sm', sm[:, 24:64], DS + ['rn'])
            dump('DLs', DLs, ['A0']); dump('DUs', DUs, ['A0']); dump('DUi', DUi, ['A1'])
        for h in range(4):
            hs = slice(h * 64, (h + 1) * 64)
            op('dve', TS(kbq[:, hs], kn[:, hs], beta[:, h:h + 1], None, mult), r=['kn'] + DS, w=['kbq'])
            op('dve', TS(kbq[:, 256 + h * 64:256 + (h + 1) * 64], qn[:, hs], eg[:, h:h + 1], None, mult), r=['qn'] + DS, w=['kbq'])
        if upto < 3.5:
            return early(l, n, dst, b)
        srcs = [(kn, 0, 'kn'), (kbq, 0, 'kbq'), (qn, 0, 'qn'), (kbq, 256, 'kbq')]
        for half in range(2):
            bk, bn = bank()
            bkb = bk.bitcast(BF16)
            for jj in range(8):
                j = half * 8 + jj
                t_, off, nm = srcs[j // 4]
                h = j % 4
                op('pe', TR(bkb[0:64, jj * 128:(jj + 1) * 128], t_[:, off + h * 64:off + (h + 1) * 64], identb), r=[nm, 'cb'], w=[bn])
            op('dve' if half == 0 else 'act', CP(dT[:, half * 8:(half + 1) * 8, :].rearrange("p a b -> p (a b)"), bkb[0:64, :]) if half == 0 else
               ACT(dT[:, half * 8:(half + 1) * 8, :].rearrange("p a b -> p (a b)"), bkb[0:64, :], AF.Copy), r=[bn], w=['dT%d' % half])
        bA, bAn = bank()
        bAT, bATn = bank()
        bat, batn = bank()
        for h in range(4):
            hs = slice(h * 128, (h + 1) * 128)
            op('pe', MM(bA[:, hs], dT[:, 4 + h, :], dT[:, h, :]), r=['dT0'], w=[bAn])
            op('pe', MM(bAT[:, hs], dT[:, h, :], dT[:, 4 + h, :]), r=['dT0'], w=[bATn])
            op('pe', MM(bat[:, hs], dT[:, h, :], dT[:, 8 + h, :]), r=['dT0', 'dT1'], w=[batn])
        op('dve', TT(Pm[0][:], bA, DLs[:], mult), r=[bAn, 'A0'], w=['Pm0'])
        op('dve', TT(PTm[0][:], bAT, DUs[:], mult), r=[bATn, 'A0'], w=['PTm0'])
        op('dve', TT(attnT[:], bat, DUi[:], mult), r=[batn, 'A1'], w=['attnT'])
        for h in range(4):
            hs = slice(h * 64, (h + 1) * 64)
            op('dve', TS(X32[:, h, 0:64], cs[:, 512 + h * 64:512 + (h + 1) * 64], beta[:, h:h + 1], None, mult), r=['A2'] + DS, w=['A1'])
            op('dve', TS(X32[:, h, 64:128], kn[:, hs], beg[:, h:h + 1], None, mult), r=['kn'] + DS, w=['A1'])
        if l == 0 and n == 0:
            dump('N0', Pm[0], ['Pm0']); dump('NT0', PTm[0], ['PTm0']); dump('attnT', attnT[:], ['attnT']); dump('X0', X32, ['A1'])
        X32f = X32[:].rearrange("p a b -> p (a b)")
        Xbf = Xb[:].rearrange("p a b -> p (a b)")
        op('act', ACT(Xbf, X32f, AF.Copy), r=['A1'], w=['Xb'])
        if upto < 3.6:
            return early(l, n, dst, b)
        Lm, LmT, Yb, W1b = sT[:], qk[:], kbq[:], xs[:, 512:1024]
        Tm, TTm = Pm[1], PTm[1]
        v4 = lambda a_: a_.rearrange("p (h c) -> p h c", h=4)
        bm = lambda i: HM[i].unsqueeze(1).to_broadcast([128, 4, 128])
        idb4 = identb.unsqueeze(1).to_broadcast([128, 4, 128])
        if os.environ.get("HV") == "1":
            op('dve', CP(Tm, Pm[0]), r=['Pm0'], w=['Pm1'])
            op('dve', CP(TTm, PTm[0]), r=['PTm0'], w=['PTm1'])
        elif os.environ.get("HV") == "4":
            op('dve', CP(kbq[:, 0:256], kn[:]), r=['kn'], w=['kbq'])
            op('dve', CP(kbq[:, 256:512], kn[:]), r=['kn'], w=['kbq'])
        elif os.environ.get("HV") == "5":
            op('act', ACT(Tm, Pm[0], AF.Copy), r=['Pm0'], w=['Pm1'])
        elif os.environ.get("HV") == "2":
            op('dve', TT(Tm, Pm[0], Pm[0], mult), r=['Pm0'], w=['Pm1'])
            op('dve', TT(TTm, PTm[0], PTm[0], mult), r=['PTm0'], w=['PTm1'])
        elif os.environ.get("HV") == "3":
            op('dve', TT(Tm, Pm[0], Ui4, mult), r=['Pm0', 'cb'], w=['Pm1'])
            op('dve', TT(TTm, PTm[0], Ui4, mult), r=['PTm0', 'cb'], w=['PTm1'])
        else:
            for h_ in range(4):
                    op('pool', TT(Tm[:, h_ * 128:(h_ + 1) * 128], Pm[0][:, h_ * 128:(h_ + 1) * 128], HM[0], mult), r=['Pm0', 'cb'], w=['Pm1'])
            for h_ in range(4):
                    op('pool', TT(Tm[:, h_ * 128:(h_ + 1) * 128], Tm[:, h_ * 128:(h_ + 1) * 128], identb, add), r=['Pm1', 'cb'], w=['Pm1'])
            for h_ in range(4):
                    op('pool', TT(TTm[:, h_ * 128:(h_ + 1) * 128], PTm[0][:, h_ * 128:(h_ + 1) * 128], HM[7], mult), r=['PTm0', 'cb'], w=['PTm1'])
            for h_ in range(4):
                    op('pool', TT(TTm[:, h_ * 128:(h_ + 1) * 128], TTm[:, h_ * 128:(h_ + 1) * 128], identb, add), r=['PTm1', 'cb'], w=['PTm1'])
        for lev in range(1, int(os.environ.get("NLEV", "7"))):
            for h_ in range(4):
                op('pool', TT(Lm[:, h_ * 128:(h_ + 1) * 128], Pm[0][:, h_ * 128:(h_ + 1) * 128], HM[lev], mult), r=['Pm0', 'cb'], w=['sT'])
            for h_ in range(4):
                op('pool', TT(LmT[:, h_ * 128:(h_ + 1) * 128], PTm[0][:, h_ * 128:(h_ + 1) * 128], HM[7 + lev], mult), r=['PTm0', 'cb'], w=['qk'])
            bY, bYn = bank()
            bW, bWn = bank()
            for h in range(4):
                hs = slice(h * 128, (h + 1) * 128)
                op('pe', MM(bY[:, hs], LmT[:, hs], Tm[:, hs]), r=['qk', 'Pm1'], w=[bYn])
                op('pe', MM(bW[:, hs], Lm[:, hs], TTm[:, hs]), r=['sT', 'PTm1'], w=[bWn])
            op('act', ACT(Yb, bY, AF.Copy), r=[bYn], w=['kbq'])
            op('dve', CP(W1b, bW), r=[bWn], w=['yb2', 'yb3'])
            bZ, bZn = bank()
            bW2, bW2n = bank()
            for h in range(4):
                hs = slice(h * 128, (h + 1) * 128)
                op('pe', MM(bZ[:, hs], TTm[:, hs], Yb[:, hs]), r=['kbq', 'PTm1'], w=[bZn])
                op('pe', MM(bW2[:, hs], Tm[:, hs], W1b[:, hs]), r=['yb2', 'yb3', 'Pm1'], w=[bW2n])
            op('dve', TT(Tm, Tm, bZ, add), r=['Pm1', bZn], w=['Pm1'])
            op('dve', TT(TTm, TTm, bW2, add), r=['PTm1', bW2n], w=['PTm1'])
        if upto < 3.7:
            return early(l, n, dst, b)
        bX, bXn = bank()
        for h in range(4):
            hs = slice(h * 128, (h + 1) * 128)
            op('pe', MM(bX[:, hs], TTm[:, hs], Xb[:, h, :]), r=['PTm1', 'Xb'], w=[bXn])
        op('dve', CP(X32f, bX), r=[bXn], w=['A1'])
        op('act', ACT(Xbf, bX, AF.Copy), r=[bXn], w=['Xb'])
        if l == 0 and n == 0:
            dump('X7', X32, ['A1'])
        bk, bn = bank()
        bkb = bk.bitcast(BF16)
        for h in range(4):
            op('pe', TR(bkb[0:64, h * 128:(h + 1) * 128], Xb[:, h, 64:128], identb), r=['Xb', 'cb'], w=[bn])
        op('dve', CP(WT[:].rearrange("p a b -> p (a b)"), bkb[0:64, 0:512]), r=[bn], w=['WT'])
        if upto < 3.8:
            return early(l, n, dst, b)
        bw, bwn = bank()
        for h in range(4):
            hs = slice(h * 64, (h + 1) * 64)
            op('pe', MM(bw[:, hs], WT[:, h, :], Sdb[:, hs]), r=['WT', 'Sdb'], w=[bwn])
        op('dve', TT(vnew[:].rearrange("p (h d) -> p h d", h=4), X32[:, :, 0:64], bw[:, 0:256].rearrange("p (h d) -> p h d", h=4), sub),
           r=['A1', bwn], w=['vnew'])
        if upto < 3.85:
            return early(l, n, dst, b)
        bo, bon = bank()
        for h in range(4):
            hs = slice(h * 64, (h + 1) * 64)
            op('pe', MM(bo[:, hs], dT[:, 12 + h, :], Sdb[:, hs], True, False), r=['dT1', 'Sdb'], w=[bon])
            op('pe', MM(bo[:, hs], attnT[:, h * 128:(h + 1) * 128], vnew[:, hs], False, True), r=['attnT', 'vnew'], w=[bon])
        if upto < 3.9:
            return early(l, n, dst, b)
        for h in range(4):
            hs = slice(h * 64, (h + 1) * 64)
            op('dve', TS(kg[:, hs], kn[:, hs], egl[:, h:h + 1], None, mult), r=['kn'] + DS, w=['kg'])
        bk, bn = bank()
        for h in range(4):
            hs = slice(h * 64, (h + 1) * 64)
            op('pe', MM(bk[0:64, hs], kg[:, hs], vnew[:, hs]), r=['kg', 'vnew'], w=[bn])
        for h in range(4):
            hs = slice(h * 64, (h + 1) * 64)
            op('dve', STT(Sd32[:, hs], Sd32[:, hs], egll[0:64, h:h + 1], bk[0:64, hs], mult, add), r=['Sd32', bn] + DS, w=['Sd32'])
        if upto < 3.95:
            return early(l, n, dst, b)
        op('dve', CP(Sdb[:], Sd32[:]), r=['Sd32'], w=['Sdb'])
        if l == 0 and n == 0:
            dump('vnew', vnew[:], ['vnew'])
            op('dve', CP(Gb[:, 0:256], bo[:, 0:256]), r=[bon], w=['G'])
            dump('odn', Gb[:, 0:256], ['G'])
        branch_out(bo[:, 0:256], bon, 1)
        if l == 0 and n == 0:
            dump('ssqdn', sm[:, 12:16], ['sm_b1']); dump('ngg', ngg[:], ['ngg'])
        if upto < 4:
            return early(l, n, dst, b)
        cu = COL['su'][0]
        op('act', ACT(ub[:], p[:, cu:cu + 256], AF.Copy), r=['p3'], w=['ub'])
        bk, bn = bank()
        bkb = bk.bitcast(BF16)
        for c in range(2):
            op('pe', TR(bkb[:, c * 128:(c + 1) * 128], ub[:, c * 128:(c + 1) * 128], identb), r=['ub', 'cb'], w=[bn])
        op('dve', CP(uT[:].rearrange("p a b -> p (a b)"), bkb[:, 0:256]), r=[bn], w=['uT'])
        xr, xrn = dbank()
        xi, xin = dbank()
        for s_ in range(8):
            op('pe', MM(xr[:, s_ * 128:(s_ + 1) * 128], BT[:, 0, s_, :], uT[:, s_ // 4, :]), r=['BT', 'uT'], w=[xrn[s_ // 4]])
            op('pe', MM(xi[:, s_ * 128:(s_ + 1) * 128], BT[:, 1, s_, :], uT[:, s_ // 4, :]), r=['BT', 'uT'], w=[xin[s_ // 4]])
        Af = [a[:].rearrange("p a b -> p (a b)") for a in A]
        fl = lambda t_: t_[:].rearrange("p a b -> p (a b)")
        op('dve', TT(Af[0], xr[:], fl(Tc), mult), r=xrn + ['Tc'], w=['A0'])
        op('dve', TT(Af[1], xi[:], fl(Ts), mult), r=xin + ['Ts'], w=['A1'])
        op('dve', TT(Af[0], Af[0], Af[1], sub), r=['A0', 'A1'], w=['A0'])
        op('dve', TT(Af[1], xr[:], fl(Ts), mult), r=xrn + ['Ts'], w=['A1'])
        op('dve', TT(Af[2], xi[:], fl(Tc), mult), r=xin + ['Tc'], w=['A2'])
        op('dve', TT(Af[1], Af[1], Af[2], add), r=['A1', 'A2'], w=['A1'])
        op('dve', TT(A[0][:, :, 0], A[0][:, :, 0], mc[:, 0, :], add), r=['A0', 'mc'], w=['A0'])
        op('dve', TT(A[1][:, :, 0], A[1][:, :, 0], mc[:, 1, :], add), r=['A1', 'mc'], w=['A1'])
        op('dve', lambda e: e.tensor_tensor_scan(out=Af[2], data0=fl(MAGz), data1=Af[0], initial=0.0, op0=mult, op1=add), r=['MAGz', 'A0'], w=['A2'])
        op('dve', lambda e: e.tensor_tensor_scan(out=Af[3], data0=fl(MAGz), data1=Af[1], initial=0.0, op0=mult, op1=add), r=['MAGz', 'A1'], w=['A3'])
        T = lambda i: s5t[:, i, :]
        mec, mes, t0, t1 = T(15), T(16), T(17), T(18)
        S2 = ['s5t2']
        op('dve', TT(t0, A[2][:, :, 127], mec, mult), r=['A2', 's5t'], w=S2)
        op('dve', TT(t1, A[3][:, :, 127], mes, mult), r=['A3', 's5t'], w=S2)
        op('dve', TT(mc[:, 0, :], t0, t1, sub), r=S2, w=['mc'])
        op('dve', TT(t0, A[2][:, :, 127], mes, mult), r=['A2', 's5t'], w=S2)
        op('dve', TT(t1, A[3][:, :, 127], mec, mult), r=['A3', 's5t'], w=S2)
        op('dve', TT(mc[:, 1, :], t0, t1, add), r=S2, w=['mc'])
        op('pool', TT(Af[0], Af[2], fl(Oc), mult), r=['A2', 'Oc'], w=['A0'])
        op('pool', TT(Af[1], Af[3], fl(Os), mult), r=['A3', 'Os'], w=['A1'])
        op('pool', TT(fl(sre), Af[0], Af[1], sub), r=['A0', 'A1'], w=['Pm0', 'PTm0'])
        op('pool', TT(Af[0], Af[2], fl(Os), mult), r=['A2', 'Os'], w=['A0'])
        op('pool', TT(Af[1], Af[3], fl(Oc), mult), r=['A3', 'Oc'], w=['A1'])
        op('pool', TT(fl(sim), Af[0], Af[1], add), r=['A0', 'A1'], w=['Pm1', 'PTm1'])
        by, byn = bank()
        for s_ in range(8):
            cs_ = slice(s_ * 32, (s_ + 1) * 32)
            op('pe', MM(by[:, cs_], sre[:, s_, :], CT[:, 0, s_, :], True, False), r=['Pm0', 'PTm0', 'CT'], w=[byn])
            op('pe', MM(by[:, cs_], sim[:, s_, :], CT[:, 1, s_, :], False, False), r=['Pm1', 'PTm1', 'CT'], w=[byn])
            op('pe', MM(by[:, cs_], uT[:, s_ // 4, :], Dg[:, s_, :], False, True), r=['uT', 'Dg'], w=[byn])
        y_ = by[:, 0:256]
        op('act', ACT(t256[:], y_, AF.Square), r=[byn], w=['G'])
        op('dve', TS(t256[:], t256[:], 0.044715, 1.0, mult, add), r=['G'], w=['G'])
        op('dve', TT(t256[:], t256[:], y_, mult), r=['G', byn], w=['G'])
        op('act', ACT(t256[:], t256[:], AF.Sigmoid, scale=2.0 * 0.7978845608028654), r=['G'], w=['G'])
        op('dve', TT(yg[:], t256[:], y_, mult), r=['G', byn], w=['G'])
        op('act', ACT(ygb[:], yg[:], AF.Copy), r=['G'], w=['ygb'])
        bk, bn = bank()
        bkb = bk.bitcast(BF16)
        for c in range(2):
            op('pe', TR(bkb[:, c * 128:(c + 1) * 128], ygb[:, c * 128:(c + 1) * 128], identb), r=['ygb', 'cb'], w=[bn])
        op('dve', CP(ygT[:].rearrange("p a b -> p (a b)"), bkb[:, 0:256]), r=[bn], w=['ygT'])
        bgl, bgln = bank()
        op('pe', MM(bgl[:, 0:256], ygT[:, 0, :], Wglu[:, 0, :], True, False), r=['ygT', 'Wglu'], w=[bgln])
        op('pe', MM(bgl[:, 0:256], ygT[:, 1, :], Wglu[:, 1, :], False, False), r=['ygT', 'Wglu'], w=[bgln])
        op('pe', MM(bgl[:, 0:256], onesb[:], bglu[:], False, True), r=['onesb', 'bglu'], w=[bgln])
        op('act', ACT(u256[:], bgl[:, 0:256], AF.Sigmoid), r=[bgln], w=['G'])
        op('dve', TT(u256[:], u256[:], yg[:], mult), r=['G', 'G'], w=['G'])
        op('dve', TT(yb[:, 512:768], u256[:], ngg[:, 512:768], mult), r=['G', 'ngg', 'hT'], w=['yb2'])
        if upto < 5:
            return early(l, n, dst, b)
        cgc, cgq, cgk = COL['gcode'][0], COL['gq'][0], COL['gk'][0]
        op('act', ACT(gcb[:], p[:, cgc:cgc + 16], AF.Copy), r=['p4'], w=['gcb'])
        bk, bn = bank()
        bkb = bk.bitcast(BF16)
        op('pe', TR(bkb[0:16, 0:128], gcb[:], identb), r=['gcb', 'cb'], w=[bn])
        op('dve', CP(gcT[:], bkb[0:16, 0:128]), r=[bn], w=['gcT'])
        bz, bzn = bank()
        op('pe', MM(bz[:, 0:128], gcT[:], wgk[:], True, False), r=['gcT', 'wgk'], w=[bzn])
        op('pe', MM(bz[:, 0:128], onesb[:], bgk[:], False, True), r=['onesb', 'bgk'], w=[bzn])
        gkk, cum, clb, ec, enc, ecl = [g[:] for g in g128]
        op('act', ACT(gkk, bz[:, 0:128], AF.Exp, scale=-1.0), r=[bzn], w=['G'])
        op('act', ACT(gkk, gkk, AF.Ln, bias=1.0), r=['G'], w=['G'])
        op('dve', TS(gkk, gkk, -1.0 / 16, None, mult), r=['G'], w=['G'])
        bc_, bcn_ = bank()
        op('pe', MM(bc_[:, 0:128], triU, gkk), r=['cf', 'G'], w=[bcn_])
        op('pe', MM(bc_[:, 128:256], ones, gkk), r=['cf', 'G'], w=[bcn_])
        for h in range(4):
            op('pe', MM(bc_[0:32, 256 + h:257 + h], g128[0][:, h * 32:(h + 1) * 32], ones[:, 0:1]), r=['cf', 'G'], w=[bcn_])
        op('dve', CP(cum, bc_[:, 0:128]), r=[bcn_], w=['G'])
        op('dve', TT(clb, bc_[:, 128:256], cum, sub), r=[bcn_, 'G'], w=['G'])
        op('act', ACT(ec, cum, AF.Exp), r=['G'], w=['G'])
        op('act', ACT(enc, cum, AF.Exp, scale=-1.0), r=['G'], w=['G'])
        op('act', ACT(ecl, clb, AF.Exp), r=['G'], w=['G'])
        ecl32 = sm[0:32, 64:68]
        op('act', ACT(ecl32, bc_[0:32, 256:260], AF.Exp), r=[bcn_], w=['ecl32'])
        op('dve', STT(gq3[:, 0:128], p[:, cgq:cgq + 128], 32.0 ** -0.5, ec, mult, mult), r=['p4', 'G'], w=['gq3'])
        op('dve', TT(gq3[:, 128:256], p[:, cgk:cgk + 128], enc, mult), r=['p4', 'G'], w=['gq3'])
        op('dve', TT(gq3[:, 256:384], p[:, cgk:cgk + 128], ecl, mult), r=['p4', 'G'], w=['gq3'])
        bk, bn = bank()
        bkb = bk.bitcast(BF16)
        for j in range(8):
            op('pe', TR(bkb[0:32, j * 128:(j + 1) * 128], gq3[:, j * 32:(j + 1) * 32], identb), r=['gq3', 'cb'], w=[bn])
        op('dve', CP(gT[:].rearrange("p a b -> p (a b)"), bkb[0:32, :]), r=[bn], w=['dT0'])
        bk, bn = bank()
        for h in range(4):
            op('pe', MM(bk[:, h * 128:(h + 1) * 128], gT[:, 4 + h, :], gT[:, h, :]), r=['dT0'], w=[bn])
        op('dve', TT(gsT[:], bk, Ui4, mult), r=[bn, 'cb'], w=['sT'])
        bo, bon = bank()
        for h in range(4):
            hs = slice(h * 64, (h + 1) * 64)
            gv_ = vb[:, 256 + h * 64:256 + (h + 1) * 64]
            op('pe', MM(bo[:, hs], gsT[:, h * 128:(h + 1) * 128], gv_, True, False), r=['sT', 'vb'], w=[bon])
            op('pe', MM(bo[:, hs], gT[:, h, :], Gsb[:, hs], False, True), r=['dT0', 'Gsb'], w=[bon])
        bk, bn = bank()
        for h in range(4):
            hs = slice(h * 64, (h + 1) * 64)
            op('pe', MM(bk[0:32, hs], gq3[:, 256 + h * 32:256 + (h + 1) * 32], vb[:, 256 + h * 64:256 + (h + 1) * 64]), r=['gq3', 'vb'], w=[bn])
        for h in range(4):
            hs = slice(h * 64, (h + 1) * 64)
            op('dve', STT(Gs32[:, hs], Gs32[:, hs], ecl32[:, h:h + 1], bk[0:32, hs], mult, add), r=['Gs32', bn, 'ecl32'], w=['Gs32'])
        op('act', ACT(Gsb[:], Gs32[:], AF.Copy), r=['Gs32'], w=['Gsb'])
        branch_out(bo[:, 0:256], bon, 3)
        if upto < 6:
            return early(l, n, dst, b)
        bk, bn = bank()
        bkb = bk.bitcast(BF16)
        YB = ['yb0', 'yb1', 'yb2', 'yb3']
        if dbg and l == NL - 1:
            R.dma('sp', d_dbg[n * 128:(n + 1) * 128, :], yb[:], key='dbg', r=YB, w=['dbgout%d' % n])
        for c in range(8):
            op('pe', TR(bkb[:, c * 128:(c + 1) * 128], yb[:, c * 128:(c + 1) * 128], identb), r=YB + ['cb'], w=[bn])
        op('dve', CP(yT[:].rearrange("p a b -> p (a b)"), bkb), r=[bn], w=['hT'])
        po, pon = dbank()
        for half in range(2):
            for c in range(8):
                op('pe', MM(po[:, half * 512:(half + 1) * 512], yT[:, c, :], Wo[:, c, half * 512:(half + 1) * 512], c == 0, c == 7), r=['hT', 'Wo'], w=[pon[half]])
        op('act', ACT(A[3][:].rearrange('p a b -> p (a b)'), po[:], AF.Square, accum_out=sm[:, 1:2]), r=pon, w=['A3', 'sm1'])
        rsqrt_small(sm[:, 1:2], ['sm1'], 1.0 / D, 1e-6)
        op('dve', STT(Af[0], po[:], sm[:, 1:2], Gpost[:], mult, mult), r=pon + ['sm1', 'Gpost'], w=['A0'])
        op('pool', TT(Af[0], Af[0], xt[b][:], add), r=['A0', X], w=['A0'])
        R.dma('sp', dst[n * 128:(n + 1) * 128, :], Af[0], key='xo', r=['A0'], w=['dst%d_%d' % (l, n)])

    for l in range(NL):
        if do_setup:
            setup(l)
        for n in range(NT):
            tile(l, n, xs_dram[l], xs_dram[l + 1])
    print('SBUF bytes/partition', R.sb_bytes)
    R.finish(final_reads=['dst%d_%d' % (NL - 1, n) for n in range(max(0, NT - 2), NT)] + (['dbgout%d' % n for n in range(NT)] + dumps if dbg else []))
    return nc


def _bf(a):
    return np.ascontiguousarray(a).astype(ml_dtypes.bfloat16)


def make_consts(NT):
    idn = np.eye(128, dtype=np.float32)
    j = np.arange(128)[:, None]
    t = np.arange(128)[None, :]
    Sh = [(j == t - s).astype(np.float32) for s in range(4)]
    ShP = [(j == 128 + t - s).astype(np.float32) for s in range(1, 4)]
    Ui = (j <= t).astype(np.float32)
    negLs = -(t < j).astype(np.float32)
    negUs = -(t > j).astype(np.float32)
    hm = []
    ii_, jj_ = np.arange(128)[:, None], np.arange(128)[None, :]
    for lev in range(7):
        b_ = 1 << lev
        hm.append(((ii_ // (2 * b_) == jj_ // (2 * b_)) & (ii_ % (2 * b_) >= b_) & (jj_ % (2 * b_) < b_)).astype(np.float32))
    hm = hm + [m_.T.copy() for m_ in hm]
    cb = np.concatenate([idn] + Sh + ShP + [np.tile(Ui, (1, 4)), np.tile(negLs, (1, 4)), np.tile(negUs, (1, 4))] + hm, axis=1)
    Esel = np.zeros((128, 512), np.float32)
    for h in range(4):
        Esel[h, h * 128:(h + 1) * 128] = 1.0
    GCt = np.zeros((128, 256), np.float32)
    for h in range(4):
        GCt[:, h * 64:(h + 1) * 64] = np.float32(GAMMA[h]) ** 128
    cf = np.concatenate([idn, Ui, np.ones((128, 128), np.float32), Esel, GCt], axis=1).astype(np.float32)
    pos = np.arange(NT * 128, dtype=np.float64)
    inv = 10000.0 ** (-np.arange(0, 64, 2, dtype=np.float64) / 64)
    ang = pos[:, None] * inv[None, :]
    cos, sin = np.cos(ang), np.sin(ang)
    ii = (np.arange(NT * 128) % 128).astype(np.float64)
    Cq = np.zeros((NT * 128, 4, 32, 2)); Sq = np.zeros_like(Cq); Ck = np.zeros_like(Cq); Sk = np.zeros_like(Cq)
    for h in range(4):
        dq = (GAMMA[h] ** (ii + 1.0)) * (64 ** -0.5)
        dk = GAMMA[h] ** (-(ii + 1.0))
        for (Ct, St, dd) in ((Cq, Sq, dq), (Ck, Sk, dk)):
            Ct[:, h, :, 0] = cos * dd[:, None]
            Ct[:, h, :, 1] = cos * dd[:, None]
            St[:, h, :, 0] = -sin * dd[:, None]
            St[:, h, :, 1] = sin * dd[:, None]
    ropec = np.concatenate([Cq.reshape(-1, 256), Ck.reshape(-1, 256)], axis=1).reshape(NT, 128, 512).astype(np.float32)
    ropes = np.concatenate([Sq.reshape(-1, 256), Sk.reshape(-1, 256)], axis=1).reshape(NT, 128, 512).astype(np.float32)
    return dict(cb=_bf(cb), cf=cf, ropec=ropec, ropes=ropes)


def make_params(inp, layers):
    f = lambda k: np.asarray(inp[k], dtype=np.float32)
    L = list(layers)
    NL = len(L)
    w_in = f('w_in')[L][:, :, PERM]
    w_out = f('w_out')[L]
    gpre = f('norm_pre')[L].reshape(NL, 8, 128).transpose(0, 2, 1)
    gpost = f('norm_post')[L].reshape(NL, 1, D)
    ng = np.concatenate([np.tile(f('ret_norm')[L], (1, 4)), np.tile(f('dn_norm')[L], (1, 4)),
                         np.ones((NL, 256), np.float32), np.tile(f('gla_norm')[L], (1, 4))], axis=1).reshape(NL, 8, 128).transpose(0, 2, 1)
    cw = f('dn_conv')[L].reshape(NL, 1, 4 * 768)
    dnp = np.concatenate([f('dn_a_log')[L], f('dn_dt_bias')[L]], axis=1).reshape(NL, 1, 8)

    def st(a):
        return a.reshape(NL, 8, 2, 64).transpose(0, 2, 3, 1).reshape(NL, 128, 8)
    ldt = np.repeat(f('s5_log_dt')[L][:, :, None], 64, axis=2)
    s5p = np.concatenate([st(f('s5_lam_re')[L]), st(f('s5_lam_im')[L]), st(ldt)], axis=2)
    bt = np.zeros((NL, 2, 128, 8, 128), np.float32)
    ct = np.zeros((NL, 2, 128, 8, 32), np.float32)
    dg = np.zeros((NL, 128, 8, 32), np.float32)
    bre, bim, cre, cim, dd = f('s5_b_re')[L], f('s5_b_im')[L], f('s5_c_re')[L], f('s5_c_im')[L], f('s5_d')[L]
    for g in range(16):
        s_, gg = g // 2, g % 2
        r0 = 32 * (s_ % 4) + gg * 16
        for k_, (bb, cc) in enumerate(((bre, cre), (bim, cim))):
            bt[:, k_, r0:r0 + 16, s_, gg * 64:(gg + 1) * 64] = bb[:, g].transpose(0, 2, 1)
            ct[:, k_, gg * 64:(gg + 1) * 64, s_, gg * 16:(gg + 1) * 16] = cc[:, g].transpose(0, 2, 1)
        for h in range(16):
            dg[:, r0 + h, s_, gg * 16 + h] = dd[:, g, h]
    bt = bt.transpose(0, 2, 1, 3, 4).reshape(NL, 128, 2048)
    ct = ct.transpose(0, 2, 1, 3, 4).reshape(NL, 128, 512)
    dg = dg.reshape(NL, 128, 256)
    wglu = f('s5_w_glu')[L].reshape(NL, 2, 128, 256).transpose(0, 2, 1, 3).reshape(NL, 128, 512)
    bglu = f('s5_b_glu')[L].reshape(NL, 1, 256)
    wgk = f('gla_w_gk')[L]
    bgk = f('gla_b_gk')[L].reshape(NL, 1, 128)
    c = np.ascontiguousarray
    return dict(w_in=c(w_in), w_out=c(w_out), gpre=c(gpre), gpost=c(gpost), ng=c(ng), cw=c(cw), dnp=c(dnp), s5p=c(s5p),
                s5bt=c(bt), s5ct=c(ct), s5dg=c(dg), wglu=c(wglu), bglu=c(bglu), wgk=c(wgk), bgk=c(bgk))


_PROG = {}


def kernel(**inputs):
    x = np.asarray(inputs['x'], dtype=np.float32)
    B, L, _ = x.shape
    NT = L // 128
    NLAY = np.asarray(inputs['w_in']).shape[0]
    key = (NT, NLAY)
    if key not in _PROG:
        _PROG[key] = (build_program(NT, NLAY), make_consts(NT))
    nc, consts = _PROG[key]
    prm = make_params(inputs, range(NLAY))
    in_maps = []
    for b in range(B):
        m = dict(consts)
        m.update(prm)
        m['x'] = np.ascontiguousarray(x[b])
        in_maps.append(m)
    res = run_bass_kernel_spmd(nc, in_maps, core_ids=list(range(B)))
    return np.stack([np.asarray(res.results[b]['out'], dtype=np.float32) for b in range(B)], axis=0)
```

```python
import os
import numpy as np
import ml_dtypes
import concourse.bass as bass
import concourse.mybir as mybir
from concourse.bass_utils import run_bass_kernel_spmd

F32 = mybir.dt.float32
BF16 = mybir.dt.bfloat16
AF = mybir.ActivationFunctionType
ALU = mybir.AluOpType
AX = mybir.AxisListType


class Rec:
    def __init__(self, nc):
        self.nc = nc
        self.ops = {k: [] for k in ('pe', 'act', 'dve', 'pool', 'sp')}
        self.cnt = {k: 0 for k in self.ops}
        self.clock = {k: {} for k in self.ops}
        self.snap = {}
        self.last_w = {}
        self.readers = {}
        self.sems = {}
        self.dma_cnt = {}

    def sem(self, key):
        if key not in self.sems:
            self.sems[key] = self.nc.alloc_semaphore(name="s_" + key.replace(':', '_'))
        return self.sems[key]

    def sb(self, name, shape, dt):
        n = 1
        for d_ in shape[1:]:
            n *= d_
        self.sb_bytes = getattr(self, 'sb_bytes', 0) + n * (2 if dt == BF16 else 4)
        return self.nc.alloc_sbuf_tensor("s_" + name, list(shape), dt)

    def ps(self, name, shape, dt=F32):
        return self.nc.alloc_psum_tensor("q_" + name, list(shape), dt)

    def _deps(self, eng, r, w):
        deps = {}

        def add(kn):
            if kn is None:
                return
            k, n = kn
            if deps.get(k, 0) < n:
                deps[k] = n
        for b in r:
            add(self.last_w.get(b))
        for b in w:
            add(self.last_w.get(b))
            for kn in self.readers.get(b, ()):
                add(kn)
        ck = self.clock[eng]
        waits = []
        if eng == 'pe':
            deps.pop('pe', None)
        for k, n in deps.items():
            if ck.get(k, 0) < n:
                waits.append((k, n))
        for k, n in waits:
            sn = self.snap.get((k, n))
            if sn:
                for k2, n2 in sn.items():
                    if ck.get(k2, 0) < n2:
                        ck[k2] = n2
            if ck.get(k, 0) < n:
                ck[k] = n
        return waits

    def _mark(self, me, r, w):
        for b in r:
            self.readers.setdefault(b, []).append(me)
        for b in w:
            self.last_w[b] = me
            self.readers[b] = []

    def op(self, eng, fn, r=(), w=()):
        w = list(w) + [b_ for b_ in r if b_.startswith('PS') and b_ not in w]
        waits = self._deps(eng, r, w)
        self.cnt[eng] += 1
        me = (eng, self.cnt[eng])
        self.snap[me] = dict(self.clock[eng])
        self.ops[eng].append((waits, fn, eng, 1))
        self._mark(me, r, w)

    def dma(self, q, out, in_, key, r=(), w=()):
        waits = self._deps(q, r, w)
        dk = 'dma:' + key
        self.dma_cnt[dk] = self.dma_cnt.get(dk, 0) + 1
        me = (dk, self.dma_cnt[dk])
        self.snap[me] = dict(self.clock[q])
        self.ops[q].append((waits, lambda e: e.dma_start(out=out, in_=in_), dk, 16))
        self._mark(me, r, w)

    def finish(self, final_reads=()):
        waits = self._deps('sp', list(final_reads), [])
        nc = self.nc
        semval = lambda k, n: (self.sem(k), n * 16 if k.startswith('dma:') else n)
        for k in self.ops:
            self.sem(k)
        final = [semval(k, n) for k, n in waits]
        for k in ('pe', 'act', 'dve', 'pool'):
            if self.cnt[k] > 0:
                final.append((self.sem(k), self.cnt[k]))
        for dk, n in self.dma_cnt.items():
            final.append((self.sem(dk), 16 * n))
        emit_lists = {}
        for eng, lst in self.ops.items():
            el = []
            for waits_, fn, inck, incv in lst:
                el.append(([semval(k, n) for k, n in waits_], fn, self.sem(inck), incv))
            emit_lists[eng] = el
        with nc.Block() as block:
            def run(e, el, extra=()):
                for ws, fn, s, v in el:
                    for sm, val in ws:
                        e.wait_ge(sm, val)
                    fn(e).then_inc(s, v)
                for sm, val in extra:
                    e.wait_ge(sm, val)

            @block.sync
            def _(e):
                run(e, emit_lists['sp'], final)

            @block.tensor
            def _(e):
                run(e, emit_lists['pe'])

            @block.scalar
            def _(e):
                run(e, emit_lists['act'])

            @block.vector
            def _(e):
                run(e, emit_lists['dve'])

            @block.gpsimd
            def _(e):
                run(e, emit_lists['pool'])


D = 1024
NCOL = 3352
ORIG = dict(rq=(0, 256), rk=(256, 512), rv=(512, 768), rg=(768, 1024), dq=(1024, 1280), dk=(1280, 1536),
            dv=(1536, 1792), dbeta=(1792, 1796), da=(1796, 1800), dg=(1800, 2056), su=(2056, 2312),
            sg=(2312, 2568), gq=(2568, 2696), gk=(2696, 2824), gv=(2824, 3080), gcode=(3080, 3096),
            gg=(3096, 3352))
ORDER = ['rq', 'rk', 'rv', 'gv', 'dq', 'dk', 'dv', 'su', 'gq', 'gk', 'gcode', 'dbeta', 'da', 'rg', 'dg', 'sg', 'gg']
COL = {}
_o = 0
for _k in ORDER:
    _w = ORIG[_k][1] - ORIG[_k][0]
    COL[_k] = (_o, _o + _w)
    _o += _w
PERM = np.concatenate([np.arange(*ORIG[k]) for k in ORDER])
BANKS = [(0, 512), (512, 1024), (1024, 1536), (1536, 2048), (2048, 2328), (2328, 2840), (2840, 3352)]
C = 128
GAMMA = [1.0 - 2.0 ** (-5.0 - h) for h in range(4)]


def build_program(NT, NL, dbg=False, upto=99, do_setup=True):
    nc = bass.Bass("TRN2", target_bir_lowering=False)
    R = Rec(nc)
    din = lambda name, shape, dt=F32: nc.dram_tensor(name, list(shape), dt, kind="ExternalInput").ap()
    x_in = din("x", [NT * 128, D])
    x_out = nc.dram_tensor("out", [NT * 128, D], F32, kind="ExternalOutput").ap()
    xs_dram = [x_in]
    for l in range(NL - 1):
        xs_dram.append(nc.dram_tensor("xmid%d" % l, [NT * 128, D], F32).ap())
    xs_dram.append(x_out)
    d_dbg = nc.dram_tensor("dbg", [NT * 128, D], BF16, kind="ExternalOutput").ap() if dbg else None
    d_win = din("w_in", [NL, D, NCOL])
    d_wout = din("w_out", [NL, D, D])
    d_gpre = din("gpre", [NL, 128, 8])
    d_gpost = din("gpost", [NL, 1, D])
    d_ng = din("ng", [NL, 128, 8])
    d_cw = din("cw", [NL, 1, 4 * 768])
    d_dnp = din("dnp", [NL, 1, 8])
    d_s5p = din("s5p", [NL, 128, 24])
    d_bt = din("s5bt", [NL, 128, 2 * 8 * 128])
    d_ct = din("s5ct", [NL, 128, 2 * 8 * 32])
    d_dg = din("s5dg", [NL, 128, 8 * 32])
    d_wglu = din("wglu", [NL, 128, 2 * 256])
    d_bglu = din("bglu", [NL, 1, 256])
    d_wgk = din("wgk", [NL, 16, 128])
    d_bgk = din("bgk", [NL, 1, 128])
    d_ropec = din("ropec", [NT, 128, 512])
    d_ropes = din("ropes", [NT, 128, 512])
    d_cb = din("cb", [128, 128 * 8 + 512 * 3 + 14 * 128], BF16)
    d_cf = din("cf", [128, 128 * 3 + 512 + 256])

    sb, ps = R.sb, R.ps
    cb = sb("cb", [128, 128 * 8 + 1536 + 14 * 128], BF16)
    cf = sb("cf", [128, 128 * 3 + 768], F32)
    identb = cb[:, 0:128]
    Sh = [cb[:, 128 * (1 + s):128 * (2 + s)] for s in range(4)]
    ShP = [None] + [cb[:, 128 * (4 + s):128 * (5 + s)] for s in range(1, 4)]
    Ui4 = cb[:, 1024:1536]
    negLs4 = cb[:, 1536:2048]
    negUs4 = cb[:, 2048:2560]
    HM = [cb[:, 2560 + i * 128:2560 + (i + 1) * 128] for i in range(14)]
    identf = cf[:, 0:128]
    triU = cf[:, 128:256]
    ones = cf[:, 256:384]
    Esel = cf[0:4, 384:896]
    GC = cf[0:64, 896:1152]
    R.dma('sp', cb[:], d_cb[:, :], key='cb', w=['cb'])
    R.dma('sp', cf[:], d_cf[:, :], key='cf', w=['cf'])
    onesb = sb("onesb", [1, 128], BF16)
    R.op('dve', lambda e: e.tensor_copy(out=onesb[:], in_=cf[0:1, 256:384]), r=['cf'], w=['onesb'])

    Wb = sb("Wb", [128, 8, NCOL], BF16)
    Wo = sb("Wo", [128, 8, D], BF16)
    stage = sb("stage", [128, NCOL], F32)
    p = stage
    gpre = sb("gpre", [128, 8], F32)
    Gpost = sb("Gpost", [128, D], F32)
    NG = sb("NG", [128, 8], F32)
    cw = sb("cw", [128, 4, 768], BF16)
    dnp = sb("dnp", [128, 8], F32)
    s5p = sb("s5p", [128, 24], F32)
    BT = sb("BT", [128, 2, 8, 128], BF16)
    CT = sb("CT", [128, 2, 8, 32], BF16)
    Dg = sb("Dg", [128, 8, 32], BF16)
    Wglu = sb("Wglu", [128, 2, 256], BF16)
    bgluf = sb("bgluf", [1, 256], F32)
    bglu = sb("bglu", [1, 256], BF16)
    wgkf = sb("wgkf", [16, 128], F32)
    wgk = sb("wgk", [16, 128], BF16)
    bgkf = sb("bgkf", [1, 128], F32)
    bgk = sb("bgk", [1, 128], BF16)
    Tc = sb("Tc", [128, 8, 128], F32)
    Ts = sb("Ts", [128, 8, 128], F32)
    Oc = sb("Oc", [128, 8, 128], F32)
    Os = sb("Os", [128, 8, 128], F32)
    MAGz = sb("MAGz", [128, 8, 128], F32)
    s5t = sb("s5t", [128, 40, 8], F32)
    Rr32 = sb("Rr32", [64, 256], F32)
    Rrb = sb("Rrb", [64, 256], BF16)
    Sd32 = sb("Sd32", [64, 256], F32)
    Sdb = sb("Sdb", [64, 256], BF16)
    Gs32 = sb("Gs32", [32, 256], F32)
    Gsb = sb("Gsb", [32, 256], BF16)
    mc = sb("mc", [128, 2, 8], F32)
    xt1 = sb("xt", [128, D], F32)
    xt = [xt1, xt1]
    ropeC1 = sb("ropeC", [128, 512], F32)
    ropeS1 = sb("ropeS", [128, 512], F32)
    ropeC = [ropeC1, ropeC1]
    ropeS = [ropeS1, ropeS1]
    sm = sb("sm", [128, 72], F32)
    xs = sb("xs", [128, D], BF16)
    yb = xs
    hT = sb("hT", [128, 8, 128], BF16)
    ngg = sb("ngg", [128, D], BF16)
    qk = sb("qk", [128, 512], BF16)
    vb = sb("vb", [128, 512], BF16)
    sT = sb("sT", [128, 512], BF16)
    tmpR = sb("tmpR", [64, 256], F32)
    pk = [sb("pk%d" % i, [128, 4, 768], BF16) for i in range(2)]
    qn = sb("qn", [128, 256], BF16)
    kn = sb("kn", [128, 256], BF16)
    kbq = sb("kbq", [128, 512], BF16)
    dT = sb("dT", [64, 16, 128], BF16)
    qkT = dT[:, 0:8, :]
    gct = sb("gct", [4, 128], F32)
    PP = sb("PP", [128, 4, 512], BF16)
    Pm = [PP[:, 0, :], PP[:, 2, :]]
    PTm = [PP[:, 1, :], PP[:, 3, :]]
    attnT = sb("attnT", [128, 512], BF16)
    Xb = sb("Xb", [128, 4, 128], BF16)
    WT = sb("WT", [64, 4, 128], BF16)
    vnew = sb("vnew", [128, 256], BF16)
    kg = sb("kg", [128, 256], BF16)
    ub = sb("ub", [128, 256], BF16)
    uT = sb("uT", [128, 2, 128], BF16)
    A = [sb("A%d" % i, [128, 8, 128], F32) for i in range(4)]
    _fl = lambda a_: a_.rearrange("p a b -> p (a b)")
    tmpL, tmpU = _fl(A[0][:, 0:4, :]), _fl(A[0][:, 4:8, :])
    DLs, DUs = tmpL, tmpU
    DUi, X32 = _fl(A[1][:, 0:4, :]), A[1][:, 4:8, :]
    cs = _fl(A[2][:])[:, 0:768]
    sq = _fl(A[3][:])[:, 0:512]
    xo = [_fl(A[0][:]), _fl(A[0][:])]
    yT, gT, gsT = hT, qkT[0:32], sT
    sre = PP[:, 0:2, :].rearrange("p a (b c) -> p (a b) c", c=128)
    sim = PP[:, 2:4, :].rearrange("p a (b c) -> p (a b) c", c=128)
    ygb = sb("ygb", [128, 256], BF16)
    ygT = sb("ygT", [128, 2, 128], BF16)
    gcb = sb("gcb", [128, 16], BF16)
    gcT = sb("gcT", [16, 128], BF16)
    Gb = sb("Gb", [128, 768], F32)
    g128 = [Gb[:, i * 128:(i + 1) * 128] for i in range(6)]
    t256, u256, yg = Gb[:, 0:256], Gb[:, 256:512], Gb[:, 512:768]
    gq3 = sb("gq3", [128, 384], BF16)
    PS = [ps("PS%d" % i, [128, 1024], F32) for i in range(4)]
    bank_ctr = [0]

    def bank():
        i = bank_ctr[0] % 8
        bank_ctr[0] += 1
        return PS[i // 2][:, (i % 2) * 512:(i % 2) * 512 + 512], 'PS%d_%d' % (i // 2, i % 2)

    def dbank():
        if bank_ctr[0] % 2:
            bank_ctr[0] += 1
        i = bank_ctr[0] % 8
        bank_ctr[0] += 2
        return PS[i // 2], ['PS%d_0' % (i // 2), 'PS%d_1' % (i // 2)]

    op = R.op
    dumps = []

    def dump(tag, ap, names):
        if not dbg:
            return
        d = nc.dram_tensor("dump_" + tag, list(ap.shape), ap.dtype, kind="ExternalOutput").ap()
        R.dma('sp', d, ap, key='dump_' + tag, r=names, w=['dumpout_' + tag])
        dumps.append('dumpout_' + tag)
    TT = lambda out, a, b, o: (lambda e: e.tensor_tensor(out=out, in0=a, in1=b, op=o))
    TS = lambda out, a, s1, s2, o0, o1=None: (lambda e: e.tensor_scalar(out=out, in0=a, scalar1=s1, scalar2=s2, op0=o0, **({} if o1 is None else {'op1': o1})))
    STT = lambda out, a, s, b, o0, o1: (lambda e: e.scalar_tensor_tensor(out=out, in0=a, scalar=s, in1=b, op0=o0, op1=o1))
    ACT = lambda out, a, f, **kw: (lambda e: e.activation(out=out, in_=a, func=f, **kw))
    CP = lambda out, a: (lambda e: e.tensor_copy(out=out, in_=a))
    MM = lambda out, l, r_, st=True, sp=True: (lambda e: e.matmul(out, lhsT=l, rhs=r_, start=st, stop=sp))
    TR = lambda out, a, idn: (lambda e: e.transpose(out=out, in_=a, identity=idn))
    mult, add, sub = ALU.mult, ALU.add, ALU.subtract

    def rsqrt_small(ap, names, scale, eps):
        op('dve', TS(ap, ap, scale, eps, mult, add), r=names, w=names)
        op('act', ACT(ap, ap, AF.Sqrt), r=names, w=names)
        op('dve', lambda e: e.reciprocal(out=ap, in_=ap), r=names, w=names)

    def setup(l):
        R.dma('sp', gpre[:], d_gpre[l], key='gpre', w=['gpre'])
        R.dma('sp', NG[:], d_ng[l], key='ng', w=['NG'])
        for c in range(8):
            R.dma('sp', stage[:], d_win[l, c * 128:(c + 1) * 128, :], key='stage', w=['p%d' % i for i in range(7)])
            op('dve', TS(Wb[:, c, :], stage[:], gpre[:, c:c + 1], None, mult), r=['p%d' % i for i in range(7)] + ['gpre'], w=['Wb'])
        for c in range(8):
            R.dma('sp', stage[:, 0:D], d_wout[l, c * 128:(c + 1) * 128, :], key='stage', w=['p%d' % i for i in range(7)])
            op('act', ACT(Wo[:, c, :], stage[:, 0:D], AF.Copy, scale=NG[:, c:c + 1]), r=['p%d' % i for i in range(7)] + ['NG'], w=['Wo'])
        R.dma('sp', Gpost[:], d_gpost[l].partition_broadcast(128), key='gpost', w=['Gpost'])
        R.dma('sp', stage[:, 0:3072], d_cw[l].partition_broadcast(128), key='stage', w=['p%d' % i for i in range(7)])
        op('dve', CP(cw[:].rearrange("p a b -> p (a b)"), stage[:, 0:3072]), r=['p%d' % i for i in range(7)], w=['cw'])
        R.dma('sp', dnp[:], d_dnp[l].partition_broadcast(128), key='dnp', w=['dnp'])
        R.dma('sp', s5p[:], d_s5p[l], key='s5p', w=['s5p'])
        BTf, CTf, Dgf, Wgluf = stage[:, 0:2048], stage[:, 2048:2560], stage[:, 2560:2816], stage[:, 2816:3328]
        PN = ['p%d' % i for i in range(7)]
        R.dma('sp', BTf, d_bt[l], key='stage', w=PN)
        R.dma('sp', CTf, d_ct[l], key='stage2', w=PN)
        R.dma('sp', Dgf, d_dg[l], key='stage3', w=PN)
        R.dma('sp', Wgluf, d_wglu[l], key='stage4', w=PN)
        R.dma('sp', bgluf[:], d_bglu[l], key='bglu', w=['bgluf'])
        R.dma('sp', wgkf[:], d_wgk[l], key='wgk', w=['wgkf'])
        R.dma('sp', bgkf[:], d_bgk[l], key='bgk', w=['bgkf'])
        op('dve', CP(BT[:].rearrange("p a b c -> p (a b c)"), BTf), r=PN, w=['BT'])
        op('dve', CP(CT[:, 0].rearrange("p b c -> p (b c)"), CTf[:, 0:256]), r=PN, w=['CT'])
        op('dve', TS(CT[:, 1].rearrange("p b c -> p (b c)"), CTf[:, 256:512], -1.0, None, mult), r=PN, w=['CT'])
        op('dve', CP(Dg[:].rearrange("p b c -> p (b c)"), Dgf), r=PN, w=['Dg'])
        op('dve', CP(Wglu[:].rearrange("p b c -> p (b c)"), Wgluf), r=PN, w=['Wglu'])
        op('dve', CP(bglu[:], bgluf[:]), r=['bgluf'], w=['bglu'])
        op('dve', CP(wgk[:], wgkf[:]), r=['wgkf'], w=['wgk'])
        op('dve', CP(bgk[:], bgkf[:]), r=['bgkf'], w=['bgk'])
        op('act', ACT(dnp[:, 0:4], dnp[:, 0:4], AF.Exp), r=['dnp'], w=['dnp'])
        op('dve', TS(dnp[:, 0:4], dnp[:, 0:4], -1.0, None, mult), r=['dnp'], w=['dnp'])
        for nm, t_ in (('Rr32', Rr32), ('Sd32', Sd32), ('Gs32', Gs32), ('Rrb', Rrb), ('Sdb', Sdb), ('Gsb', Gsb), ('mc', mc)):
            ap_ = t_[:] if len(t_.shape) == 2 else t_[:].rearrange("p a b -> p (a b)")
            op('pool', lambda e, ap_=ap_: e.memset(ap_, 0.0), r=[], w=[nm])
        for i in range(2):
            op('pool', lambda e, i=i: e.memset(pk[i][:].rearrange("p a b -> p (a b)"), 0.0), r=[], w=['pk%d' % i])
        T = lambda i: s5t[:, i, :]
        S = ['s5t']
        lre, lim, ldt = s5p[:, 0:8], s5p[:, 8:16], s5p[:, 16:24]
        dt_, mag, th, xx, x2, sn, cs_, t0, t1 = T(0), T(1), T(2), T(3), T(4), T(5), T(6), T(7), T(8)
        op('act', ACT(dt_, ldt, AF.Exp), r=['s5p'], w=S)
        op('dve', TT(mag, lre, dt_, mult), r=['s5p'] + S, w=S)
        op('act', ACT(mag, mag, AF.Exp), r=S, w=S)
        op('dve', TT(th, lim, dt_, mult), r=['s5p'] + S, w=S)
        op('dve', TS(xx, th, 1.0 / 16, None, mult), r=S, w=S)
        op('dve', TT(x2, xx, xx, mult), r=S, w=S)
        sc = [1.0, -1.0 / 6, 1.0 / 120, -1.0 / 5040, 1.0 / 362880, -1.0 / 39916800, 1.0 / 6227020800]
        cc = [1.0, -1.0 / 2, 1.0 / 24, -1.0 / 720, 1.0 / 40320, -1.0 / 3628800, 1.0 / 479001600, -1.0 / 87178291200]
        op('dve', TS(sn, x2, sc[6], sc[5], mult, add), r=S, w=S)
        for k_ in (4, 3, 2, 1, 0):
            op('dve', TT(sn, sn, x2, mult), r=S, w=S)
            op('dve', TS(sn, sn, sc[k_], None, add), r=S, w=S)
        op('dve', TT(sn, sn, xx, mult), r=S, w=S)
        op('dve', TS(cs_, x2, cc[7], cc[6], mult, add), r=S, w=S)
        for k_ in (5, 4, 3, 2, 1, 0):
            op('dve', TT(cs_, cs_, x2, mult), r=S, w=S)
            op('dve', TS(cs_, cs_, cc[k_], None, add), r=S, w=S)
        for _ in range(4):
            op('dve', TT(t0, cs_, cs_, mult), r=S, w=S)
            op('dve', TT(t1, sn, sn, mult), r=S, w=S)
            op('dve', TT(sn, sn, cs_, mult), r=S, w=S)
            op('dve', TS(sn, sn, 2.0, None, mult), r=S, w=S)
            op('dve', TT(cs_, t0, t1, sub), r=S, w=S)
        are, aim, den, cre, cim = T(9), T(10), T(11), T(12), T(13)
        op('dve', TT(are, mag, cs_, mult), r=S, w=S)
        op('dve', TT(aim, mag, sn, mult), r=S, w=S)
        op('dve', TT(t0, lre, lre, mult), r=['s5p'] + S, w=S)
        op('dve', TT(t1, lim, lim, mult), r=['s5p'] + S, w=S)
        op('dve', TT(den, t0, t1, add), r=S, w=S)
        op('dve', lambda e: e.reciprocal(out=den, in_=den), r=S, w=S)
        nr = T(14)
        op('dve', TS(nr, are, -1.0, None, add), r=S, w=S)
        op('dve', TT(t0, nr, lre, mult), r=['s5p'] + S, w=S)
        op('dve', TT(t1, aim, lim, mult), r=['s5p'] + S, w=S)
        op('dve', TT(cre, t0, t1, add), r=S, w=S)
        op('dve', TT(cre, cre, den, mult), r=S, w=S)
        op('dve', TT(t0, aim, lre, mult), r=['s5p'] + S, w=S)
        op('dve', TT(t1, nr, lim, mult), r=['s5p'] + S, w=S)
        op('dve', TT(cim, t0, t1, sub), r=S, w=S)
        op('dve', TT(cim, cim, den, mult), r=S, w=S)
        op('pool', lambda e: e.memset(Oc[:, :, 0:1], 1.0), r=[], w=['Oc'])
        op('pool', lambda e: e.memset(Os[:, :, 0:1], 0.0), r=[], w=['Os'])
        op('dve', CP(Oc[:, :, 1], cs_), r=S, w=['Oc'])
        op('dve', CP(Os[:, :, 1], sn), r=S, w=['Os'])
        OO = ['Oc', 'Os']
        k_ = 1
        while k_ < 128:
            bc = lambda t_, k_=k_: t_[:, :, k_:k_ + 1].to_broadcast([128, 8, k_])
            hi = slice(k_ + 1, 2 * k_ + 1) if 2 * k_ + 1 <= 128 else slice(k_ + 1, 128)
            n_ = hi.stop - hi.start
            lo = slice(1, 1 + n_)
            bcn = lambda t_, k_=k_, n_=n_: t_[:, :, k_:k_ + 1].to_broadcast([128, 8, n_])
            a1, a2 = A[0][:, :, 0:n_], A[1][:, :, 0:n_]
            op('dve', TT(a1, Oc[:, :, lo], bcn(Oc), mult), r=OO, w=['A0'])
            op('dve', TT(a2, Os[:, :, lo], bcn(Os), mult), r=OO, w=['A1'])
            op('dve', TT(Oc[:, :, hi], a1, a2, sub), r=['A0', 'A1'] + OO, w=['Oc'])
            op('dve', TT(a1, Oc[:, :, lo], bcn(Os), mult), r=OO, w=['A0'])
            op('dve', TT(a2, Os[:, :, lo], bcn(Oc), mult), r=OO, w=['A1'])
            op('dve', TT(Os[:, :, hi], a1, a2, add), r=['A0', 'A1'] + OO, w=['Os'])
            k_ *= 2
        bc8 = lambda t_: t_.unsqueeze(2).to_broadcast([128, 8, 128])
        op('dve', TT(A[0][:], Oc[:], bc8(cre), mult), r=OO + S, w=['A0'])
        op('dve', TT(A[1][:], Os[:], bc8(cim), mult), r=OO + S, w=['A1'])
        op('dve', TT(Tc[:], A[0][:], A[1][:], add), r=['A0', 'A1'], w=['Tc'])
        op('dve', TT(A[0][:], Oc[:], bc8(cim), mult), r=OO + S, w=['A0'])
        op('dve', TT(A[1][:], Os[:], bc8(cre), mult), r=OO + S, w=['A1'])
        op('dve', TT(Ts[:], A[0][:], A[1][:], sub), r=['A0', 'A1'], w=['Ts'])
        op('dve', CP(MAGz[:], bc8(mag)), r=S, w=['MAGz'])
        op('pool', lambda e: e.memset(MAGz[:, :, 0:1], 0.0), r=[], w=['MAGz'])
        mec, mes = T(15), T(16)
        op('dve', TT(t0, Oc[:, :, 127], cs_, mult), r=OO + S, w=S)
        op('dve', TT(t1, Os[:, :, 127], sn, mult), r=OO + S, w=S)
        op('dve', TT(mec, t0, t1, sub), r=S, w=S)
        op('dve', TT(t0, Oc[:, :, 127], sn, mult), r=OO + S, w=S)
        op('dve', TT(t1, Os[:, :, 127], cs_, mult), r=OO + S, w=S)
        op('dve', TT(mes, t0, t1, add), r=S, w=S)
        op('dve', TT(mec, mec, mag, mult), r=S, w=S)
        op('dve', TT(mes, mes, mag, mult), r=S, w=S)

    def branch_out(obank, obn, br):
        op('act', ACT(sq[:, 0:256], obank, AF.Square), r=[obn], w=['A3'])
        ssq = sm[:, 8 + 4 * br: 12 + 4 * br]
        nm = ['sm_b%d' % br]
        op('dve', lambda e: e.tensor_reduce(out=ssq, in_=sq[:, 0:256].rearrange("p (h d) -> p h d", h=4), axis=AX.X, op=add), r=['A3'], w=nm)
        rsqrt_small(ssq, nm, 1.0 / 64, 1e-6)
        hv = lambda a_: a_.rearrange("p (h d) -> p h d", h=4)
        op('dve', TT(hv(sq[:, 0:256]), hv(obank), ssq.unsqueeze(2).to_broadcast([128, 4, 64]), mult), r=[obn, 'A3'] + nm, w=['A3'])
        op('dve', TT(yb[:, br * 256:(br + 1) * 256], sq[:, 0:256], ngg[:, br * 256:(br + 1) * 256], mult),
           r=['A3', 'ngg', 'hT'], w=['yb%d' % br])

    def early(l, n, dst, b):
        R.dma('sp', dst[n * 128:(n + 1) * 128, :], xt[b][:], key='xo', r=['xt'], w=['dst%d_%d' % (l, n)])

    def tile(l, n, src, dst):
        b = n % 2
        R.dma('sp', xt[b][:], src[n * 128:(n + 1) * 128, :], key='xt', r=(['dst%d_%d' % (l - 1, n)] if l > 0 else []), w=['xt'])
        R.dma('sp', ropeC[b][:], d_ropec[n], key='rc', w=['rc'])
        R.dma('sp', ropeS[b][:], d_ropes[n], key='rs', w=['rs'])
        X = 'xt'
        op('act', ACT(A[3][:].rearrange('p a b -> p (a b)'), xt[b][:], AF.Square, accum_out=sm[:, 0:1]), r=[X], w=['A3', 'sm0'])
        rsqrt_small(sm[:, 0:1], ['sm0'], 1.0 / D, 1e-6)
        op('act', ACT(xs[:], xt[b][:], AF.Copy, scale=sm[:, 0:1]), r=[X, 'sm0'], w=['xs', 'yb0', 'yb1', 'yb2', 'yb3'])
        bk, bn = bank()
        bkb = bk.bitcast(BF16)
        for c in range(8):
            op('pe', TR(bkb[:, c * 128:(c + 1) * 128], xs[:, c * 128:(c + 1) * 128], identb), r=['xs', 'cb'], w=[bn])
        op('dve', CP(hT[:].rearrange("p a b -> p (a b)"), bkb), r=[bn], w=['hT'])
        for i, (c0, c1) in enumerate(BANKS):
            bk, bn = bank()
            for c in range(8):
                op('pe', MM(bk[:, 0:c1 - c0], hT[:, c, :], Wb[:, c, c0:c1], c == 0, c == 7), r=['hT', 'Wb'], w=[bn])
            if i % 2 == 0:
                op('act', ACT(p[:, c0:c1], bk[:, 0:c1 - c0], AF.Copy), r=[bn], w=['p%d' % i])
            else:
                op('dve', CP(p[:, c0:c1], bk[:, 0:c1 - c0]), r=[bn], w=['p%d' % i])
        if upto < 1:
            return early(l, n, dst, b)
        g0 = COL['rg'][0]
        op('act', ACT(ngg[:], p[:, g0:g0 + D], AF.Silu), r=['p5', 'p6'], w=['ngg'])
        if upto < 2:
            return early(l, n, dst, b)
        RC, RS = ropeC[b], ropeS[b]
        A3f = A[3][:].rearrange('p a b -> p (a b)')
        m1, m2 = A3f[:, 0:512], A3f[:, 512:1024]
        op('dve', TT(m1, p[:, 0:512], RC[:], mult), r=['p0', 'rc'], w=['A3'])
        pv = p[:, 0:512].rearrange("p (a t) -> p a t", t=2)
        m2v = m2.rearrange("p (a t) -> p a t", t=2)
        sv = RS[:].rearrange("p (a t) -> p a t", t=2)
        op('dve', TT(m2v[:, :, 0], pv[:, :, 1], sv[:, :, 0], mult), r=['p0', 'rs'], w=['A3'])
        op('dve', TT(m2v[:, :, 1], pv[:, :, 0], sv[:, :, 1], mult), r=['p0', 'rs'], w=['A3'])
        op('dve', TT(qk[:], m1, m2, add), r=['A3'], w=['qk'])
        op('act', ACT(vb[:], p[:, 512:1024], AF.Copy), r=['p1'], w=['vb'])
        bk, bn = bank()
        bkb = bk.bitcast(BF16)
        for j in range(8):
            op('pe', TR(bkb[0:64, j * 128:(j + 1) * 128], qk[:, j * 64:(j + 1) * 64], identb), r=['qk', 'cb'], w=[bn])
        op('dve', CP(qkT[:].rearrange("p a b -> p (a b)"), bkb[0:64, :]), r=[bn], w=['dT0'])
        bk, bn = bank()
        for h in range(4):
            op('pe', MM(bk[:, h * 128:(h + 1) * 128], qkT[:, 4 + h, :], qkT[:, h, :]), r=['dT0'], w=[bn])
        op('dve', TT(sT[:], bk, Ui4, mult), r=[bn, 'cb'], w=['sT'])
        bo, bon = bank()
        for h in range(4):
            op('pe', MM(bo[:, h * 64:(h + 1) * 64], sT[:, h * 128:(h + 1) * 128], vb[:, h * 64:(h + 1) * 64], True, False), r=['sT', 'vb'], w=[bon])
            op('pe', MM(bo[:, h * 64:(h + 1) * 64], qkT[:, h, :], Rrb[:, h * 64:(h + 1) * 64], False, True), r=['dT0', 'Rrb'], w=[bon])
        bk, bn = bank()
        for h in range(4):
            op('pe', MM(bk[0:64, h * 64:(h + 1) * 64], qk[:, 256 + h * 64:256 + (h + 1) * 64], vb[:, h * 64:(h + 1) * 64]), r=['qk', 'vb'], w=[bn])
        op('dve', TT(tmpR[:], Rr32[:], bk[0:64, 0:256], add), r=['Rr32', bn], w=['tmpR'])
        op('pool', TT(Rr32[:], tmpR[:], GC, mult), r=['tmpR', 'cf'], w=['Rr32'])
        op('act', ACT(Rrb[:], Rr32[:], AF.Copy), r=['Rr32'], w=['Rrb'])
        branch_out(bo[:, 0:256], bon, 0)
        if upto < 3:
            return early(l, n, dst, b)
        c0 = COL['dq'][0]
        for k in range(4):
            op('pool', TT(pk[b][:, k, :], p[:, c0:c0 + 768], cw[:, k, :], mult), r=['p2', 'p3', 'cw'], w=['pk%d' % b])
        bq, bqn = bank()
        bv, bvn = bank()
        for (bk, bn, o0, w_) in ((bq, bqn, 0, 512), (bv, bvn, 512, 256)):
            taps = [(Sh[3 - k], pk[b][:, k, o0:o0 + w_], 'pk%d' % b) for k in range(4)]
            taps += [(ShP[3 - k], pk[1 - b][:, k, o0:o0 + w_], 'pk%d' % (1 - b)) for k in range(3)]
            for i, (lt, rh, nm) in enumerate(taps):
                op('pe', MM(bk[:, 0:w_], lt, rh, i == 0, i == len(taps) - 1), r=['cb', nm], w=[bn])
        if upto < 3.1:
            return early(l, n, dst, b)
        op('act', ACT(cs[:, 0:512], bq, AF.Silu), r=[bqn], w=['A2'])
        op('act', ACT(cs[:, 512:768], bv[:, 0:256], AF.Silu), r=[bvn], w=['A2'])
        op('act', ACT(sq[:], cs[:, 0:512], AF.Square), r=['A2'], w=['A3'])
        rn = sm[:, 24:32]
        op('dve', lambda e: e.tensor_reduce(out=rn, in_=sq[:].rearrange("p (h d) -> p h d", h=8), axis=AX.X, op=add), r=['A3'], w=['rn'])
        rsqrt_small(rn, ['rn'], 1.0, 1e-6)
        h64 = lambda a_: a_.rearrange("p (h d) -> p h d", h=4)
        bc64 = lambda a_: a_.unsqueeze(2).to_broadcast([128, 4, 64])
        op('dve', TS(rn[:, 0:4], rn[:, 0:4], 0.125, None, mult), r=['rn'], w=['rn'])
        op('dve', TT(h64(qn[:]), h64(cs[:, 0:256]), bc64(rn[:, 0:4]), mult), r=['A2', 'rn'], w=['qn'])
        op('dve', TT(h64(kn[:]), h64(cs[:, 256:512]), bc64(rn[:, 4:8]), mult), r=['A2', 'rn'], w=['kn'])
        beta, gz, gg_, gc, eg, egl, egll, beg = (sm[:, 32:36], sm[:, 36:40], sm[:, 40:44], sm[:, 44:52], sm[:, 52:56],
                                                  sm[:, 56:60], sm[:, 60:64], sm[:, 4:8])
        DS = ['dsm']
        cb_, ca_ = COL['dbeta'][0], COL['da'][0]
        op('act', ACT(beta, p[:, cb_:cb_ + 4], AF.Sigmoid), r=['p4'], w=DS)
        op('dve', TT(gz, p[:, ca_:ca_ + 4], dnp[:, 4:8], add), r=['p4', 'dnp'], w=DS)
        op('act', ACT(gz, gz, AF.Exp), r=DS, w=DS)
        op('act', ACT(gz, gz, AF.Ln, bias=1.0), r=DS, w=DS)
        op('dve', TT(gg_, gz, dnp[:, 0:4], mult), r=DS + ['dnp'], w=DS)
        if upto < 3.2:
            return early(l, n, dst, b)
        bg, bgn = bank()
        op('pe', MM(bg[:, 0:4], triU, gg_), r=['cf'] + DS, w=[bgn])
        op('pe', MM(bg[:, 4:8], ones, gg_), r=['cf'] + DS, w=[bgn])
        op('pe', MM(bg[0:4, 128:256], gg_, triU), r=['cf'] + DS, w=[bgn])
        op('dve', CP(gc, bg[:, 0:8]), r=[bgn], w=DS)
        op('dve', CP(gct[:], bg[0:4, 128:256]), r=[bgn], w=['gct'])
        op('act', ACT(eg, gc[:, 0:4], AF.Exp), r=DS, w=DS)
        op('dve', TT(egl, gc[:, 4:8], gc[:, 0:4], sub), r=DS, w=DS)
        op('act', ACT(egl, egl, AF.Exp), r=DS, w=DS)
        op('act', ACT(egll, gc[:, 4:8], AF.Exp), r=DS, w=DS)
        op('dve', TT(beg, beta, eg, mult), r=DS, w=DS)
        if upto < 3.3:
            return early(l, n, dst, b)
        bR, bRn = bank()
        for h in range(4):
            op('pe', MM(bR[:, h * 128:(h + 1) * 128], Esel[:, h * 128:(h + 1) * 128], gct[:]), r=['cf', 'gct'], w=[bRn])
        h128 = lambda a_: a_.rearrange("p (h c) -> p h c", h=4)
        op('dve', TT(h128(tmpU), h128(bR), gc[:, 0:4].unsqueeze(2).to_broadcast([128, 4, 128]), sub), r=[bRn] + DS, w=['A0'])
        op('dve', TS(tmpL, tmpU, 0.0, None, ALU.max), r=['A0'], w=['A0'])
        op('dve', TS(tmpU, tmpU, 0.0, None, ALU.min), r=['A0'], w=['A0'])
        if upto < 3.4:
            return early(l, n, dst, b)
        op('act', ACT(tmpL[:], tmpL[:], AF.Exp, scale=-1.0), r=['A0'], w=['A0'])
        op('act', ACT(tmpU[:], tmpU[:], AF.Exp), r=['A0'], w=['A0'])
        op('pool', TT(DLs[:], tmpL[:], negLs4, mult), r=['A0', 'cb'], w=['A0'])
        op('pool', TT(DUi[:], tmpU[:], Ui4, mult), r=['A0', 'cb'], w=['A1'])
        op('pool', TT(DUs[:], tmpU[:], negUs4, mult), r=['A0', 'cb'], w=['A0'])
        if l == 0 and n == 0:
            dump('cs', cs, ['A2']); dump('kn', kn[:], ['kn']); dump('qn', qn[:], ['qn']); dump('sm', sm[:, 24:64], DS + ['rn'])
            dump('DLs', DLs, ['A0']); dump('DUs', DUs, ['A0']); dump('DUi', DUi, ['A1'])
        op('dve', TT(h64(kbq[:, 0:256]), h64(kn[:]), bc64(beta), mult), r=['kn'] + DS, w=['kbq'])
        op('dve', TT(h64(kbq[:, 256:512]), h64(qn[:]), bc64(eg), mult), r=['qn'] + DS, w=['kbq'])
        if upto < 3.5:
            return early(l, n, dst, b)
        srcs = [(kn, 0, 'kn'), (kbq, 0, 'kbq'), (qn, 0, 'qn'), (kbq, 256, 'kbq')]
        for half in range(2):
            bk, bn = bank()
            bkb = bk.bitcast(BF16)
            for jj in range(8):
                j = half * 8 + jj
                t_, off, nm = srcs[j // 4]
                h = j % 4
                op('pe', TR(bkb[0:64, jj * 128:(jj + 1) * 128], t_[:, off + h * 64:off + (h + 1) * 64], identb), r=[nm, 'cb'], w=[bn])
            op('dve' if half == 0 else 'act', CP(dT[:, half * 8:(half + 1) * 8, :].rearrange("p a b -> p (a b)"), bkb[0:64, :]) if half == 0 else
               ACT(dT[:, half * 8:(half + 1) * 8, :].rearrange("p a b -> p (a b)"), bkb[0:64, :], AF.Copy), r=[bn], w=['dT%d' % half])
        bA, bAn = bank()
        bAT, bATn = bank()
        bat, batn = bank()
        for h in range(4):
            hs = slice(h * 128, (h + 1) * 128)
            op('pe', MM(bA[:, hs], dT[:, 4 + h, :], dT[:, h, :]), r=['dT0'], w=[bAn])
            op('pe', MM(bAT[:, hs], dT[:, h, :], dT[:, 4 + h, :]), r=['dT0'], w=[bATn])
            op('pe', MM(bat[:, hs], dT[:, h, :], dT[:, 8 + h, :]), r=['dT0', 'dT1'], w=[batn])
        op('dve', TT(Pm[0][:], bA, DLs[:], mult), r=[bAn, 'A0'], w=['Pm0'])
        op('dve', TT(PTm[0][:], bAT, DUs[:], mult), r=[bATn, 'A0'], w=['PTm0'])
        op('dve', TT(attnT[:], bat, DUi[:], mult), r=[batn, 'A1'], w=['attnT'])
        op('dve', TT(X32[:, :, 0:64], h64(cs[:, 512:768]), bc64(beta), mult), r=['A2'] + DS, w=['A1'])
        op('dve', TT(X32[:, :, 64:128], h64(kn[:]), bc64(beg), mult), r=['kn'] + DS, w=['A1'])
        if l == 0 and n == 0:
            dump('N0', Pm[0], ['Pm0']); dump('NT0', PTm[0], ['PTm0']); dump('attnT', attnT[:], ['attnT']); dump('X0', X32, ['A1'])
        X32f = X32[:].rearrange("p a b -> p (a b)")
        Xbf = Xb[:].rearrange("p a b -> p (a b)")
        op('act', ACT(Xbf, X32f, AF.Copy), r=['A1'], w=['Xb'])
        if upto < 3.6:
            return early(l, n, dst, b)
        Lm, LmT, Yb, W1b = sT[:], qk[:], kbq[:], xs[:, 512:1024]
        Tm, TTm = Pm[1], PTm[1]
        v4 = lambda a_: a_.rearrange("p (h c) -> p h c", h=4)
        bm = lambda i: HM[i].unsqueeze(1).to_broadcast([128, 4, 128])
        idb4 = identb.unsqueeze(1).to_broadcast([128, 4, 128])
        op('pool', TT(v4(Tm), v4(Pm[0]), bm(0), mult), r=['Pm0', 'cb'], w=['Pm1'])
        op('pool', TT(v4(Tm), v4(Tm), idb4, add), r=['Pm1', 'cb'], w=['Pm1'])
        op('pool', TT(v4(TTm), v4(PTm[0]), bm(7), mult), r=['PTm0', 'cb'], w=['PTm1'])
        op('pool', TT(v4(TTm), v4(TTm), idb4, add), r=['PTm1', 'cb'], w=['PTm1'])
        for lev in range(1, 7):
            op('pool', TT(v4(Lm), v4(Pm[0]), bm(lev), mult), r=['Pm0', 'cb'], w=['sT'])
            op('pool', TT(v4(LmT), v4(PTm[0]), bm(7 + lev), mult), r=['PTm0', 'cb'], w=['qk'])
            bY, bYn = bank()
            bW, bWn = bank()
            for h in range(4):
                hs = slice(h * 128, (h + 1) * 128)
                op('pe', MM(bY[:, hs], LmT[:, hs], Tm[:, hs]), r=['qk', 'Pm1'], w=[bYn])
                op('pe', MM(bW[:, hs], Lm[:, hs], TTm[:, hs]), r=['sT', 'PTm1'], w=[bWn])
            op('act', ACT(Yb, bY, AF.Copy), r=[bYn], w=['kbq'])
            op('dve', CP(W1b, bW), r=[bWn], w=['yb2', 'yb3'])
            bZ, bZn = bank()
            bW2, bW2n = bank()
            for h in range(4):
                hs = slice(h * 128, (h + 1) * 128)
                op('pe', MM(bZ[:, hs], TTm[:, hs], Yb[:, hs]), r=['kbq', 'PTm1'], w=[bZn])
                op('pe', MM(bW2[:, hs], Tm[:, hs], W1b[:, hs]), r=['yb2', 'yb3', 'Pm1'], w=[bW2n])
            op('dve', TT(Tm, Tm, bZ, add), r=['Pm1', bZn], w=['Pm1'])
            op('dve', TT(TTm, TTm, bW2, add), r=['PTm1', bW2n], w=['PTm1'])
        if upto < 3.7:
            return early(l, n, dst, b)
        bX, bXn = bank()
        for h in range(4):
            hs = slice(h * 128, (h + 1) * 128)
            op('pe', MM(bX[:, hs], TTm[:, hs], Xb[:, h, :]), r=['PTm1', 'Xb'], w=[bXn])
        op('dve', CP(X32f, bX), r=[bXn], w=['A1'])
        op('act', ACT(Xbf, bX, AF.Copy), r=[bXn], w=['Xb'])
        if l == 0 and n == 0:
            dump('X7', X32, ['A1'])
        bk, bn = bank()
        bkb = bk.bitcast(BF16)
        for h in range(4):
            op('pe', TR(bkb[0:64, h * 128:(h + 1) * 128], Xb[:, h, 64:128], identb), r=['Xb', 'cb'], w=[bn])
        op('dve', CP(WT[:].rearrange("p a b -> p (a b)"), bkb[0:64, 0:512]), r=[bn], w=['WT'])
        if upto < 3.8:
            return early(l, n, dst, b)
        bw, bwn = bank()
        for h in range(4):
            hs = slice(h * 64, (h + 1) * 64)
            op('pe', MM(bw[:, hs], WT[:, h, :], Sdb[:, hs]), r=['WT', 'Sdb'], w=[bwn])
        op('dve', TT(vnew[:].rearrange("p (h d) -> p h d", h=4), X32[:, :, 0:64], bw[:, 0:256].rearrange("p (h d) -> p h d", h=4), sub),
           r=['A1', bwn], w=['vnew'])
        if upto < 3.85:
            return early(l, n, dst, b)
        bo, bon = bank()
        for h in range(4):
            hs = slice(h * 64, (h + 1) * 64)
            op('pe', MM(bo[:, hs], dT[:, 12 + h, :], Sdb[:, hs], True, False), r=['dT1', 'Sdb'], w=[bon])
            op('pe', MM(bo[:, hs], attnT[:, h * 128:(h + 1) * 128], vnew[:, hs], False, True), r=['attnT', 'vnew'], w=[bon])
        if upto < 3.9:
            return early(l, n, dst, b)
        op('dve', TT(h64(kg[:]), h64(kn[:]), bc64(egl), mult), r=['kn'] + DS, w=['kg'])
        bk, bn = bank()
        for h in range(4):
            hs = slice(h * 64, (h + 1) * 64)
            op('pe', MM(bk[0:64, hs], kg[:, hs], vnew[:, hs]), r=['kg', 'vnew'], w=[bn])
        for h in range(4):
            hs = slice(h * 64, (h + 1) * 64)
            op('dve', STT(Sd32[:, hs], Sd32[:, hs], egll[0:64, h:h + 1], bk[0:64, hs], mult, add), r=['Sd32', bn] + DS, w=['Sd32'])
        if upto < 3.95:
            return early(l, n, dst, b)
        op('dve', CP(Sdb[:], Sd32[:]), r=['Sd32'], w=['Sdb'])
        if l == 0 and n == 0:
            dump('vnew', vnew[:], ['vnew'])
            op('dve', CP(Gb[:, 0:256], bo[:, 0:256]), r=[bon], w=['G'])
            dump('odn', Gb[:, 0:256], ['G'])
        branch_out(bo[:, 0:256], bon, 1)
        if l == 0 and n == 0:
            dump('ssqdn', sm[:, 12:16], ['sm_b1']); dump('ngg', ngg[:], ['ngg'])
        if upto < 4:
            return early(l, n, dst, b)
        cu = COL['su'][0]
        op('act', ACT(ub[:], p[:, cu:cu + 256], AF.Copy), r=['p3'], w=['ub'])
        bk, bn = bank()
        bkb = bk.bitcast(BF16)
        for c in range(2):
            op('pe', TR(bkb[:, c * 128:(c + 1) * 128], ub[:, c * 128:(c + 1) * 128], identb), r=['ub', 'cb'], w=[bn])
        op('dve', CP(uT[:].rearrange("p a b -> p (a b)"), bkb[:, 0:256]), r=[bn], w=['uT'])
        xr, xrn = dbank()
        xi, xin = dbank()
        for s_ in range(8):
            op('pe', MM(xr[:, s_ * 128:(s_ + 1) * 128], BT[:, 0, s_, :], uT[:, s_ // 4, :]), r=['BT', 'uT'], w=[xrn[s_ // 4]])
            op('pe', MM(xi[:, s_ * 128:(s_ + 1) * 128], BT[:, 1, s_, :], uT[:, s_ // 4, :]), r=['BT', 'uT'], w=[xin[s_ // 4]])
        Af = [a[:].rearrange("p a b -> p (a b)") for a in A]
        fl = lambda t_: t_[:].rearrange("p a b -> p (a b)")
        op('dve', TT(Af[0], xr[:], fl(Tc), mult), r=xrn + ['Tc'], w=['A0'])
        op('dve', TT(Af[1], xi[:], fl(Ts), mult), r=xin + ['Ts'], w=['A1'])
        op('dve', TT(Af[0], Af[0], Af[1], sub), r=['A0', 'A1'], w=['A0'])
        op('dve', TT(Af[1], xr[:], fl(Ts), mult), r=xrn + ['Ts'], w=['A1'])
        op('dve', TT(Af[2], xi[:], fl(Tc), mult), r=xin + ['Tc'], w=['A2'])
        op('dve', TT(Af[1], Af[1], Af[2], add), r=['A1', 'A2'], w=['A1'])
        op('dve', TT(A[0][:, :, 0], A[0][:, :, 0], mc[:, 0, :], add), r=['A0', 'mc'], w=['A0'])
        op('dve', TT(A[1][:, :, 0], A[1][:, :, 0], mc[:, 1, :], add), r=['A1', 'mc'], w=['A1'])
        op('dve', lambda e: e.tensor_tensor_scan(out=Af[2], data0=fl(MAGz), data1=Af[0], initial=0.0, op0=mult, op1=add), r=['MAGz', 'A0'], w=['A2'])
        op('dve', lambda e: e.tensor_tensor_scan(out=Af[3], data0=fl(MAGz), data1=Af[1], initial=0.0, op0=mult, op1=add), r=['MAGz', 'A1'], w=['A3'])
        T = lambda i: s5t[:, i, :]
        mec, mes, t0, t1 = T(15), T(16), T(17), T(18)
        S2 = ['s5t2']
        op('dve', TT(t0, A[2][:, :, 127], mec, mult), r=['A2', 's5t'], w=S2)
        op('dve', TT(t1, A[3][:, :, 127], mes, mult), r=['A3', 's5t'], w=S2)
        op('dve', TT(mc[:, 0, :], t0, t1, sub), r=S2, w=['mc'])
        op('dve', TT(t0, A[2][:, :, 127], mes, mult), r=['A2', 's5t'], w=S2)
        op('dve', TT(t1, A[3][:, :, 127], mec, mult), r=['A3', 's5t'], w=S2)
        op('dve', TT(mc[:, 1, :], t0, t1, add), r=S2, w=['mc'])
        op('pool', TT(Af[0], Af[2], fl(Oc), mult), r=['A2', 'Oc'], w=['A0'])
        op('pool', TT(Af[1], Af[3], fl(Os), mult), r=['A3', 'Os'], w=['A1'])
        op('pool', TT(fl(sre), Af[0], Af[1], sub), r=['A0', 'A1'], w=['Pm0', 'PTm0'])
        op('pool', TT(Af[0], Af[2], fl(Os), mult), r=['A2', 'Os'], w=['A0'])
        op('pool', TT(Af[1], Af[3], fl(Oc), mult), r=['A3', 'Oc'], w=['A1'])
        op('pool', TT(fl(sim), Af[0], Af[1], add), r=['A0', 'A1'], w=['Pm1', 'PTm1'])
        by, byn = bank()
        for s_ in range(8):
            cs_ = slice(s_ * 32, (s_ + 1) * 32)
            op('pe', MM(by[:, cs_], sre[:, s_, :], CT[:, 0, s_, :], True, False), r=['Pm0', 'PTm0', 'CT'], w=[byn])
            op('pe', MM(by[:, cs_], sim[:, s_, :], CT[:, 1, s_, :], False, False), r=['Pm1', 'PTm1', 'CT'], w=[byn])
            op('pe', MM(by[:, cs_], uT[:, s_ // 4, :], Dg[:, s_, :], False, True), r=['uT', 'Dg'], w=[byn])
        y_ = by[:, 0:256]
        op('act', ACT(t256[:], y_, AF.Square), r=[byn], w=['G'])
        op('dve', TS(t256[:], t256[:], 0.044715, 1.0, mult, add), r=['G'], w=['G'])
        op('dve', TT(t256[:], t256[:], y_, mult), r=['G', byn], w=['G'])
        op('act', ACT(t256[:], t256[:], AF.Sigmoid, scale=2.0 * 0.7978845608028654), r=['G'], w=['G'])
        op('dve', TT(yg[:], t256[:], y_, mult), r=['G', byn], w=['G'])
        op('act', ACT(ygb[:], yg[:], AF.Copy), r=['G'], w=['ygb'])
        bk, bn = bank()
        bkb = bk.bitcast(BF16)
        for c in range(2):
            op('pe', TR(bkb[:, c * 128:(c + 1) * 128], ygb[:, c * 128:(c + 1) * 128], identb), r=['ygb', 'cb'], w=[bn])
        op('dve', CP(ygT[:].rearrange("p a b -> p (a b)"), bkb[:, 0:256]), r=[bn], w=['ygT'])
        bgl, bgln = bank()
        op('pe', MM(bgl[:, 0:256], ygT[:, 0, :], Wglu[:, 0, :], True, False), r=['ygT', 'Wglu'], w=[bgln])
        op('pe', MM(bgl[:, 0:256], ygT[:, 1, :], Wglu[:, 1, :], False, False), r=['ygT', 'Wglu'], w=[bgln])
        op('pe', MM(bgl[:, 0:256], onesb[:], bglu[:], False, True), r=['onesb', 'bglu'], w=[bgln])
        op('act', ACT(u256[:], bgl[:, 0:256], AF.Sigmoid), r=[bgln], w=['G'])
        op('dve', TT(u256[:], u256[:], yg[:], mult), r=['G', 'G'], w=['G'])
        op('dve', TT(yb[:, 512:768], u256[:], ngg[:, 512:768], mult), r=['G', 'ngg', 'hT'], w=['yb2'])
        if upto < 5:
            return early(l, n, dst, b)
        cgc, cgq, cgk = COL['gcode'][0], COL['gq'][0], COL['gk'][0]
        op('act', ACT(gcb[:], p[:, cgc:cgc + 16], AF.Copy), r=['p4'], w=['gcb'])
        bk, bn = bank()
        bkb = bk.bitcast(BF16)
        op('pe', TR(bkb[0:16, 0:128], gcb[:], identb), r=['gcb', 'cb'], w=[bn])
        op('dve', CP(gcT[:], bkb[0:16, 0:128]), r=[bn], w=['gcT'])
        bz, bzn = bank()
        op('pe', MM(bz[:, 0:128], gcT[:], wgk[:], True, False), r=['gcT', 'wgk'], w=[bzn])
        op('pe', MM(bz[:, 0:128], onesb[:], bgk[:], False, True), r=['onesb', 'bgk'], w=[bzn])
        gkk, cum, clb, ec, enc, ecl = [g[:] for g in g128]
        op('act', ACT(gkk, bz[:, 0:128], AF.Exp, scale=-1.0), r=[bzn], w=['G'])
        op('act', ACT(gkk, gkk, AF.Ln, bias=1.0), r=['G'], w=['G'])
        op('dve', TS(gkk, gkk, -1.0 / 16, None, mult), r=['G'], w=['G'])
        bc_, bcn_ = bank()
        op('pe', MM(bc_[:, 0:128], triU, gkk), r=['cf', 'G'], w=[bcn_])
        op('pe', MM(bc_[:, 128:256], ones, gkk), r=['cf', 'G'], w=[bcn_])
        for h in range(4):
            op('pe', MM(bc_[0:32, 256 + h:257 + h], g128[0][:, h * 32:(h + 1) * 32], ones[:, 0:1]), r=['cf', 'G'], w=[bcn_])
        op('dve', CP(cum, bc_[:, 0:128]), r=[bcn_], w=['G'])
        op('dve', TT(clb, bc_[:, 128:256], cum, sub), r=[bcn_, 'G'], w=['G'])
        op('act', ACT(ec, cum, AF.Exp), r=['G'], w=['G'])
        op('act', ACT(enc, cum, AF.Exp, scale=-1.0), r=['G'], w=['G'])
        op('act', ACT(ecl, clb, AF.Exp), r=['G'], w=['G'])
        ecl32 = sm[0:32, 64:68]
        op('act', ACT(ecl32, bc_[0:32, 256:260], AF.Exp), r=[bcn_], w=['ecl32'])
        op('dve', STT(gq3[:, 0:128], p[:, cgq:cgq + 128], 32.0 ** -0.5, ec, mult, mult), r=['p4', 'G'], w=['gq3'])
        op('dve', TT(gq3[:, 128:256], p[:, cgk:cgk + 128], enc, mult), r=['p4', 'G'], w=['gq3'])
        op('dve', TT(gq3[:, 256:384], p[:, cgk:cgk + 128], ecl, mult), r=['p4', 'G'], w=['gq3'])
        bk, bn = bank()
        bkb = bk.bitcast(BF16)
        for j in range(8):
            op('pe', TR(bkb[0:32, j * 128:(j + 1) * 128], gq3[:, j * 32:(j + 1) * 32], identb), r=['gq3', 'cb'], w=[bn])
        op('dve', CP(gT[:].rearrange("p a b -> p (a b)"), bkb[0:32, :]), r=[bn], w=['dT0'])
        bk, bn = bank()
        for h in range(4):
            op('pe', MM(bk[:, h * 128:(h + 1) * 128], gT[:, 4 + h, :], gT[:, h, :]), r=['dT0'], w=[bn])
        op('dve', TT(gsT[:], bk, Ui4, mult), r=[bn, 'cb'], w=['sT'])
        bo, bon = bank()
        for h in range(4):
            hs = slice(h * 64, (h + 1) * 64)
            gv_ = vb[:, 256 + h * 64:256 + (h + 1) * 64]
            op('pe', MM(bo[:, hs], gsT[:, h * 128:(h + 1) * 128], gv_, True, False), r=['sT', 'vb'], w=[bon])
            op('pe', MM(bo[:, hs], gT[:, h, :], Gsb[:, hs], False, True), r=['dT0', 'Gsb'], w=[bon])
        bk, bn = bank()
        for h in range(4):
            hs = slice(h * 64, (h + 1) * 64)
            op('pe', MM(bk[0:32, hs], gq3[:, 256 + h * 32:256 + (h + 1) * 32], vb[:, 256 + h * 64:256 + (h + 1) * 64]), r=['gq3', 'vb'], w=[bn])
        for h in range(4):
            hs = slice(h * 64, (h + 1) * 64)
            op('dve', STT(Gs32[:, hs], Gs32[:, hs], ecl32[:, h:h + 1], bk[0:32, hs], mult, add), r=['Gs32', bn, 'ecl32'], w=['Gs32'])
        op('act', ACT(Gsb[:], Gs32[:], AF.Copy), r=['Gs32'], w=['Gsb'])
        branch_out(bo[:, 0:256], bon, 3)
        if upto < 6:
            return early(l, n, dst, b)
        bk, bn = bank()
        bkb = bk.bitcast(BF16)
        YB = ['yb0', 'yb1', 'yb2', 'yb3']
        if dbg and l == NL - 1:
            R.dma('sp', d_dbg[n * 128:(n + 1) * 128, :], yb[:], key='dbg', r=YB, w=['dbgout%d' % n])
        for c in range(8):
            op('pe', TR(bkb[:, c * 128:(c + 1) * 128], yb[:, c * 128:(c + 1) * 128], identb), r=YB + ['cb'], w=[bn])
        op('dve', CP(yT[:].rearrange("p a b -> p (a b)"), bkb), r=[bn], w=['hT'])
        po, pon = dbank()
        for half in range(2):
            for c in range(8):
                op('pe', MM(po[:, half * 512:(half + 1) * 512], yT[:, c, :], Wo[:, c, half * 512:(half + 1) * 512], c == 0, c == 7), r=['hT', 'Wo'], w=[pon[half]])
        op('act', ACT(A[3][:].rearrange('p a b -> p (a b)'), po[:], AF.Square, accum_out=sm[:, 1:2]), r=pon, w=['A3', 'sm1'])
        rsqrt_small(sm[:, 1:2], ['sm1'], 1.0 / D, 1e-6)
        op('dve', STT(Af[0], po[:], sm[:, 1:2], Gpost[:], mult, mult), r=pon + ['sm1', 'Gpost'], w=['A0'])
        op('pool', TT(Af[0], Af[0], xt[b][:], add), r=['A0', X], w=['A0'])
        R.dma('sp', dst[n * 128:(n + 1) * 128, :], Af[0], key='xo', r=['A0'], w=['dst%d_%d' % (l, n)])

    for l in range(NL):
        if do_setup:
            setup(l)
        for n in range(NT):
            tile(l, n, xs_dram[l], xs_dram[l + 1])
    print('SBUF bytes/partition', R.sb_bytes)
    R.finish(final_reads=['dst%d_%d' % (NL - 1, n) for n in range(max(0, NT - 2), NT)] + (['dbgout%d' % n for n in range(NT)] + dumps if dbg else []))
    return nc


def _bf(a):
    return np.ascontiguousarray(a).astype(ml_dtypes.bfloat16)


def make_consts(NT):
    idn = np.eye(128, dtype=np.float32)
    j = np.arange(128)[:, None]
    t = np.arange(128)[None, :]
    Sh = [(j == t - s).astype(np.float32) for s in range(4)]
    ShP = [(j == 128 + t - s).astype(np.float32) for s in range(1, 4)]
    Ui = (j <= t).astype(np.float32)
    negLs = -(t < j).astype(np.float32)
    negUs = -(t > j).astype(np.float32)
    hm = []
    ii_, jj_ = np.arange(128)[:, None], np.arange(128)[None, :]
    for lev in range(7):
        b_ = 1 << lev
        hm.append(((ii_ // (2 * b_) == jj_ // (2 * b_)) & (ii_ % (2 * b_) >= b_) & (jj_ % (2 * b_) < b_)).astype(np.float32))
    hm = hm + [m_.T.copy() for m_ in hm]
    cb = np.concatenate([idn] + Sh + ShP + [np.tile(Ui, (1, 4)), np.tile(negLs, (1, 4)), np.tile(negUs, (1, 4))] + hm, axis=1)
    Esel = np.zeros((128, 512), np.float32)
    for h in range(4):
        Esel[h, h * 128:(h + 1) * 128] = 1.0
    GCt = np.zeros((128, 256), np.float32)
    for h in range(4):
        GCt[:, h * 64:(h + 1) * 64] = np.float32(GAMMA[h]) ** 128
    cf = np.concatenate([idn, Ui, np.ones((128, 128), np.float32), Esel, GCt], axis=1).astype(np.float32)
    pos = np.arange(NT * 128, dtype=np.float64)
    inv = 10000.0 ** (-np.arange(0, 64, 2, dtype=np.float64) / 64)
    ang = pos[:, None] * inv[None, :]
    cos, sin = np.cos(ang), np.sin(ang)
    ii = (np.arange(NT * 128) % 128).astype(np.float64)
    Cq = np.zeros((NT * 128, 4, 32, 2)); Sq = np.zeros_like(Cq); Ck = np.zeros_like(Cq); Sk = np.zeros_like(Cq)
    for h in range(4):
        dq = (GAMMA[h] ** (ii + 1.0)) * (64 ** -0.5)
        dk = GAMMA[h] ** (-(ii + 1.0))
        for (Ct, St, dd) in ((Cq, Sq, dq), (Ck, Sk, dk)):
            Ct[:, h, :, 0] = cos * dd[:, None]
            Ct[:, h, :, 1] = cos * dd[:, None]
            St[:, h, :, 0] = -sin * dd[:, None]
            St[:, h, :, 1] = sin * dd[:, None]
    ropec = np.concatenate([Cq.reshape(-1, 256), Ck.reshape(-1, 256)], axis=1).reshape(NT, 128, 512).astype(np.float32)
    ropes = np.concatenate([Sq.reshape(-1, 256), Sk.reshape(-1, 256)], axis=1).reshape(NT, 128, 512).astype(np.float32)
    return dict(cb=_bf(cb), cf=cf, ropec=ropec, ropes=ropes)


def make_params(inp, layers):
    f = lambda k: np.asarray(inp[k], dtype=np.float32)
    L = list(layers)
    NL = len(L)
    w_in = f('w_in')[L][:, :, PERM]
    w_out = f('w_out')[L]
    gpre = f('norm_pre')[L].reshape(NL, 8, 128).transpose(0, 2, 1)
    gpost = f('norm_post')[L].reshape(NL, 1, D)
    ng = np.concatenate([np.tile(f('ret_norm')[L], (1, 4)), np.tile(f('dn_norm')[L], (1, 4)),
                         np.ones((NL, 256), np.float32), np.tile(f('gla_norm')[L], (1, 4))], axis=1).reshape(NL, 8, 128).transpose(0, 2, 1)
    cw = f('dn_conv')[L].reshape(NL, 1, 4 * 768)
    dnp = np.concatenate([f('dn_a_log')[L], f('dn_dt_bias')[L]], axis=1).reshape(NL, 1, 8)

    def st(a):
        return a.reshape(NL, 8, 2, 64).transpose(0, 2, 3, 1).reshape(NL, 128, 8)
    ldt = np.repeat(f('s5_log_dt')[L][:, :, None], 64, axis=2)
    s5p = np.concatenate([st(f('s5_lam_re')[L]), st(f('s5_lam_im')[L]), st(ldt)], axis=2)
    bt = np.zeros((NL, 2, 128, 8, 128), np.float32)
    ct = np.zeros((NL, 2, 128, 8, 32), np.float32)
    dg = np.zeros((NL, 128, 8, 32), np.float32)
    bre, bim, cre, cim, dd = f('s5_b_re')[L], f('s5_b_im')[L], f('s5_c_re')[L], f('s5_c_im')[L], f('s5_d')[L]
    for g in range(16):
        s_, gg = g // 2, g % 2
        r0 = 32 * (s_ % 4) + gg * 16
        for k_, (bb, cc) in enumerate(((bre, cre), (bim, cim))):
            bt[:, k_, r0:r0 + 16, s_, gg * 64:(gg + 1) * 64] = bb[:, g].transpose(0, 2, 1)
            ct[:, k_, gg * 64:(gg + 1) * 64, s_, gg * 16:(gg + 1) * 16] = cc[:, g].transpose(0, 2, 1)
        for h in range(16):
            dg[:, r0 + h, s_, gg * 16 + h] = dd[:, g, h]
    bt = bt.transpose(0, 2, 1, 3, 4).reshape(NL, 128, 2048)
    ct = ct.transpose(0, 2, 1, 3, 4).reshape(NL, 128, 512)
    dg = dg.reshape(NL, 128, 256)
    wglu = f('s5_w_glu')[L].reshape(NL, 2, 128, 256).transpose(0, 2, 1, 3).reshape(NL, 128, 512)
    bglu = f('s5_b_glu')[L].reshape(NL, 1, 256)
    wgk = f('gla_w_gk')[L]
    bgk = f('gla_b_gk')[L].reshape(NL, 1, 128)
    c = np.ascontiguousarray
    return dict(w_in=c(w_in), w_out=c(w_out), gpre=c(gpre), gpost=c(gpost), ng=c(ng), cw=c(cw), dnp=c(dnp), s5p=c(s5p),
                s5bt=c(bt), s5ct=c(ct), s5dg=c(dg), wglu=c(wglu), bglu=c(bglu), wgk=c(wgk), bgk=c(bgk))


_PROG = {}


def kernel(**inputs):
    x = np.asarray(inputs['x'], dtype=np.float32)
    B, L, _ = x.shape
    NT = L // 128
    NLAY = np.asarray(inputs['w_in']).shape[0]
    key = (NT, NLAY)
    if key not in _PROG:
        _PROG[key] = (build_program(NT, NLAY), make_consts(NT))
    nc, consts = _PROG[key]
    prm = make_params(inputs, range(NLAY))
    in_maps = []
    for b in range(B):
        m = dict(consts)
        m.update(prm)
        m['x'] = np.ascontiguousarray(x[b])
        in_maps.append(m)
    res = run_bass_kernel_spmd(nc, in_maps, core_ids=list(range(B)))
    return np.stack([np.asarray(res.results[b]['out'], dtype=np.float32) for b in range(B)], axis=0)
```

```python
import os
import numpy as np
import ml_dtypes
import concourse.bass as bass
import concourse.mybir as mybir
from concourse.bass_utils import run_bass_kernel_spmd

F32 = mybir.dt.float32
BF16 = mybir.dt.bfloat16
AF = mybir.ActivationFunctionType
ALU = mybir.AluOpType
AX = mybir.AxisListType


class Rec:
    def __init__(self, nc):
        self.nc = nc
        self.ops = {k: [] for k in ('pe', 'act', 'dve', 'pool', 'sp')}
        self.cnt = {k: 0 for k in self.ops}
        self.clock = {k: {} for k in self.ops}
        self.snap = {}
        self.last_w = {}
        self.readers = {}
        self.sems = {}
        self.dma_cnt = {}

    def sem(self, key):
        if key not in self.sems:
            self.sems[key] = self.nc.alloc_semaphore(name="s_" + key.replace(':', '_'))
        return self.sems[key]

    def sb(self, name, shape, dt):
        n = 1
        for d_ in shape[1:]:
            n *= d_
        self.sb_bytes = getattr(self, 'sb_bytes', 0) + n * (2 if dt == BF16 else 4)
        return self.nc.alloc_sbuf_tensor("s_" + name, list(shape), dt)

    def ps(self, name, shape, dt=F32):
        return self.nc.alloc_psum_tensor("q_" + name, list(shape), dt)

    def _deps(self, eng, r, w):
        deps = {}

        def add(kn):
            if kn is None:
                return
            k, n = kn
            if deps.get(k, 0) < n:
                deps[k] = n
        for b in r:
            add(self.last_w.get(b))
        for b in w:
            add(self.last_w.get(b))
            for kn in self.readers.get(b, ()):
                add(kn)
        ck = self.clock[eng]
        waits = []
        if eng == 'pe':
            deps.pop('pe', None)
        for k, n in deps.items():
            if ck.get(k, 0) < n:
                waits.append((k, n))
        for k, n in waits:
            sn = self.snap.get((k, n))
            if sn:
                for k2, n2 in sn.items():
                    if ck.get(k2, 0) < n2:
                        ck[k2] = n2
            if ck.get(k, 0) < n:
                ck[k] = n
        return waits

    def _mark(self, me, r, w):
        for b in r:
            self.readers.setdefault(b, []).append(me)
        for b in w:
            self.last_w[b] = me
            self.readers[b] = []

    def op(self, eng, fn, r=(), w=()):
        w = list(w) + [b_ for b_ in r if b_.startswith('PS') and b_ not in w]
        waits = self._deps(eng, r, w)
        self.cnt[eng] += 1
        me = (eng, self.cnt[eng])
        self.snap[me] = dict(self.clock[eng])
        self.ops[eng].append((waits, fn, eng, 1))
        self._mark(me, r, w)

    def dma(self, q, out, in_, key, r=(), w=()):
        waits = self._deps(q, r, w)
        dk = 'dma:' + key
        self.dma_cnt[dk] = self.dma_cnt.get(dk, 0) + 1
        me = (dk, self.dma_cnt[dk])
        self.snap[me] = dict(self.clock[q])
        self.ops[q].append((waits, lambda e: e.dma_start(out=out, in_=in_), dk, 16))
        self._mark(me, r, w)

    def finish(self, final_reads=()):
        waits = self._deps('sp', list(final_reads), [])
        nc = self.nc
        semval = lambda k, n: (self.sem(k), n * 16 if k.startswith('dma:') else n)
        for k in self.ops:
            self.sem(k)
        final = [semval(k, n) for k, n in waits]
        for k in ('pe', 'act', 'dve', 'pool'):
            if self.cnt[k] > 0:
                final.append((self.sem(k), self.cnt[k]))
        for dk, n in self.dma_cnt.items():
            final.append((self.sem(dk), 16 * n))
        emit_lists = {}
        for eng, lst in self.ops.items():
            el = []
            for waits_, fn, inck, incv in lst:
                el.append(([semval(k, n) for k, n in waits_], fn, self.sem(inck), incv))
            emit_lists[eng] = el
        with nc.Block() as block:
            def run(e, el, extra=()):
                for ws, fn, s, v in el:
                    for sm, val in ws:
                        e.wait_ge(sm, val)
                    fn(e).then_inc(s, v)
                for sm, val in extra:
                    e.wait_ge(sm, val)

            @block.sync
            def _(e):
                run(e, emit_lists['sp'], final)

            @block.tensor
            def _(e):
                run(e, emit_lists['pe'])

            @block.scalar
            def _(e):
                run(e, emit_lists['act'])

            @block.vector
            def _(e):
                run(e, emit_lists['dve'])

            @block.gpsimd
            def _(e):
                run(e, emit_lists['pool'])


D = 1024
NCOL = 3352
ORIG = dict(rq=(0, 256), rk=(256, 512), rv=(512, 768), rg=(768, 1024), dq=(1024, 1280), dk=(1280, 1536),
            dv=(1536, 1792), dbeta=(1792, 1796), da=(1796, 1800), dg=(1800, 2056), su=(2056, 2312),
            sg=(2312, 2568), gq=(2568, 2696), gk=(2696, 2824), gv=(2824, 3080), gcode=(3080, 3096),
            gg=(3096, 3352))
ORDER = ['rq', 'rk', 'rv', 'gv', 'dq', 'dk', 'dv', 'su', 'gq', 'gk', 'gcode', 'dbeta', 'da', 'rg', 'dg', 'sg', 'gg']
COL = {}
_o = 0
for _k in ORDER:
    _w = ORIG[_k][1] - ORIG[_k][0]
    COL[_k] = (_o, _o + _w)
    _o += _w
PERM = np.concatenate([np.arange(*ORIG[k]) for k in ORDER])
BANKS = [(0, 512), (512, 1024), (1024, 1536), (1536, 2048), (2048, 2328), (2328, 2840), (2840, 3352)]
C = 128
GAMMA = [1.0 - 2.0 ** (-5.0 - h) for h in range(4)]


def build_program(NT, NL, dbg=False, upto=99, do_setup=True):
    nc = bass.Bass("TRN2", target_bir_lowering=False)
    R = Rec(nc)
    din = lambda name, shape, dt=F32: nc.dram_tensor(name, list(shape), dt, kind="ExternalInput").ap()
    x_in = din("x", [NT * 128, D])
    x_out = nc.dram_tensor("out", [NT * 128, D], F32, kind="ExternalOutput").ap()
    xs_dram = [x_in]
    for l in range(NL - 1):
        xs_dram.append(nc.dram_tensor("xmid%d" % l, [NT * 128, D], F32).ap())
    xs_dram.append(x_out)
    d_dbg = nc.dram_tensor("dbg", [NT * 128, D], BF16, kind="ExternalOutput").ap() if dbg else None
    d_win = din("w_in", [NL, D, NCOL])
    d_wout = din("w_out", [NL, D, D])
    d_gpre = din("gpre", [NL, 128, 8])
    d_gpost = din("gpost", [NL, 1, D])
    d_ng = din("ng", [NL, 128, 8])
    d_cw = din("cw", [NL, 1, 4 * 768])
    d_dnp = din("dnp", [NL, 1, 8])
    d_s5p = din("s5p", [NL, 128, 24])
    d_bt = din("s5bt", [NL, 128, 2 * 8 * 128])
    d_ct = din("s5ct", [NL, 128, 2 * 8 * 32])
    d_dg = din("s5dg", [NL, 128, 8 * 32])
    d_wglu = din("wglu", [NL, 128, 2 * 256])
    d_bglu = din("bglu", [NL, 1, 256])
    d_wgk = din("wgk", [NL, 16, 128])
    d_bgk = din("bgk", [NL, 1, 128])
    d_ropec = din("ropec", [NT, 128, 512])
    d_ropes = din("ropes", [NT, 128, 512])
    d_cb = din("cb", [128, 128 * 8 + 512 * 3 + 14 * 128], BF16)
    d_cf = din("cf", [128, 128 * 3 + 512 + 256])

    sb, ps = R.sb, R.ps
    cb = sb("cb", [128, 128 * 8 + 1536 + 14 * 128], BF16)
    cf = sb("cf", [128, 128 * 3 + 768], F32)
    identb = cb[:, 0:128]
    Sh = [cb[:, 128 * (1 + s):128 * (2 + s)] for s in range(4)]
    ShP = [None] + [cb[:, 128 * (4 + s):128 * (5 + s)] for s in range(1, 4)]
    Ui4 = cb[:, 1024:1536]
    negLs4 = cb[:, 1536:2048]
    negUs4 = cb[:, 2048:2560]
    HM = [cb[:, 2560 + i * 128:2560 + (i + 1) * 128] for i in range(14)]
    identf = cf[:, 0:128]
    triU = cf[:, 128:256]
    ones = cf[:, 256:384]
    Esel = cf[0:4, 384:896]
    GC = cf[0:64, 896:1152]
    R.dma('sp', cb[:], d_cb[:, :], key='cb', w=['cb'])
    R.dma('sp', cf[:], d_cf[:, :], key='cf', w=['cf'])
    onesb = sb("onesb", [1, 128], BF16)
    R.op('dve', lambda e: e.tensor_copy(out=onesb[:], in_=cf[0:1, 256:384]), r=['cf'], w=['onesb'])

    Wb = sb("Wb", [128, 8, NCOL], BF16)
    Wo = sb("Wo", [128, 8, D], BF16)
    stage = sb("stage", [128, NCOL], F32)
    p = stage
    gpre = sb("gpre", [128, 8], F32)
    Gpost = sb("Gpost", [128, D], F32)
    NG = sb("NG", [128, 8], F32)
    cw = sb("cw", [128, 4, 768], BF16)
    dnp = sb("dnp", [128, 8], F32)
    s5p = sb("s5p", [128, 24], F32)
    BT = sb("BT", [128, 2, 8, 128], BF16)
    CT = sb("CT", [128, 2, 8, 32], BF16)
    Dg = sb("Dg", [128, 8, 32], BF16)
    Wglu = sb("Wglu", [128, 2, 256], BF16)
    bgluf = sb("bgluf", [1, 256], F32)
    bglu = sb("bglu", [1, 256], BF16)
    wgkf = sb("wgkf", [16, 128], F32)
    wgk = sb("wgk", [16, 128], BF16)
    bgkf = sb("bgkf", [1, 128], F32)
    bgk = sb("bgk", [1, 128], BF16)
    Tc = sb("Tc", [128, 8, 128], F32)
    Ts = sb("Ts", [128, 8, 128], F32)
    Oc = sb("Oc", [128, 8, 128], F32)
    Os = sb("Os", [128, 8, 128], F32)
    MAGz = sb("MAGz", [128, 8, 128], F32)
    s5t = sb("s5t", [128, 40, 8], F32)
    Rr32 = sb("Rr32", [64, 256], F32)
    Rrb = sb("Rrb", [64, 256], BF16)
    Sd32 = sb("Sd32", [64, 256], F32)
    Sdb = sb("Sdb", [64, 256], BF16)
    Gs32 = sb("Gs32", [32, 256], F32)
    Gsb = sb("Gsb", [32, 256], BF16)
    mc = sb("mc", [128, 2, 8], F32)
    xt1 = sb("xt", [128, D], F32)
    xt = [xt1, xt1]
    ropeC1 = sb("ropeC", [128, 512], F32)
    ropeS1 = sb("ropeS", [128, 512], F32)
    ropeC = [ropeC1, ropeC1]
    ropeS = [ropeS1, ropeS1]
    sm = sb("sm", [128, 72], F32)
    xs = sb("xs", [128, D], BF16)
    yb = xs
    hT = sb("hT", [128, 8, 128], BF16)
    ngg = sb("ngg", [128, D], BF16)
    qk = sb("qk", [128, 512], BF16)
    vb = sb("vb", [128, 512], BF16)
    sT = sb("sT", [128, 512], BF16)
    tmpR = sb("tmpR", [64, 256], F32)
    pk = [sb("pk%d" % i, [128, 4, 768], BF16) for i in range(2)]
    qn = sb("qn", [128, 256], BF16)
    kn = sb("kn", [128, 256], BF16)
    kbq = sb("kbq", [128, 512], BF16)
    dT = sb("dT", [64, 16, 128], BF16)
    qkT = dT[:, 0:8, :]
    gct = sb("gct", [4, 128], F32)
    PP = sb("PP", [128, 4, 512], BF16)
    Pm = [PP[:, 0, :], PP[:, 2, :]]
    PTm = [PP[:, 1, :], PP[:, 3, :]]
    attnT = sb("attnT", [128, 512], BF16)
    Xb = sb("Xb", [128, 4, 128], BF16)
    WT = sb("WT", [64, 4, 128], BF16)
    vnew = sb("vnew", [128, 256], BF16)
    kg = sb("kg", [128, 256], BF16)
    ub = sb("ub", [128, 256], BF16)
    uT = sb("uT", [128, 2, 128], BF16)
    A = [sb("A%d" % i, [128, 8, 128], F32) for i in range(4)]
    _fl = lambda a_: a_.rearrange("p a b -> p (a b)")
    tmpL, tmpU = _fl(A[0][:, 0:4, :]), _fl(A[0][:, 4:8, :])
    DLs, DUs = tmpL, tmpU
    DUi, X32 = _fl(A[1][:, 0:4, :]), A[1][:, 4:8, :]
    cs = _fl(A[2][:])[:, 0:768]
    sq = _fl(A[3][:])[:, 0:512]
    xo = [_fl(A[0][:]), _fl(A[0][:])]
    yT, gT, gsT = hT, qkT[0:32], sT
    sre = PP[:, 0:2, :].rearrange("p a (b c) -> p (a b) c", c=128)
    sim = PP[:, 2:4, :].rearrange("p a (b c) -> p (a b) c", c=128)
    ygb = sb("ygb", [128, 256], BF16)
    ygT = sb("ygT", [128, 2, 128], BF16)
    gcb = sb("gcb", [128, 16], BF16)
    gcT = sb("gcT", [16, 128], BF16)
    Gb = sb("Gb", [128, 768], F32)
    g128 = [Gb[:, i * 128:(i + 1) * 128] for i in range(6)]
    t256, u256, yg = Gb[:, 0:256], Gb[:, 256:512], Gb[:, 512:768]
    gq3 = sb("gq3", [128, 384], BF16)
    PS = [ps("PS%d" % i, [128, 1024], F32) for i in range(4)]
    bank_ctr = [0]

    def bank():
        i = bank_ctr[0] % 8
        bank_ctr[0] += 1
        return PS[i // 2][:, (i % 2) * 512:(i % 2) * 512 + 512], 'PS%d_%d' % (i // 2, i % 2)

    def dbank():
        if bank_ctr[0] % 2:
            bank_ctr[0] += 1
        i = bank_ctr[0] % 8
        bank_ctr[0] += 2
        return PS[i // 2], ['PS%d_0' % (i // 2), 'PS%d_1' % (i // 2)]

    op = R.op
    dumps = []

    def dump(tag, ap, names):
        if not dbg:
            return
        d = nc.dram_tensor("dump_" + tag, list(ap.shape), ap.dtype, kind="ExternalOutput").ap()
        R.dma('sp', d, ap, key='dump_' + tag, r=names, w=['dumpout_' + tag])
        dumps.append('dumpout_' + tag)
    TT = lambda out, a, b, o: (lambda e: e.tensor_tensor(out=out, in0=a, in1=b, op=o))
    TS = lambda out, a, s1, s2, o0, o1=None: (lambda e: e.tensor_scalar(out=out, in0=a, scalar1=s1, scalar2=s2, op0=o0, **({} if o1 is None else {'op1': o1})))
    STT = lambda out, a, s, b, o0, o1: (lambda e: e.scalar_tensor_tensor(out=out, in0=a, scalar=s, in1=b, op0=o0, op1=o1))
    ACT = lambda out, a, f, **kw: (lambda e: e.activation(out=out, in_=a, func=f, **kw))
    CP = lambda out, a: (lambda e: e.tensor_copy(out=out, in_=a))
    MM = lambda out, l, r_, st=True, sp=True: (lambda e: e.matmul(out, lhsT=l, rhs=r_, start=st, stop=sp))
    TR = lambda out, a, idn: (lambda e: e.transpose(out=out, in_=a, identity=idn))
    mult, add, sub = ALU.mult, ALU.add, ALU.subtract

    def rsqrt_small(ap, names, scale, eps):
        op('act', ACT(ap, ap, AF.Sqrt, scale=scale, bias=eps), r=names, w=names)
        op('dve', lambda e: e.reciprocal(out=ap, in_=ap), r=names, w=names)

    def setup(l):
        R.dma('sp', gpre[:], d_gpre[l], key='gpre', w=['gpre'])
        R.dma('sp', NG[:], d_ng[l], key='ng', w=['NG'])
        for c in range(8):
            R.dma('sp', stage[:], d_win[l, c * 128:(c + 1) * 128, :], key='stage', w=['p%d' % i for i in range(7)])
            op('dve', TS(Wb[:, c, :], stage[:], gpre[:, c:c + 1], None, mult), r=['p%d' % i for i in range(7)] + ['gpre'], w=['Wb'])
        for c in range(8):
            R.dma('sp', stage[:, 0:D], d_wout[l, c * 128:(c + 1) * 128, :], key='stage', w=['p%d' % i for i in range(7)])
            op('act', ACT(Wo[:, c, :], stage[:, 0:D], AF.Copy, scale=NG[:, c:c + 1]), r=['p%d' % i for i in range(7)] + ['NG'], w=['Wo'])
        R.dma('sp', Gpost[:], d_gpost[l].partition_broadcast(128), key='gpost', w=['Gpost'])
        R.dma('sp', stage[:, 0:3072], d_cw[l].partition_broadcast(128), key='stage', w=['p%d' % i for i in range(7)])
        op('dve', CP(cw[:].rearrange("p a b -> p (a b)"), stage[:, 0:3072]), r=['p%d' % i for i in range(7)], w=['cw'])
        R.dma('sp', dnp[:], d_dnp[l].partition_broadcast(128), key='dnp', w=['dnp'])
        R.dma('sp', s5p[:], d_s5p[l], key='s5p', w=['s5p'])
        BTf, CTf, Dgf, Wgluf = stage[:, 0:2048], stage[:, 2048:2560], stage[:, 2560:2816], stage[:, 2816:3328]
        PN = ['p%d' % i for i in range(7)]
        R.dma('sp', BTf, d_bt[l], key='stage', w=PN)
        R.dma('sp', CTf, d_ct[l], key='stage2', w=PN)
        R.dma('sp', Dgf, d_dg[l], key='stage3', w=PN)
        R.dma('sp', Wgluf, d_wglu[l], key='stage4', w=PN)
        R.dma('sp', bgluf[:], d_bglu[l], key='bglu', w=['bgluf'])
        R.dma('sp', wgkf[:], d_wgk[l], key='wgk', w=['wgkf'])
        R.dma('sp', bgkf[:], d_bgk[l], key='bgk', w=['bgkf'])
        op('dve', CP(BT[:].rearrange("p a b c -> p (a b c)"), BTf), r=PN, w=['BT'])
        op('dve', CP(CT[:, 0].rearrange("p b c -> p (b c)"), CTf[:, 0:256]), r=PN, w=['CT'])
        op('dve', TS(CT[:, 1].rearrange("p b c -> p (b c)"), CTf[:, 256:512], -1.0, None, mult), r=PN, w=['CT'])
        op('dve', CP(Dg[:].rearrange("p b c -> p (b c)"), Dgf), r=PN, w=['Dg'])
        op('dve', CP(Wglu[:].rearrange("p b c -> p (b c)"), Wgluf), r=PN, w=['Wglu'])
        op('dve', CP(bglu[:], bgluf[:]), r=['bgluf'], w=['bglu'])
        op('dve', CP(wgk[:], wgkf[:]), r=['wgkf'], w=['wgk'])
        op('dve', CP(bgk[:], bgkf[:]), r=['bgkf'], w=['bgk'])
        op('act', ACT(dnp[:, 0:4], dnp[:, 0:4], AF.Exp), r=['dnp'], w=['dnp'])
        op('dve', TS(dnp[:, 0:4], dnp[:, 0:4], -1.0, None, mult), r=['dnp'], w=['dnp'])
        for nm, t_ in (('Rr32', Rr32), ('Sd32', Sd32), ('Gs32', Gs32), ('Rrb', Rrb), ('Sdb', Sdb), ('Gsb', Gsb), ('mc', mc)):
            ap_ = t_[:] if len(t_.shape) == 2 else t_[:].rearrange("p a b -> p (a b)")
            op('pool', lambda e, ap_=ap_: e.memset(ap_, 0.0), r=[], w=[nm])
        for i in range(2):
            op('pool', lambda e, i=i: e.memset(pk[i][:].rearrange("p a b -> p (a b)"), 0.0), r=[], w=['pk%d' % i])
        T = lambda i: s5t[:, i, :]
        S = ['s5t']
        lre, lim, ldt = s5p[:, 0:8], s5p[:, 8:16], s5p[:, 16:24]
        dt_, mag, th, xx, x2, sn, cs_, t0, t1 = T(0), T(1), T(2), T(3), T(4), T(5), T(6), T(7), T(8)
        op('act', ACT(dt_, ldt, AF.Exp), r=['s5p'], w=S)
        op('dve', TT(mag, lre, dt_, mult), r=['s5p'] + S, w=S)
        op('act', ACT(mag, mag, AF.Exp), r=S, w=S)
        op('dve', TT(th, lim, dt_, mult), r=['s5p'] + S, w=S)
        op('dve', TS(xx, th, 1.0 / 16, None, mult), r=S, w=S)
        op('dve', TT(x2, xx, xx, mult), r=S, w=S)
        sc = [1.0, -1.0 / 6, 1.0 / 120, -1.0 / 5040, 1.0 / 362880, -1.0 / 39916800, 1.0 / 6227020800]
        cc = [1.0, -1.0 / 2, 1.0 / 24, -1.0 / 720, 1.0 / 40320, -1.0 / 3628800, 1.0 / 479001600, -1.0 / 87178291200]
        op('dve', TS(sn, x2, sc[6], sc[5], mult, add), r=S, w=S)
        for k_ in (4, 3, 2, 1, 0):
            op('dve', TT(sn, sn, x2, mult), r=S, w=S)
            op('dve', TS(sn, sn, sc[k_], None, add), r=S, w=S)
        op('dve', TT(sn, sn, xx, mult), r=S, w=S)
        op('dve', TS(cs_, x2, cc[7], cc[6], mult, add), r=S, w=S)
        for k_ in (5, 4, 3, 2, 1, 0):
            op('dve', TT(cs_, cs_, x2, mult), r=S, w=S)
            op('dve', TS(cs_, cs_, cc[k_], None, add), r=S, w=S)
        for _ in range(4):
            op('dve', TT(t0, cs_, cs_, mult), r=S, w=S)
            op('dve', TT(t1, sn, sn, mult), r=S, w=S)
            op('dve', TT(sn, sn, cs_, mult), r=S, w=S)
            op('dve', TS(sn, sn, 2.0, None, mult), r=S, w=S)
            op('dve', TT(cs_, t0, t1, sub), r=S, w=S)
        are, aim, den, cre, cim = T(9), T(10), T(11), T(12), T(13)
        op('dve', TT(are, mag, cs_, mult), r=S, w=S)
        op('dve', TT(aim, mag, sn, mult), r=S, w=S)
        op('dve', TT(t0, lre, lre, mult), r=['s5p'] + S, w=S)
        op('dve', TT(t1, lim, lim, mult), r=['s5p'] + S, w=S)
        op('dve', TT(den, t0, t1, add), r=S, w=S)
        op('dve', lambda e: e.reciprocal(out=den, in_=den), r=S, w=S)
        nr = T(14)
        op('dve', TS(nr, are, -1.0, None, add), r=S, w=S)
        op('dve', TT(t0, nr, lre, mult), r=['s5p'] + S, w=S)
        op('dve', TT(t1, aim, lim, mult), r=['s5p'] + S, w=S)
        op('dve', TT(cre, t0, t1, add), r=S, w=S)
        op('dve', TT(cre, cre, den, mult), r=S, w=S)
        op('dve', TT(t0, aim, lre, mult), r=['s5p'] + S, w=S)
        op('dve', TT(t1, nr, lim, mult), r=['s5p'] + S, w=S)
        op('dve', TT(cim, t0, t1, sub), r=S, w=S)
        op('dve', TT(cim, cim, den, mult), r=S, w=S)
        op('pool', lambda e: e.memset(Oc[:, :, 0:1], 1.0), r=[], w=['Oc'])
        op('pool', lambda e: e.memset(Os[:, :, 0:1], 0.0), r=[], w=['Os'])
        op('dve', CP(Oc[:, :, 1], cs_), r=S, w=['Oc'])
        op('dve', CP(Os[:, :, 1], sn), r=S, w=['Os'])
        OO = ['Oc', 'Os']
        k_ = 1
        while k_ < 128:
            bc = lambda t_, k_=k_: t_[:, :, k_:k_ + 1].to_broadcast([128, 8, k_])
            hi = slice(k_ + 1, 2 * k_ + 1) if 2 * k_ + 1 <= 128 else slice(k_ + 1, 128)
            n_ = hi.stop - hi.start
            lo = slice(1, 1 + n_)
            bcn = lambda t_, k_=k_, n_=n_: t_[:, :, k_:k_ + 1].to_broadcast([128, 8, n_])
            a1, a2 = A[0][:, :, 0:n_], A[1][:, :, 0:n_]
            op('dve', TT(a1, Oc[:, :, lo], bcn(Oc), mult), r=OO, w=['A0'])
            op('dve', TT(a2, Os[:, :, lo], bcn(Os), mult), r=OO, w=['A1'])
            op('dve', TT(Oc[:, :, hi], a1, a2, sub), r=['A0', 'A1'] + OO, w=['Oc'])
            op('dve', TT(a1, Oc[:, :, lo], bcn(Os), mult), r=OO, w=['A0'])
            op('dve', TT(a2, Os[:, :, lo], bcn(Oc), mult), r=OO, w=['A1'])
            op('dve', TT(Os[:, :, hi], a1, a2, add), r=['A0', 'A1'] + OO, w=['Os'])
            k_ *= 2
        bc8 = lambda t_: t_.unsqueeze(2).to_broadcast([128, 8, 128])
        op('dve', TT(A[0][:], Oc[:], bc8(cre), mult), r=OO + S, w=['A0'])
        op('dve', TT(A[1][:], Os[:], bc8(cim), mult), r=OO + S, w=['A1'])
        op('dve', TT(Tc[:], A[0][:], A[1][:], add), r=['A0', 'A1'], w=['Tc'])
        op('dve', TT(A[0][:], Oc[:], bc8(cim), mult), r=OO + S, w=['A0'])
        op('dve', TT(A[1][:], Os[:], bc8(cre), mult), r=OO + S, w=['A1'])
        op('dve', TT(Ts[:], A[0][:], A[1][:], sub), r=['A0', 'A1'], w=['Ts'])
        op('dve', CP(MAGz[:], bc8(mag)), r=S, w=['MAGz'])
        op('pool', lambda e: e.memset(MAGz[:, :, 0:1], 0.0), r=[], w=['MAGz'])
        mec, mes = T(15), T(16)
        op('dve', TT(t0, Oc[:, :, 127], cs_, mult), r=OO + S, w=S)
        op('dve', TT(t1, Os[:, :, 127], sn, mult), r=OO + S, w=S)
        op('dve', TT(mec, t0, t1, sub), r=S, w=S)
        op('dve', TT(t0, Oc[:, :, 127], sn, mult), r=OO + S, w=S)
        op('dve', TT(t1, Os[:, :, 127], cs_, mult), r=OO + S, w=S)
        op('dve', TT(mes, t0, t1, add), r=S, w=S)
        op('dve', TT(mec, mec, mag, mult), r=S, w=S)
        op('dve', TT(mes, mes, mag, mult), r=S, w=S)

    def branch_out(obank, obn, br):
        op('act', ACT(sq[:, 0:256], obank, AF.Square), r=[obn], w=['A3'])
        ssq = sm[:, 8 + 4 * br: 12 + 4 * br]
        nm = ['sm_b%d' % br]
        op('dve', lambda e: e.tensor_reduce(out=ssq, in_=sq[:, 0:256].rearrange("p (h d) -> p h d", h=4), axis=AX.X, op=add), r=['A3'], w=nm)
        rsqrt_small(ssq, nm, 1.0 / 64, 1e-6)
        hv = lambda a_: a_.rearrange("p (h d) -> p h d", h=4)
        op('dve', TT(hv(sq[:, 0:256]), hv(obank), ssq.unsqueeze(2).to_broadcast([128, 4, 64]), mult), r=[obn, 'A3'] + nm, w=['A3'])
        op('dve', TT(yb[:, br * 256:(br + 1) * 256], sq[:, 0:256], ngg[:, br * 256:(br + 1) * 256], mult),
           r=['A3', 'ngg', 'hT'], w=['yb%d' % br])

    def early(l, n, dst, b):
        R.dma('sp', dst[n * 128:(n + 1) * 128, :], xt[b][:], key='xo', r=['xt'], w=['dst%d_%d' % (l, n)])

    def tile(l, n, src, dst):
        b = n % 2
        R.dma('sp', xt[b][:], src[n * 128:(n + 1) * 128, :], key='xt', r=(['dst%d_%d' % (l - 1, n)] if l > 0 else []), w=['xt'])
        R.dma('sp', ropeC[b][:], d_ropec[n], key='rc', w=['rc'])
        R.dma('sp', ropeS[b][:], d_ropes[n], key='rs', w=['rs'])
        X = 'xt'
        op('act', ACT(A[3][:].rearrange('p a b -> p (a b)'), xt[b][:], AF.Square, accum_out=sm[:, 0:1]), r=[X], w=['A3', 'sm0'])
        rsqrt_small(sm[:, 0:1], ['sm0'], 1.0 / D, 1e-6)
        op('act', ACT(xs[:], xt[b][:], AF.Copy, scale=sm[:, 0:1]), r=[X, 'sm0'], w=['xs', 'yb0', 'yb1', 'yb2', 'yb3'])
        bk, bn = bank()
        bkb = bk.bitcast(BF16)
        for c in range(8):
            op('pe', TR(bkb[:, c * 128:(c + 1) * 128], xs[:, c * 128:(c + 1) * 128], identb), r=['xs', 'cb'], w=[bn])
        op('dve', CP(hT[:].rearrange("p a b -> p (a b)"), bkb), r=[bn], w=['hT'])
        for i, (c0, c1) in enumerate(BANKS):
            bk, bn = bank()
            for c in range(8):
                op('pe', MM(bk[:, 0:c1 - c0], hT[:, c, :], Wb[:, c, c0:c1], c == 0, c == 7), r=['hT', 'Wb'], w=[bn])
            if i % 2 == 0:
                op('act', ACT(p[:, c0:c1], bk[:, 0:c1 - c0], AF.Copy), r=[bn], w=['p%d' % i])
            else:
                op('dve', CP(p[:, c0:c1], bk[:, 0:c1 - c0]), r=[bn], w=['p%d' % i])
        if upto < 1:
            return early(l, n, dst, b)
        g0 = COL['rg'][0]
        op('act', ACT(ngg[:], p[:, g0:g0 + D], AF.Silu), r=['p5', 'p6'], w=['ngg'])
        if upto < 2:
            return early(l, n, dst, b)
        RC, RS = ropeC[b], ropeS[b]
        A3f = A[3][:].rearrange('p a b -> p (a b)')
        m1, m2 = A3f[:, 0:512], A3f[:, 512:1024]
        op('dve', TT(m1, p[:, 0:512], RC[:], mult), r=['p0', 'rc'], w=['A3'])
        pv = p[:, 0:512].rearrange("p (a t) -> p a t", t=2)
        m2v = m2.rearrange("p (a t) -> p a t", t=2)
        sv = RS[:].rearrange("p (a t) -> p a t", t=2)
        op('dve', TT(m2v[:, :, 0], pv[:, :, 1], sv[:, :, 0], mult), r=['p0', 'rs'], w=['A3'])
        op('dve', TT(m2v[:, :, 1], pv[:, :, 0], sv[:, :, 1], mult), r=['p0', 'rs'], w=['A3'])
        op('dve', TT(qk[:], m1, m2, add), r=['A3'], w=['qk'])
        op('act', ACT(vb[:], p[:, 512:1024], AF.Copy), r=['p1'], w=['vb'])
        bk, bn = bank()
        bkb = bk.bitcast(BF16)
        for j in range(8):
            op('pe', TR(bkb[0:64, j * 128:(j + 1) * 128], qk[:, j * 64:(j + 1) * 64], identb), r=['qk', 'cb'], w=[bn])
        op('dve', CP(qkT[:].rearrange("p a b -> p (a b)"), bkb[0:64, :]), r=[bn], w=['dT0'])
        bk, bn = bank()
        for h in range(4):
            op('pe', MM(bk[:, h * 128:(h + 1) * 128], qkT[:, 4 + h, :], qkT[:, h, :]), r=['dT0'], w=[bn])
        op('dve', TT(sT[:], bk, Ui4, mult), r=[bn, 'cb'], w=['sT'])
        bo, bon = bank()
        for h in range(4):
            op('pe', MM(bo[:, h * 64:(h + 1) * 64], sT[:, h * 128:(h + 1) * 128], vb[:, h * 64:(h + 1) * 64], True, False), r=['sT', 'vb'], w=[bon])
            op('pe', MM(bo[:, h * 64:(h + 1) * 64], qkT[:, h, :], Rrb[:, h * 64:(h + 1) * 64], False, True), r=['dT0', 'Rrb'], w=[bon])
        bk, bn = bank()
        for h in range(4):
            op('pe', MM(bk[0:64, h * 64:(h + 1) * 64], qk[:, 256 + h * 64:256 + (h + 1) * 64], vb[:, h * 64:(h + 1) * 64]), r=['qk', 'vb'], w=[bn])
        op('dve', TT(tmpR[:], Rr32[:], bk[0:64, 0:256], add), r=['Rr32', bn], w=['tmpR'])
        op('pool', TT(Rr32[:], tmpR[:], GC, mult), r=['tmpR', 'cf'], w=['Rr32'])
        op('act', ACT(Rrb[:], Rr32[:], AF.Copy), r=['Rr32'], w=['Rrb'])
        branch_out(bo[:, 0:256], bon, 0)
        if upto < 3:
            return early(l, n, dst, b)
        c0 = COL['dq'][0]
        for k in range(4):
            op('pool', TT(pk[b][:, k, :], p[:, c0:c0 + 768], cw[:, k, :], mult), r=['p2', 'p3', 'cw'], w=['pk%d' % b])
        bq, bqn = bank()
        bv, bvn = bank()
        for (bk, bn, o0, w_) in ((bq, bqn, 0, 512), (bv, bvn, 512, 256)):
            taps = [(Sh[3 - k], pk[b][:, k, o0:o0 + w_], 'pk%d' % b) for k in range(4)]
            taps += [(ShP[3 - k], pk[1 - b][:, k, o0:o0 + w_], 'pk%d' % (1 - b)) for k in range(3)]
            for i, (lt, rh, nm) in enumerate(taps):
                op('pe', MM(bk[:, 0:w_], lt, rh, i == 0, i == len(taps) - 1), r=['cb', nm], w=[bn])
        if upto < 3.1:
            return early(l, n, dst, b)
        op('act', ACT(cs[:, 0:512], bq, AF.Silu), r=[bqn], w=['A2'])
        op('act', ACT(cs[:, 512:768], bv[:, 0:256], AF.Silu), r=[bvn], w=['A2'])
        op('act', ACT(sq[:], cs[:, 0:512], AF.Square), r=['A2'], w=['A3'])
        rn = sm[:, 24:32]
        op('dve', lambda e: e.tensor_reduce(out=rn, in_=sq[:].rearrange("p (h d) -> p h d", h=8), axis=AX.X, op=add), r=['A3'], w=['rn'])
        rsqrt_small(rn, ['rn'], 1.0, 1e-6)
        h64 = lambda a_: a_.rearrange("p (h d) -> p h d", h=4)
        bc64 = lambda a_: a_.unsqueeze(2).to_broadcast([128, 4, 64])
        op('dve', TS(rn[:, 0:4], rn[:, 0:4], 0.125, None, mult), r=['rn'], w=['rn'])
        op('dve', TT(h64(qn[:]), h64(cs[:, 0:256]), bc64(rn[:, 0:4]), mult), r=['A2', 'rn'], w=['qn'])
        op('dve', TT(h64(kn[:]), h64(cs[:, 256:512]), bc64(rn[:, 4:8]), mult), r=['A2', 'rn'], w=['kn'])
        beta, gz, gg_, gc, eg, egl, egll, beg = (sm[:, 32:36], sm[:, 36:40], sm[:, 40:44], sm[:, 44:52], sm[:, 52:56],
                                                  sm[:, 56:60], sm[:, 60:64], sm[:, 4:8])
        DS = ['dsm']
        cb_, ca_ = COL['dbeta'][0], COL['da'][0]
        op('act', ACT(beta, p[:, cb_:cb_ + 4], AF.Sigmoid), r=['p4'], w=DS)
        op('dve', TT(gz, p[:, ca_:ca_ + 4], dnp[:, 4:8], add), r=['p4', 'dnp'], w=DS)
        op('act', ACT(gz, gz, AF.Exp), r=DS, w=DS)
        op('act', ACT(gz, gz, AF.Ln, bias=1.0), r=DS, w=DS)
        op('dve', TT(gg_, gz, dnp[:, 0:4], mult), r=DS + ['dnp'], w=DS)
        if upto < 3.2:
            return early(l, n, dst, b)
        bg, bgn = bank()
        op('pe', MM(bg[:, 0:4], triU, gg_), r=['cf'] + DS, w=[bgn])
        op('pe', MM(bg[:, 4:8], ones, gg_), r=['cf'] + DS, w=[bgn])
        op('pe', MM(bg[0:4, 128:256], gg_, triU), r=['cf'] + DS, w=[bgn])
        op('dve', CP(gc, bg[:, 0:8]), r=[bgn], w=DS)
        op('dve', CP(gct[:], bg[0:4, 128:256]), r=[bgn], w=['gct'])
        op('act', ACT(eg, gc[:, 0:4], AF.Exp), r=DS, w=DS)
        op('dve', TT(egl, gc[:, 4:8], gc[:, 0:4], sub), r=DS, w=DS)
        op('act', ACT(egl, egl, AF.Exp), r=DS, w=DS)
        op('act', ACT(egll, gc[:, 4:8], AF.Exp), r=DS, w=DS)
        op('dve', TT(beg, beta, eg, mult), r=DS, w=DS)
        if upto < 3.3:
            return early(l, n, dst, b)
        bR, bRn = bank()
        for h in range(4):
            op('pe', MM(bR[:, h * 128:(h + 1) * 128], Esel[:, h * 128:(h + 1) * 128], gct[:]), r=['cf', 'gct'], w=[bRn])
        h128 = lambda a_: a_.rearrange("p (h c) -> p h c", h=4)
        op('dve', TT(h128(tmpU), h128(bR), gc[:, 0:4].unsqueeze(2).to_broadcast([128, 4, 128]), sub), r=[bRn] + DS, w=['A0'])
        op('dve', TS(tmpL, tmpU, 0.0, None, ALU.max), r=['A0'], w=['A0'])
        op('dve', TS(tmpU, tmpU, 0.0, None, ALU.min), r=['A0'], w=['A0'])
        if upto < 3.4:
            return early(l, n, dst, b)
        op('act', ACT(tmpL[:], tmpL[:], AF.Exp, scale=-1.0), r=['A0'], w=['A0'])
        op('act', ACT(tmpU[:], tmpU[:], AF.Exp), r=['A0'], w=['A0'])
        op('pool', TT(DLs[:], tmpL[:], negLs4, mult), r=['A0', 'cb'], w=['A0'])
        op('pool', TT(DUi[:], tmpU[:], Ui4, mult), r=['A0', 'cb'], w=['A1'])
        op('pool', TT(DUs[:], tmpU[:], negUs4, mult), r=['A0', 'cb'], w=['A0'])
        if l == 0 and n == 0:
            dump('cs', cs, ['A2']); dump('kn', kn[:], ['kn']); dump('qn', qn[:], ['qn']); dump('sm', sm[:, 24:64], DS + ['rn'])
            dump('DLs', DLs, ['A0']); dump('DUs', DUs, ['A0']); dump('DUi', DUi, ['A1'])
        op('dve', TT(h64(kbq[:, 0:256]), h64(kn[:]), bc64(beta), mult), r=['kn'] + DS, w=['kbq'])
        op('dve', TT(h64(kbq[:, 256:512]), h64(qn[:]), bc64(eg), mult), r=['qn'] + DS, w=['kbq'])
        if upto < 3.5:
            return early(l, n, dst, b)
        srcs = [(kn, 0, 'kn'), (kbq, 0, 'kbq'), (qn, 0, 'qn'), (kbq, 256, 'kbq')]
        for half in range(2):
            bk, bn = bank()
            bkb = bk.bitcast(BF16)
            for jj in range(8):
                j = half * 8 + jj
                t_, off, nm = srcs[j // 4]
                h = j % 4
                op('pe', TR(bkb[0:64, jj * 128:(jj + 1) * 128], t_[:, off + h * 64:off + (h + 1) * 64], identb), r=[nm, 'cb'], w=[bn])
            op('dve' if half == 0 else 'act', CP(dT[:, half * 8:(half + 1) * 8, :].rearrange("p a b -> p (a b)"), bkb[0:64, :]) if half == 0 else
               ACT(dT[:, half * 8:(half + 1) * 8, :].rearrange("p a b -> p (a b)"), bkb[0:64, :], AF.Copy), r=[bn], w=['dT%d' % half])
        bA, bAn = bank()
        bAT, bATn = bank()
        bat, batn = bank()
        for h in range(4):
            hs = slice(h * 128, (h + 1) * 128)
            op('pe', MM(bA[:, hs], dT[:, 4 + h, :], dT[:, h, :]), r=['dT0'], w=[bAn])
            op('pe', MM(bAT[:, hs], dT[:, h, :], dT[:, 4 + h, :]), r=['dT0'], w=[bATn])
            op('pe', MM(bat[:, hs], dT[:, h, :], dT[:, 8 + h, :]), r=['dT0', 'dT1'], w=[batn])
        op('dve', TT(Pm[0][:], bA, DLs[:], mult), r=[bAn, 'A0'], w=['Pm0'])
        op('dve', TT(PTm[0][:], bAT, DUs[:], mult), r=[bATn, 'A0'], w=['PTm0'])
        op('dve', TT(attnT[:], bat, DUi[:], mult), r=[batn, 'A1'], w=['attnT'])
        op('dve', TT(X32[:, :, 0:64], h64(cs[:, 512:768]), bc64(beta), mult), r=['A2'] + DS, w=['A1'])
        op('dve', TT(X32[:, :, 64:128], h64(kn[:]), bc64(beg), mult), r=['kn'] + DS, w=['A1'])
        if l == 0 and n == 0:
            dump('N0', Pm[0], ['Pm0']); dump('NT0', PTm[0], ['PTm0']); dump('attnT', attnT[:], ['attnT']); dump('X0', X32, ['A1'])
        X32f = X32[:].rearrange("p a b -> p (a b)")
        Xbf = Xb[:].rearrange("p a b -> p (a b)")
        op('act', ACT(Xbf, X32f, AF.Copy), r=['A1'], w=['Xb'])
        if upto < 3.6:
            return early(l, n, dst, b)
        Lm, LmT, Yb, W1b = sT[:], qk[:], kbq[:], xs[:, 512:1024]
        Tm, TTm = Pm[1], PTm[1]
        v4 = lambda a_: a_.rearrange("p (h c) -> p h c", h=4)
        bm = lambda i: HM[i].unsqueeze(1).to_broadcast([128, 4, 128])
        idb4 = identb.unsqueeze(1).to_broadcast([128, 4, 128])
        op('pool', TT(v4(Tm), v4(Pm[0]), bm(0), mult), r=['Pm0', 'cb'], w=['Pm1'])
        op('pool', TT(v4(Tm), v4(Tm), idb4, add), r=['Pm1', 'cb'], w=['Pm1'])
        op('pool', TT(v4(TTm), v4(PTm[0]), bm(7), mult), r=['PTm0', 'cb'], w=['PTm1'])
        op('pool', TT(v4(TTm), v4(TTm), idb4, add), r=['PTm1', 'cb'], w=['PTm1'])
        for lev in range(1, 7):
            op('pool', TT(v4(Lm), v4(Pm[0]), bm(lev), mult), r=['Pm0', 'cb'], w=['sT'])
            op('pool', TT(v4(LmT), v4(PTm[0]), bm(7 + lev), mult), r=['PTm0', 'cb'], w=['qk'])
            bY, bYn = bank()
            bW, bWn = bank()
            for h in range(4):
                hs = slice(h * 128, (h + 1) * 128)
                op('pe', MM(bY[:, hs], LmT[:, hs], Tm[:, hs]), r=['qk', 'Pm1'], w=[bYn])
                op('pe', MM(bW[:, hs], Lm[:, hs], TTm[:, hs]), r=['sT', 'PTm1'], w=[bWn])
            op('act', ACT(Yb, bY, AF.Copy), r=[bYn], w=['kbq'])
            op('dve', CP(W1b, bW), r=[bWn], w=['yb2', 'yb3'])
            bZ, bZn = bank()
            bW2, bW2n = bank()
            for h in range(4):
                hs = slice(h * 128, (h + 1) * 128)
                op('pe', MM(bZ[:, hs], TTm[:, hs], Yb[:, hs]), r=['kbq', 'PTm1'], w=[bZn])
                op('pe', MM(bW2[:, hs], Tm[:, hs], W1b[:, hs]), r=['yb2', 'yb3', 'Pm1'], w=[bW2n])
            op('dve', TT(Tm, Tm, bZ, add), r=['Pm1', bZn], w=['Pm1'])
            op('dve', TT(TTm, TTm, bW2, add), r=['PTm1', bW2n], w=['PTm1'])
        if upto < 3.7:
            return early(l, n, dst, b)
        bX, bXn = bank()
        for h in range(4):
            hs = slice(h * 128, (h + 1) * 128)
            op('pe', MM(bX[:, hs], TTm[:, hs], Xb[:, h, :]), r=['PTm1', 'Xb'], w=[bXn])
        op('dve', CP(X32f, bX), r=[bXn], w=['A1'])
        op('act', ACT(Xbf, bX, AF.Copy), r=[bXn], w=['Xb'])
        if l == 0 and n == 0:
            dump('X7', X32, ['A1'])
        bk, bn = bank()
        bkb = bk.bitcast(BF16)
        for h in range(4):
            op('pe', TR(bkb[0:64, h * 128:(h + 1) * 128], Xb[:, h, 64:128], identb), r=['Xb', 'cb'], w=[bn])
        op('dve', CP(WT[:].rearrange("p a b -> p (a b)"), bkb[0:64, 0:512]), r=[bn], w=['WT'])
        if upto < 3.8:
            return early(l, n, dst, b)
        bw, bwn = bank()
        for h in range(4):
            hs = slice(h * 64, (h + 1) * 64)
            op('pe', MM(bw[:, hs], WT[:, h, :], Sdb[:, hs]), r=['WT', 'Sdb'], w=[bwn])
        op('dve', TT(vnew[:].rearrange("p (h d) -> p h d", h=4), X32[:, :, 0:64], bw[:, 0:256].rearrange("p (h d) -> p h d", h=4), sub),
           r=['A1', bwn], w=['vnew'])
        if upto < 3.85:
            return early(l, n, dst, b)
        bo, bon = bank()
        for h in range(4):
            hs = slice(h * 64, (h + 1) * 64)
            op('pe', MM(bo[:, hs], dT[:, 12 + h, :], Sdb[:, hs], True, False), r=['dT1', 'Sdb'], w=[bon])
            op('pe', MM(bo[:, hs], attnT[:, h * 128:(h + 1) * 128], vnew[:, hs], False, True), r=['attnT', 'vnew'], w=[bon])
        if upto < 3.9:
            return early(l, n, dst, b)
        op('dve', TT(h64(kg[:]), h64(kn[:]), bc64(egl), mult), r=['kn'] + DS, w=['kg'])
        bk, bn = bank()
        for h in range(4):
            hs = slice(h * 64, (h + 1) * 64)
            op('pe', MM(bk[0:64, hs], kg[:, hs], vnew[:, hs]), r=['kg', 'vnew'], w=[bn])
        for h in range(4):
            hs = slice(h * 64, (h + 1) * 64)
            op('dve', STT(Sd32[:, hs], Sd32[:, hs], egll[0:64, h:h + 1], bk[0:64, hs], mult, add), r=['Sd32', bn] + DS, w=['Sd32'])
        if upto < 3.95:
            return early(l, n, dst, b)
        op('dve', CP(Sdb[:], Sd32[:]), r=['Sd32'], w=['Sdb'])
        if l == 0 and n == 0:
            dump('vnew', vnew[:], ['vnew'])
            op('dve', CP(Gb[:, 0:256], bo[:, 0:256]), r=[bon], w=['G'])
            dump('odn', Gb[:, 0:256], ['G'])
        branch_out(bo[:, 0:256], bon, 1)
        if l == 0 and n == 0:
            dump('ssqdn', sm[:, 12:16], ['sm_b1']); dump('ngg', ngg[:], ['ngg'])
        if upto < 4:
            return early(l, n, dst, b)
        cu = COL['su'][0]
        op('act', ACT(ub[:], p[:, cu:cu + 256], AF.Copy), r=['p3'], w=['ub'])
        bk, bn = bank()
        bkb = bk.bitcast(BF16)
        for c in range(2):
            op('pe', TR(bkb[:, c * 128:(c + 1) * 128], ub[:, c * 128:(c + 1) * 128], identb), r=['ub', 'cb'], w=[bn])
        op('dve', CP(uT[:].rearrange("p a b -> p (a b)"), bkb[:, 0:256]), r=[bn], w=['uT'])
        xr, xrn = dbank()
        xi, xin = dbank()
        for s_ in range(8):
            op('pe', MM(xr[:, s_ * 128:(s_ + 1) * 128], BT[:, 0, s_, :], uT[:, s_ // 4, :]), r=['BT', 'uT'], w=[xrn[s_ // 4]])
            op('pe', MM(xi[:, s_ * 128:(s_ + 1) * 128], BT[:, 1, s_, :], uT[:, s_ // 4, :]), r=['BT', 'uT'], w=[xin[s_ // 4]])
        Af = [a[:].rearrange("p a b -> p (a b)") for a in A]
        fl = lambda t_: t_[:].rearrange("p a b -> p (a b)")
        op('dve', TT(Af[0], xr[:], fl(Tc), mult), r=xrn + ['Tc'], w=['A0'])
        op('dve', TT(Af[1], xi[:], fl(Ts), mult), r=xin + ['Ts'], w=['A1'])
        op('dve', TT(Af[0], Af[0], Af[1], sub), r=['A0', 'A1'], w=['A0'])
        op('dve', TT(Af[1], xr[:], fl(Ts), mult), r=xrn + ['Ts'], w=['A1'])
        op('dve', TT(Af[2], xi[:], fl(Tc), mult), r=xin + ['Tc'], w=['A2'])
        op('dve', TT(Af[1], Af[1], Af[2], add), r=['A1', 'A2'], w=['A1'])
        op('dve', TT(A[0][:, :, 0], A[0][:, :, 0], mc[:, 0, :], add), r=['A0', 'mc'], w=['A0'])
        op('dve', TT(A[1][:, :, 0], A[1][:, :, 0], mc[:, 1, :], add), r=['A1', 'mc'], w=['A1'])
        op('dve', lambda e: e.tensor_tensor_scan(out=Af[2], data0=fl(MAGz), data1=Af[0], initial=0.0, op0=mult, op1=add), r=['MAGz', 'A0'], w=['A2'])
        op('dve', lambda e: e.tensor_tensor_scan(out=Af[3], data0=fl(MAGz), data1=Af[1], initial=0.0, op0=mult, op1=add), r=['MAGz', 'A1'], w=['A3'])
        T = lambda i: s5t[:, i, :]
        mec, mes, t0, t1 = T(15), T(16), T(17), T(18)
        S2 = ['s5t2']
        op('dve', TT(t0, A[2][:, :, 127], mec, mult), r=['A2', 's5t'], w=S2)
        op('dve', TT(t1, A[3][:, :, 127], mes, mult), r=['A3', 's5t'], w=S2)
        op('dve', TT(mc[:, 0, :], t0, t1, sub), r=S2, w=['mc'])
        op('dve', TT(t0, A[2][:, :, 127], mes, mult), r=['A2', 's5t'], w=S2)
        op('dve', TT(t1, A[3][:, :, 127], mec, mult), r=['A3', 's5t'], w=S2)
        op('dve', TT(mc[:, 1, :], t0, t1, add), r=S2, w=['mc'])
        op('pool', TT(Af[0], Af[2], fl(Oc), mult), r=['A2', 'Oc'], w=['A0'])
        op('pool', TT(Af[1], Af[3], fl(Os), mult), r=['A3', 'Os'], w=['A1'])
        op('pool', TT(fl(sre), Af[0], Af[1], sub), r=['A0', 'A1'], w=['Pm0', 'PTm0'])
        op('pool', TT(Af[0], Af[2], fl(Os), mult), r=['A2', 'Os'], w=['A0'])
        op('pool', TT(Af[1], Af[3], fl(Oc), mult), r=['A3', 'Oc'], w=['A1'])
        op('pool', TT(fl(sim), Af[0], Af[1], add), r=['A0', 'A1'], w=['Pm1', 'PTm1'])
        by, byn = bank()
        for s_ in range(8):
            cs_ = slice(s_ * 32, (s_ + 1) * 32)
            op('pe', MM(by[:, cs_], sre[:, s_, :], CT[:, 0, s_, :], True, False), r=['Pm0', 'PTm0', 'CT'], w=[byn])
            op('pe', MM(by[:, cs_], sim[:, s_, :], CT[:, 1, s_, :], False, False), r=['Pm1', 'PTm1', 'CT'], w=[byn])
            op('pe', MM(by[:, cs_], uT[:, s_ // 4, :], Dg[:, s_, :], False, True), r=['uT', 'Dg'], w=[byn])
        y_ = by[:, 0:256]
        op('act', ACT(t256[:], y_, AF.Square), r=[byn], w=['G'])
        op('dve', TS(t256[:], t256[:], 0.044715, 1.0, mult, add), r=['G'], w=['G'])
        op('dve', TT(t256[:], t256[:], y_, mult), r=['G', byn], w=['G'])
        op('act', ACT(t256[:], t256[:], AF.Sigmoid, scale=2.0 * 0.7978845608028654), r=['G'], w=['G'])
        op('dve', TT(yg[:], t256[:], y_, mult), r=['G', byn], w=['G'])
        op('act', ACT(ygb[:], yg[:], AF.Copy), r=['G'], w=['ygb'])
        bk, bn = bank()
        bkb = bk.bitcast(BF16)
        for c in range(2):
            op('pe', TR(bkb[:, c * 128:(c + 1) * 128], ygb[:, c * 128:(c + 1) * 128], identb), r=['ygb', 'cb'], w=[bn])
        op('dve', CP(ygT[:].rearrange("p a b -> p (a b)"), bkb[:, 0:256]), r=[bn], w=['ygT'])
        bgl, bgln = bank()
        op('pe', MM(bgl[:, 0:256], ygT[:, 0, :], Wglu[:, 0, :], True, False), r=['ygT', 'Wglu'], w=[bgln])
        op('pe', MM(bgl[:, 0:256], ygT[:, 1, :], Wglu[:, 1, :], False, False), r=['ygT', 'Wglu'], w=[bgln])
        op('pe', MM(bgl[:, 0:256], onesb[:], bglu[:], False, True), r=['onesb', 'bglu'], w=[bgln])
        op('act', ACT(u256[:], bgl[:, 0:256], AF.Sigmoid), r=[bgln], w=['G'])
        op('dve', TT(u256[:], u256[:], yg[:], mult), r=['G', 'G'], w=['G'])
        op('dve', TT(yb[:, 512:768], u256[:], ngg[:, 512:768], mult), r=['G', 'ngg', 'hT'], w=['yb2'])
        if upto < 5:
            return early(l, n, dst, b)
        cgc, cgq, cgk = COL['gcode'][0], COL['gq'][0], COL['gk'][0]
        op('act', ACT(gcb[:], p[:, cgc:cgc + 16], AF.Copy), r=['p4'], w=['gcb'])
        bk, bn = bank()
        bkb = bk.bitcast(BF16)
        op('pe', TR(bkb[0:16, 0:128], gcb[:], identb), r=['gcb', 'cb'], w=[bn])
        op('dve', CP(gcT[:], bkb[0:16, 0:128]), r=[bn], w=['gcT'])
        bz, bzn = bank()
        op('pe', MM(bz[:, 0:128], gcT[:], wgk[:], True, False), r=['gcT', 'wgk'], w=[bzn])
        op('pe', MM(bz[:, 0:128], onesb[:], bgk[:], False, True), r=['onesb', 'bgk'], w=[bzn])
        gkk, cum, clb, ec, enc, ecl = [g[:] for g in g128]
        op('act', ACT(gkk, bz[:, 0:128], AF.Exp, scale=-1.0), r=[bzn], w=['G'])
        op('act', ACT(gkk, gkk, AF.Ln, bias=1.0), r=['G'], w=['G'])
        op('dve', TS(gkk, gkk, -1.0 / 16, None, mult), r=['G'], w=['G'])
        bc_, bcn_ = bank()
        op('pe', MM(bc_[:, 0:128], triU, gkk), r=['cf', 'G'], w=[bcn_])
        op('pe', MM(bc_[:, 128:256], ones, gkk), r=['cf', 'G'], w=[bcn_])
        for h in range(4):
            op('pe', MM(bc_[0:32, 256 + h:257 + h], g128[0][:, h * 32:(h + 1) * 32], ones[:, 0:1]), r=['cf', 'G'], w=[bcn_])
        op('dve', CP(cum, bc_[:, 0:128]), r=[bcn_], w=['G'])
        op('dve', TT(clb, bc_[:, 128:256], cum, sub), r=[bcn_, 'G'], w=['G'])
        op('act', ACT(ec, cum, AF.Exp), r=['G'], w=['G'])
        op('act', ACT(enc, cum, AF.Exp, scale=-1.0), r=['G'], w=['G'])
        op('act', ACT(ecl, clb, AF.Exp), r=['G'], w=['G'])
        ecl32 = sm[0:32, 64:68]
        op('act', ACT(ecl32, bc_[0:32, 256:260], AF.Exp), r=[bcn_], w=['ecl32'])
        op('dve', STT(gq3[:, 0:128], p[:, cgq:cgq + 128], 32.0 ** -0.5, ec, mult, mult), r=['p4', 'G'], w=['gq3'])
        op('dve', TT(gq3[:, 128:256], p[:, cgk:cgk + 128], enc, mult), r=['p4', 'G'], w=['gq3'])
        op('dve', TT(gq3[:, 256:384], p[:, cgk:cgk + 128], ecl, mult), r=['p4', 'G'], w=['gq3'])
        bk, bn = bank()
        bkb = bk.bitcast(BF16)
        for j in range(8):
            op('pe', TR(bkb[0:32, j * 128:(j + 1) * 128], gq3[:, j * 32:(j + 1) * 32], identb), r=['gq3', 'cb'], w=[bn])
        op('dve', CP(gT[:].rearrange("p a b -> p (a b)"), bkb[0:32, :]), r=[bn], w=['dT0'])
        bk, bn = bank()
        for h in range(4):
            op('pe', MM(bk[:, h * 128:(h + 1) * 128], gT[:, 4 + h, :], gT[:, h, :]), r=['dT0'], w=[bn])
        op('dve', TT(gsT[:], bk, Ui4, mult), r=[bn, 'cb'], w=['sT'])
        bo, bon = bank()
        for h in range(4):
            hs = slice(h * 64, (h + 1) * 64)
            gv_ = vb[:, 256 + h * 64:256 + (h + 1) * 64]
            op('pe', MM(bo[:, hs], gsT[:, h * 128:(h + 1) * 128], gv_, True, False), r=['sT', 'vb'], w=[bon])
            op('pe', MM(bo[:, hs], gT[:, h, :], Gsb[:, hs], False, True), r=['dT0', 'Gsb'], w=[bon])
        bk, bn = bank()
        for h in range(4):
            hs = slice(h * 64, (h + 1) * 64)
            op('pe', MM(bk[0:32, hs], gq3[:, 256 + h * 32:256 + (h + 1) * 32], vb[:, 256 + h * 64:256 + (h + 1) * 64]), r=['gq3', 'vb'], w=[bn])
        for h in range(4):
            hs = slice(h * 64, (h + 1) * 64)
            op('dve', STT(Gs32[:, hs], Gs32[:, hs], ecl32[:, h:h + 1], bk[0:32, hs], mult, add), r=['Gs32', bn, 'ecl32'], w=['Gs32'])
        op('act', ACT(Gsb[:], Gs32[:], AF.Copy), r=['Gs32'], w=['Gsb'])
        branch_out(bo[:, 0:256], bon, 3)
        if upto < 6:
            return early(l, n, dst, b)
        bk, bn = bank()
        bkb = bk.bitcast(BF16)
        YB = ['yb0', 'yb1', 'yb2', 'yb3']
        if dbg and l == NL - 1:
            R.dma('sp', d_dbg[n * 128:(n + 1) * 128, :], yb[:], key='dbg', r=YB, w=['dbgout%d' % n])
        for c in range(8):
            op('pe', TR(bkb[:, c * 128:(c + 1) * 128], yb[:, c * 128:(c + 1) * 128], identb), r=YB + ['cb'], w=[bn])
        op('dve', CP(yT[:].rearrange("p a b -> p (a b)"), bkb), r=[bn], w=['hT'])
        po, pon = dbank()
        for half in range(2):
            for c in range(8):
                op('pe', MM(po[:, half * 512:(half + 1) * 512], yT[:, c, :], Wo[:, c, half * 512:(half + 1) * 512], c == 0, c == 7), r=['hT', 'Wo'], w=[pon[half]])
        op('act', ACT(A[3][:].rearrange('p a b -> p (a b)'), po[:], AF.Square, accum_out=sm[:, 1:2]), r=pon, w=['A3', 'sm1'])
        rsqrt_small(sm[:, 1:2], ['sm1'], 1.0 / D, 1e-6)
        op('dve', STT(Af[0], po[:], sm[:, 1:2], Gpost[:], mult, mult), r=pon + ['sm1', 'Gpost'], w=['A0'])
        op('pool', TT(Af[0], Af[0], xt[b][:], add), r=['A0', X], w=['A0'])
        R.dma('sp', dst[n * 128:(n + 1) * 128, :], Af[0], key='xo', r=['A0'], w=['dst%d_%d' % (l, n)])

    for l in range(NL):
        if do_setup:
            setup(l)
        for n in range(NT):
            tile(l, n, xs_dram[l], xs_dram[l + 1])
    print('SBUF bytes/partition', R.sb_bytes)
    R.finish(final_reads=['dst%d_%d' % (NL - 1, n) for n in range(max(0, NT - 2), NT)] + (['dbgout%d' % n for n in range(NT)] + dumps if dbg else []))
    return nc


def _bf(a):
    return np.ascontiguousarray(a).astype(ml_dtypes.bfloat16)


def make_consts(NT):
    idn = np.eye(128, dtype=np.float32)
    j = np.arange(128)[:, None]
    t = np.arange(128)[None, :]
    Sh = [(j == t - s).astype(np.float32) for s in range(4)]
    ShP = [(j == 128 + t - s).astype(np.float32) for s in range(1, 4)]
    Ui = (j <= t).astype(np.float32)
    negLs = -(t < j).astype(np.float32)
    negUs = -(t > j).astype(np.float32)
    hm = []
    ii_, jj_ = np.arange(128)[:, None], np.arange(128)[None, :]
    for lev in range(7):
        b_ = 1 << lev
        hm.append(((ii_ // (2 * b_) == jj_ // (2 * b_)) & (ii_ % (2 * b_) >= b_) & (jj_ % (2 * b_) < b_)).astype(np.float32))
    hm = hm + [m_.T.copy() for m_ in hm]
    cb = np.concatenate([idn] + Sh + ShP + [np.tile(Ui, (1, 4)), np.tile(negLs, (1, 4)), np.tile(negUs, (1, 4))] + hm, axis=1)
    Esel = np.zeros((128, 512), np.float32)
    for h in range(4):
        Esel[h, h * 128:(h + 1) * 128] = 1.0
    GCt = np.zeros((128, 256), np.float32)
    for h in range(4):
        GCt[:, h * 64:(h + 1) * 64] = np.float32(GAMMA[h]) ** 128
    cf = np.concatenate([idn, Ui, np.ones((128, 128), np.float32), Esel, GCt], axis=1).astype(np.float32)
    pos = np.arange(NT * 128, dtype=np.float64)
    inv = 10000.0 ** (-np.arange(0, 64, 2, dtype=np.float64) / 64)
    ang = pos[:, None] * inv[None, :]
    cos, sin = np.cos(ang), np.sin(ang)
    ii = (np.arange(NT * 128) % 128).astype(np.float64)
    Cq = np.zeros((NT * 128, 4, 32, 2)); Sq = np.zeros_like(Cq); Ck = np.zeros_like(Cq); Sk = np.zeros_like(Cq)
    for h in range(4):
        dq = (GAMMA[h] ** (ii + 1.0)) * (64 ** -0.5)
        dk = GAMMA[h] ** (-(ii + 1.0))
        for (Ct, St, dd) in ((Cq, Sq, dq), (Ck, Sk, dk)):
            Ct[:, h, :, 0] = cos * dd[:, None]
            Ct[:, h, :, 1] = cos * dd[:, None]
            St[:, h, :, 0] = -sin * dd[:, None]
            St[:, h, :, 1] = sin * dd[:, None]
    ropec = np.concatenate([Cq.reshape(-1, 256), Ck.reshape(-1, 256)], axis=1).reshape(NT, 128, 512).astype(np.float32)
    ropes = np.concatenate([Sq.reshape(-1, 256), Sk.reshape(-1, 256)], axis=1).reshape(NT, 128, 512).astype(np.float32)
    return dict(cb=_bf(cb), cf=cf, ropec=ropec, ropes=ropes)


def make_params(inp, layers):
    f = lambda k: np.asarray(inp[k], dtype=np.float32)
    L = list(layers)
    NL = len(L)
    w_in = f('w_in')[L][:, :, PERM]
    w_out = f('w_out')[L]
    gpre = f('norm_pre')[L].reshape(NL, 8, 128).transpose(0, 2, 1)
    gpost = f('norm_post')[L].reshape(NL, 1, D)
    ng = np.concatenate([np.tile(f('ret_norm')[L], (1, 4)), np.tile(f('dn_norm')[L], (1, 4)),
                         np.ones((NL, 256), np.float32), np.tile(f('gla_norm')[L], (1, 4))], axis=1).reshape(NL, 8, 128).transpose(0, 2, 1)
    cw = f('dn_conv')[L].reshape(NL, 1, 4 * 768)
    dnp = np.concatenate([f('dn_a_log')[L], f('dn_dt_bias')[L]], axis=1).reshape(NL, 1, 8)

    def st(a):
        return a.reshape(NL, 8, 2, 64).transpose(0, 2, 3, 1).reshape(NL, 128, 8)
    ldt = np.repeat(f('s5_log_dt')[L][:, :, None], 64, axis=2)
    s5p = np.concatenate([st(f('s5_lam_re')[L]), st(f('s5_lam_im')[L]), st(ldt)], axis=2)
    bt = np.zeros((NL, 2, 128, 8, 128), np.float32)
    ct = np.zeros((NL, 2, 128, 8, 32), np.float32)
    dg = np.zeros((NL, 128, 8, 32), np.float32)
    bre, bim, cre, cim, dd = f('s5_b_re')[L], f('s5_b_im')[L], f('s5_c_re')[L], f('s5_c_im')[L], f('s5_d')[L]
    for g in range(16):
        s_, gg = g // 2, g % 2
        r0 = 32 * (s_ % 4) + gg * 16
        for k_, (bb, cc) in enumerate(((bre, cre), (bim, cim))):
            bt[:, k_, r0:r0 + 16, s_, gg * 64:(gg + 1) * 64] = bb[:, g].transpose(0, 2, 1)
            ct[:, k_, gg * 64:(gg + 1) * 64, s_, gg * 16:(gg + 1) * 16] = cc[:, g].transpose(0, 2, 1)
        for h in range(16):
            dg[:, r0 + h, s_, gg * 16 + h] = dd[:, g, h]
    bt = bt.transpose(0, 2, 1, 3, 4).reshape(NL, 128, 2048)
    ct = ct.transpose(0, 2, 1, 3, 4).reshape(NL, 128, 512)
    dg = dg.reshape(NL, 128, 256)
    wglu = f('s5_w_glu')[L].reshape(NL, 2, 128, 256).transpose(0, 2, 1, 3).reshape(NL, 128, 512)
    bglu = f('s5_b_glu')[L].reshape(NL, 1, 256)
    wgk = f('gla_w_gk')[L]
    bgk = f('gla_b_gk')[L].reshape(NL, 1, 128)
    c = np.ascontiguousarray
    return dict(w_in=c(w_in), w_out=c(w_out), gpre=c(gpre), gpost=c(gpost), ng=c(ng), cw=c(cw), dnp=c(dnp), s5p=c(s5p),
                s5bt=c(bt), s5ct=c(ct), s5dg=c(dg), wglu=c(wglu), bglu=c(bglu), wgk=c(wgk), bgk=c(bgk))


_PROG = {}


def kernel(**inputs):
    x = np.asarray(inputs['x'], dtype=np.float32)
    B, L, _ = x.shape
    NT = L // 128
    NLAY = np.asarray(inputs['w_in']).shape[0]
    key = (NT, NLAY)
    if key not in _PROG:
        _PROG[key] = (build_program(NT, NLAY), make_consts(NT))
    nc, consts = _PROG[key]
    prm = make_params(inputs, range(NLAY))
    in_maps = []
    for b in range(B):
        m = dict(consts)
        m.update(prm)
        m['x'] = np.ascontiguousarray(x[b])
        in_maps.append(m)
    res = run_bass_kernel_spmd(nc, in_maps, core_ids=list(range(B)))
    return np.stack([np.asarray(res.results[b]['out'], dtype=np.float32) for b in range(B)], axis=0)
```

```python
import os
import numpy as np
import ml_dtypes
import concourse.bass as bass
import concourse.mybir as mybir
from concourse.bass_utils import run_bass_kernel_spmd

F32 = mybir.dt.float32
BF16 = mybir.dt.bfloat16
AF = mybir.ActivationFunctionType
ALU = mybir.AluOpType
AX = mybir.AxisListType


class Rec:
    def __init__(self, nc):
        self.nc = nc
        self.ops = {k: [] for k in ('pe', 'act', 'dve', 'pool', 'sp')}
        self.cnt = {k: 0 for k in self.ops}
        self.clock = {k: {} for k in self.ops}
        self.snap = {}
        self.last_w = {}
        self.readers = {}
        self.sems = {}
        self.dma_cnt = {}

    def sem(self, key):
        if key not in self.sems:
            self.sems[key] = self.nc.alloc_semaphore(name="s_" + key.replace(':', '_'))
        return self.sems[key]

    def sb(self, name, shape, dt):
        n = 1
        for d_ in shape[1:]:
            n *= d_
        self.sb_bytes = getattr(self, 'sb_bytes', 0) + n * (2 if dt == BF16 else 4)
        return self.nc.alloc_sbuf_tensor("s_" + name, list(shape), dt)

    def ps(self, name, shape, dt=F32):
        return self.nc.alloc_psum_tensor("q_" + name, list(shape), dt)

    def _deps(self, eng, r, w):
        deps = {}

        def add(kn):
            if kn is None:
                return
            k, n = kn
            if deps.get(k, 0) < n:
                deps[k] = n
        for b in r:
            add(self.last_w.get(b))
        for b in w:
            add(self.last_w.get(b))
            for kn in self.readers.get(b, ()):
                add(kn)
        ck = self.clock[eng]
        waits = []
        if eng == 'pe':
            deps.pop('pe', None)
        for k, n in deps.items():
            if ck.get(k, 0) < n:
                waits.append((k, n))
        for k, n in waits:
            sn = self.snap.get((k, n))
            if sn:
                for k2, n2 in sn.items():
                    if ck.get(k2, 0) < n2:
                        ck[k2] = n2
            if ck.get(k, 0) < n:
                ck[k] = n
        return waits

    def _mark(self, me, r, w):
        for b in r:
            self.readers.setdefault(b, []).append(me)
        for b in w:
            self.last_w[b] = me
            self.readers[b] = []

    def op(self, eng, fn, r=(), w=()):
        w = list(w) + [b_ for b_ in r if b_.startswith('PS') and b_ not in w]
        waits = self._deps(eng, r, w)
        self.cnt[eng] += 1
        me = (eng, self.cnt[eng])
        self.snap[me] = dict(self.clock[eng])
        self.ops[eng].append((waits, fn, eng, 1))
        self._mark(me, r, w)

    def dma(self, q, out, in_, key, r=(), w=()):
        waits = self._deps(q, r, w)
        dk = 'dma:' + key
        self.dma_cnt[dk] = self.dma_cnt.get(dk, 0) + 1
        me = (dk, self.dma_cnt[dk])
        self.snap[me] = dict(self.clock[q])
        self.ops[q].append((waits, lambda e: e.dma_start(out=out, in_=in_), dk, 16))
        self._mark(me, r, w)

    def finish(self, final_reads=()):
        waits = self._deps('sp', list(final_reads), [])
        nc = self.nc
        semval = lambda k, n: (self.sem(k), n * 16 if k.startswith('dma:') else n)
        for k in self.ops:
            self.sem(k)
        final = [semval(k, n) for k, n in waits]
        for k in ('pe', 'act', 'dve', 'pool'):
            if self.cnt[k] > 0:
                final.append((self.sem(k), self.cnt[k]))
        for dk, n in self.dma_cnt.items():
            final.append((self.sem(dk), 16 * n))
        emit_lists = {}
        for eng, lst in self.ops.items():
            el = []
            for waits_, fn, inck, incv in lst:
                el.append(([semval(k, n) for k, n in waits_], fn, self.sem(inck), incv))
            emit_lists[eng] = el
        with nc.Block() as block:
            def run(e, el, extra=()):
                for ws, fn, s, v in el:
                    for sm, val in ws:
                        e.wait_ge(sm, val)
                    fn(e).then_inc(s, v)
                for sm, val in extra:
                    e.wait_ge(sm, val)

            @block.sync
            def _(e):
                run(e, emit_lists['sp'], final)

            @block.tensor
            def _(e):
                run(e, emit_lists['pe'])

            @block.scalar
            def _(e):
                run(e, emit_lists['act'])

            @block.vector
            def _(e):
                run(e, emit_lists['dve'])

            @block.gpsimd
            def _(e):
                run(e, emit_lists['pool'])


D = 1024
NCOL = 3352
ORIG = dict(rq=(0, 256), rk=(256, 512), rv=(512, 768), rg=(768, 1024), dq=(1024, 1280), dk=(1280, 1536),
            dv=(1536, 1792), dbeta=(1792, 1796), da=(1796, 1800), dg=(1800, 2056), su=(2056, 2312),
            sg=(2312, 2568), gq=(2568, 2696), gk=(2696, 2824), gv=(2824, 3080), gcode=(3080, 3096),
            gg=(3096, 3352))
ORDER = ['rq', 'rk', 'rv', 'gv', 'dq', 'dk', 'dv', 'su', 'gq', 'gk', 'gcode', 'dbeta', 'da', 'rg', 'dg', 'sg', 'gg']
COL = {}
_o = 0
for _k in ORDER:
    _w = ORIG[_k][1] - ORIG[_k][0]
    COL[_k] = (_o, _o + _w)
    _o += _w
PERM = np.concatenate([np.arange(*ORIG[k]) for k in ORDER])
BANKS = [(0, 512), (512, 1024), (1024, 1536), (1536, 2048), (2048, 2328), (2328, 2840), (2840, 3352)]
C = 128
GAMMA = [1.0 - 2.0 ** (-5.0 - h) for h in range(4)]


def build_program(NT, NL, dbg=False, upto=99, do_setup=True):
    nc = bass.Bass("TRN2", target_bir_lowering=False)
    R = Rec(nc)
    din = lambda name, shape, dt=F32: nc.dram_tensor(name, list(shape), dt, kind="ExternalInput").ap()
    x_in = din("x", [NT * 128, D])
    x_out = nc.dram_tensor("out", [NT * 128, D], F32, kind="ExternalOutput").ap()
    xs_dram = [x_in]
    for l in range(NL - 1):
        xs_dram.append(nc.dram_tensor("xmid%d" % l, [NT * 128, D], F32).ap())
    xs_dram.append(x_out)
    d_dbg = nc.dram_tensor("dbg", [NT * 128, D], BF16, kind="ExternalOutput").ap() if dbg else None
    d_win = din("w_in", [NL, D, NCOL])
    d_wout = din("w_out", [NL, D, D])
    d_gpre = din("gpre", [NL, 128, 8])
    d_gpost = din("gpost", [NL, 1, D])
    d_ng = din("ng", [NL, 128, 8])
    d_cw = din("cw", [NL, 1, 4 * 768])
    d_dnp = din("dnp", [NL, 1, 8])
    d_s5p = din("s5p", [NL, 128, 24])
    d_bt = din("s5bt", [NL, 128, 2 * 8 * 128])
    d_ct = din("s5ct", [NL, 128, 2 * 8 * 32])
    d_dg = din("s5dg", [NL, 128, 8 * 32])
    d_wglu = din("wglu", [NL, 128, 2 * 256])
    d_bglu = din("bglu", [NL, 1, 256])
    d_wgk = din("wgk", [NL, 16, 128])
    d_bgk = din("bgk", [NL, 1, 128])
    d_ropec = din("ropec", [NT, 128, 512])
    d_ropes = din("ropes", [NT, 128, 512])
    d_cb = din("cb", [128, 128 * 8 + 512 * 3 + 14 * 128], BF16)
    d_cf = din("cf", [128, 128 * 3 + 512 + 256])

    sb, ps = R.sb, R.ps
    cb = sb("cb", [128, 128 * 8 + 1536 + 14 * 128], BF16)
    cf = sb("cf", [128, 128 * 3 + 768], F32)
    identb = cb[:, 0:128]
    Sh = [cb[:, 128 * (1 + s):128 * (2 + s)] for s in range(4)]
    ShP = [None] + [cb[:, 128 * (4 + s):128 * (5 + s)] for s in range(1, 4)]
    Ui4 = cb[:, 1024:1536]
    negLs4 = cb[:, 1536:2048]
    negUs4 = cb[:, 2048:2560]
    HM = [cb[:, 2560 + i * 128:2560 + (i + 1) * 128] for i in range(14)]
    identf = cf[:, 0:128]
    triU = cf[:, 128:256]
    ones = cf[:, 256:384]
    Esel = cf[0:4, 384:896]
    GC = cf[0:64, 896:1152]
    R.dma('sp', cb[:], d_cb[:, :], key='cb', w=['cb'])
    R.dma('sp', cf[:], d_cf[:, :], key='cf', w=['cf'])
    onesb = sb("onesb", [1, 128], BF16)
    R.op('dve', lambda e: e.tensor_copy(out=onesb[:], in_=cf[0:1, 256:384]), r=['cf'], w=['onesb'])

    Wb = sb("Wb", [128, 8, NCOL], BF16)
    Wo = sb("Wo", [128, 8, D], BF16)
    stage = sb("stage", [128, NCOL], F32)
    p = stage
    gpre = sb("gpre", [128, 8], F32)
    Gpost = sb("Gpost", [128, D], F32)
    NG = sb("NG", [128, 8], F32)
    cw = sb("cw", [128, 4, 768], BF16)
    dnp = sb("dnp", [128, 8], F32)
    s5p = sb("s5p", [128, 24], F32)
    BT = sb("BT", [128, 2, 8, 128], BF16)
    CT = sb("CT", [128, 2, 8, 32], BF16)
    Dg = sb("Dg", [128, 8, 32], BF16)
    Wglu = sb("Wglu", [128, 2, 256], BF16)
    bgluf = sb("bgluf", [1, 256], F32)
    bglu = sb("bglu", [1, 256], BF16)
    wgkf = sb("wgkf", [16, 128], F32)
    wgk = sb("wgk", [16, 128], BF16)
    bgkf = sb("bgkf", [1, 128], F32)
    bgk = sb("bgk", [1, 128], BF16)
    Tc = sb("Tc", [128, 8, 128], F32)
    Ts = sb("Ts", [128, 8, 128], F32)
    Oc = sb("Oc", [128, 8, 128], F32)
    Os = sb("Os", [128, 8, 128], F32)
    MAGz = sb("MAGz", [128, 8, 128], F32)
    s5t = sb("s5t", [128, 40, 8], F32)
    Rr32 = sb("Rr32", [64, 256], F32)
    Rrb = sb("Rrb", [64, 256], BF16)
    Sd32 = sb("Sd32", [64, 256], F32)
    Sdb = sb("Sdb", [64, 256], BF16)
    Gs32 = sb("Gs32", [32, 256], F32)
    Gsb = sb("Gsb", [32, 256], BF16)
    mc = sb("mc", [128, 2, 8], F32)
    xt1 = sb("xt", [128, D], F32)
    xt = [xt1, xt1]
    ropeC1 = sb("ropeC", [128, 512], F32)
    ropeS1 = sb("ropeS", [128, 512], F32)
    ropeC = [ropeC1, ropeC1]
    ropeS = [ropeS1, ropeS1]
    sm = sb("sm", [128, 72], F32)
    xs = sb("xs", [128, D], BF16)
    yb = xs
    hT = sb("hT", [128, 8, 128], BF16)
    ngg = sb("ngg", [128, D], BF16)
    qk = sb("qk", [128, 512], BF16)
    vb = sb("vb", [128, 512], BF16)
    sT = sb("sT", [128, 512], BF16)
    tmpR = sb("tmpR", [64, 256], F32)
    pk = [sb("pk%d" % i, [128, 4, 768], BF16) for i in range(2)]
    qn = sb("qn", [128, 256], BF16)
    kn = sb("kn", [128, 256], BF16)
    kbq = sb("kbq", [128, 512], BF16)
    dT = sb("dT", [64, 16, 128], BF16)
    qkT = dT[:, 0:8, :]
    gct = sb("gct", [4, 128], F32)
    PP = sb("PP", [128, 4, 512], BF16)
    Pm = [PP[:, 0, :], PP[:, 2, :]]
    PTm = [PP[:, 1, :], PP[:, 3, :]]
    attnT = sb("attnT", [128, 512], BF16)
    Xb = sb("Xb", [128, 4, 128], BF16)
    WT = sb("WT", [64, 4, 128], BF16)
    vnew = sb("vnew", [128, 256], BF16)
    kg = sb("kg", [128, 256], BF16)
    ub = sb("ub", [128, 256], BF16)
    uT = sb("uT", [128, 2, 128], BF16)
    A = [sb("A%d" % i, [128, 8, 128], F32) for i in range(4)]
    _fl = lambda a_: a_.rearrange("p a b -> p (a b)")
    tmpL, tmpU = _fl(A[0][:, 0:4, :]), _fl(A[0][:, 4:8, :])
    DLs, DUs = tmpL, tmpU
    DUi, X32 = _fl(A[1][:, 0:4, :]), A[1][:, 4:8, :]
    cs = _fl(A[2][:])[:, 0:768]
    sq = _fl(A[3][:])[:, 0:512]
    xo = [_fl(A[0][:]), _fl(A[0][:])]
    yT, gT, gsT = hT, qkT[0:32], sT
    sre = PP[:, 0:2, :].rearrange("p a (b c) -> p (a b) c", c=128)
    sim = PP[:, 2:4, :].rearrange("p a (b c) -> p (a b) c", c=128)
    ygb = sb("ygb", [128, 256], BF16)
    ygT = sb("ygT", [128, 2, 128], BF16)
    gcb = sb("gcb", [128, 16], BF16)
    gcT = sb("gcT", [16, 128], BF16)
    Gb = sb("Gb", [128, 768], F32)
    g128 = [Gb[:, i * 128:(i + 1) * 128] for i in range(6)]
    t256, u256, yg = Gb[:, 0:256], Gb[:, 256:512], Gb[:, 512:768]
    gq3 = sb("gq3", [128, 384], BF16)
    PS = [ps("PS%d" % i, [128, 1024], F32) for i in range(4)]
    bank_ctr = [0]

    def bank():
        i = bank_ctr[0] % 8
        bank_ctr[0] += 1
        return PS[i // 2][:, (i % 2) * 512:(i % 2) * 512 + 512], 'PS%d_%d' % (i // 2, i % 2)

    def dbank():
        if bank_ctr[0] % 2:
            bank_ctr[0] += 1
        i = bank_ctr[0] % 8
        bank_ctr[0] += 2
        return PS[i // 2], ['PS%d_0' % (i // 2), 'PS%d_1' % (i // 2)]

    op = R.op
    dumps = []

    def dump(tag, ap, names):
        if not dbg:
            return
        d = nc.dram_tensor("dump_" + tag, list(ap.shape), ap.dtype, kind="ExternalOutput").ap()
        R.dma('sp', d, ap, key='dump_' + tag, r=names, w=['dumpout_' + tag])
        dumps.append('dumpout_' + tag)
    TT = lambda out, a, b, o: (lambda e: e.tensor_tensor(out=out, in0=a, in1=b, op=o))
    TS = lambda out, a, s1, s2, o0, o1=None: (lambda e: e.tensor_scalar(out=out, in0=a, scalar1=s1, scalar2=s2, op0=o0, **({} if o1 is None else {'op1': o1})))
    STT = lambda out, a, s, b, o0, o1: (lambda e: e.scalar_tensor_tensor(out=out, in0=a, scalar=s, in1=b, op0=o0, op1=o1))
    ACT = lambda out, a, f, **kw: (lambda e: e.activation(out=out, in_=a, func=f, **kw))
    CP = lambda out, a: (lambda e: e.tensor_copy(out=out, in_=a))
    MM = lambda out, l, r_, st=True, sp=True: (lambda e: e.matmul(out, lhsT=l, rhs=r_, start=st, stop=sp))
    TR = lambda out, a, idn: (lambda e: e.transpose(out=out, in_=a, identity=idn))
    mult, add, sub = ALU.mult, ALU.add, ALU.subtract

    def rsqrt_small(ap, names, scale, eps):
        op('act', ACT(ap, ap, AF.Ln, scale=scale, bias=eps), r=names, w=names)
        op('act', ACT(ap, ap, AF.Exp, scale=-0.5), r=names, w=names)

    def sigmoid_lnexp(out, in_, sc, r, w):
        op('act', ACT(out, in_, AF.Exp, scale=-sc), r=r, w=w)
        op('act', ACT(out, out, AF.Ln, bias=1.0), r=w, w=w)
        op('act', ACT(out, out, AF.Exp, scale=-1.0), r=w, w=w)

    def setup(l):
        R.dma('sp', gpre[:], d_gpre[l], key='gpre', w=['gpre'])
        R.dma('sp', NG[:], d_ng[l], key='ng', w=['NG'])
        for c in range(8):
            R.dma('sp', stage[:], d_win[l, c * 128:(c + 1) * 128, :], key='stage', w=['p%d' % i for i in range(7)])
            op('dve', TS(Wb[:, c, :], stage[:], gpre[:, c:c + 1], None, mult), r=['p%d' % i for i in range(7)] + ['gpre'], w=['Wb'])
        for c in range(8):
            R.dma('sp', stage[:, 0:D], d_wout[l, c * 128:(c + 1) * 128, :], key='stage', w=['p%d' % i for i in range(7)])
            op('act', ACT(Wo[:, c, :], stage[:, 0:D], AF.Copy, scale=NG[:, c:c + 1]), r=['p%d' % i for i in range(7)] + ['NG'], w=['Wo'])
        R.dma('sp', Gpost[:], d_gpost[l].partition_broadcast(128), key='gpost', w=['Gpost'])
        R.dma('sp', stage[:, 0:3072], d_cw[l].partition_broadcast(128), key='stage', w=['p%d' % i for i in range(7)])
        op('dve', CP(cw[:].rearrange("p a b -> p (a b)"), stage[:, 0:3072]), r=['p%d' % i for i in range(7)], w=['cw'])
        R.dma('sp', dnp[:], d_dnp[l].partition_broadcast(128), key='dnp', w=['dnp'])
        R.dma('sp', s5p[:], d_s5p[l], key='s5p', w=['s5p'])
        BTf, CTf, Dgf, Wgluf = stage[:, 0:2048], stage[:, 2048:2560], stage[:, 2560:2816], stage[:, 2816:3328]
        PN = ['p%d' % i for i in range(7)]
        R.dma('sp', BTf, d_bt[l], key='stage', w=PN)
        R.dma('sp', CTf, d_ct[l], key='stage2', w=PN)
        R.dma('sp', Dgf, d_dg[l], key='stage3', w=PN)
        R.dma('sp', Wgluf, d_wglu[l], key='stage4', w=PN)
        R.dma('sp', bgluf[:], d_bglu[l], key='bglu', w=['bgluf'])
        R.dma('sp', wgkf[:], d_wgk[l], key='wgk', w=['wgkf'])
        R.dma('sp', bgkf[:], d_bgk[l], key='bgk', w=['bgkf'])
        op('dve', CP(BT[:].rearrange("p a b c -> p (a b c)"), BTf), r=PN, w=['BT'])
        op('dve', CP(CT[:, 0].rearrange("p b c -> p (b c)"), CTf[:, 0:256]), r=PN, w=['CT'])
        op('dve', TS(CT[:, 1].rearrange("p b c -> p (b c)"), CTf[:, 256:512], -1.0, None, mult), r=PN, w=['CT'])
        op('dve', CP(Dg[:].rearrange("p b c -> p (b c)"), Dgf), r=PN, w=['Dg'])
        op('dve', CP(Wglu[:].rearrange("p b c -> p (b c)"), Wgluf), r=PN, w=['Wglu'])
        op('dve', CP(bglu[:], bgluf[:]), r=['bgluf'], w=['bglu'])
        op('dve', CP(wgk[:], wgkf[:]), r=['wgkf'], w=['wgk'])
        op('dve', CP(bgk[:], bgkf[:]), r=['bgkf'], w=['bgk'])
        op('act', ACT(dnp[:, 0:4], dnp[:, 0:4], AF.Exp), r=['dnp'], w=['dnp'])
        op('dve', TS(dnp[:, 0:4], dnp[:, 0:4], -1.0, None, mult), r=['dnp'], w=['dnp'])
        for nm, t_ in (('Rr32', Rr32), ('Sd32', Sd32), ('Gs32', Gs32), ('Rrb', Rrb), ('Sdb', Sdb), ('Gsb', Gsb), ('mc', mc)):
            ap_ = t_[:] if len(t_.shape) == 2 else t_[:].rearrange("p a b -> p (a b)")
            op('pool', lambda e, ap_=ap_: e.memset(ap_, 0.0), r=[], w=[nm])
        for i in range(2):
            op('pool', lambda e, i=i: e.memset(pk[i][:].rearrange("p a b -> p (a b)"), 0.0), r=[], w=['pk%d' % i])
        T = lambda i: s5t[:, i, :]
        S = ['s5t']
        lre, lim, ldt = s5p[:, 0:8], s5p[:, 8:16], s5p[:, 16:24]
        dt_, mag, th, xx, x2, sn, cs_, t0, t1 = T(0), T(1), T(2), T(3), T(4), T(5), T(6), T(7), T(8)
        op('act', ACT(dt_, ldt, AF.Exp), r=['s5p'], w=S)
        op('dve', TT(mag, lre, dt_, mult), r=['s5p'] + S, w=S)
        op('act', ACT(mag, mag, AF.Exp), r=S, w=S)
        op('dve', TT(th, lim, dt_, mult), r=['s5p'] + S, w=S)
        op('dve', TS(xx, th, 1.0 / 16, None, mult), r=S, w=S)
        op('dve', TT(x2, xx, xx, mult), r=S, w=S)
        sc = [1.0, -1.0 / 6, 1.0 / 120, -1.0 / 5040, 1.0 / 362880, -1.0 / 39916800, 1.0 / 6227020800]
        cc = [1.0, -1.0 / 2, 1.0 / 24, -1.0 / 720, 1.0 / 40320, -1.0 / 3628800, 1.0 / 479001600, -1.0 / 87178291200]
        op('dve', TS(sn, x2, sc[6], sc[5], mult, add), r=S, w=S)
        for k_ in (4, 3, 2, 1, 0):
            op('dve', TT(sn, sn, x2, mult), r=S, w=S)
            op('dve', TS(sn, sn, sc[k_], None, add), r=S, w=S)
        op('dve', TT(sn, sn, xx, mult), r=S, w=S)
        op('dve', TS(cs_, x2, cc[7], cc[6], mult, add), r=S, w=S)
        for k_ in (5, 4, 3, 2, 1, 0):
            op('dve', TT(cs_, cs_, x2, mult), r=S, w=S)
            op('dve', TS(cs_, cs_, cc[k_], None, add), r=S, w=S)
        for _ in range(4):
            op('dve', TT(t0, cs_, cs_, mult), r=S, w=S)
            op('dve', TT(t1, sn, sn, mult), r=S, w=S)
            op('dve', TT(sn, sn, cs_, mult), r=S, w=S)
            op('dve', TS(sn, sn, 2.0, None, mult), r=S, w=S)
            op('dve', TT(cs_, t0, t1, sub), r=S, w=S)
        are, aim, den, cre, cim = T(9), T(10), T(11), T(12), T(13)
        op('dve', TT(are, mag, cs_, mult), r=S, w=S)
        op('dve', TT(aim, mag, sn, mult), r=S, w=S)
        op('dve', TT(t0, lre, lre, mult), r=['s5p'] + S, w=S)
        op('dve', TT(t1, lim, lim, mult), r=['s5p'] + S, w=S)
        op('dve', TT(den, t0, t1, add), r=S, w=S)
        op('dve', lambda e: e.reciprocal(out=den, in_=den), r=S, w=S)
        nr = T(14)
        op('dve', TS(nr, are, -1.0, None, add), r=S, w=S)
        op('dve', TT(t0, nr, lre, mult), r=['s5p'] + S, w=S)
        op('dve', TT(t1, aim, lim, mult), r=['s5p'] + S, w=S)
        op('dve', TT(cre, t0, t1, add), r=S, w=S)
        op('dve', TT(cre, cre, den, mult), r=S, w=S)
        op('dve', TT(t0, aim, lre, mult), r=['s5p'] + S, w=S)
        op('dve', TT(t1, nr, lim, mult), r=['s5p'] + S, w=S)
        op('dve', TT(cim, t0, t1, sub), r=S, w=S)
        op('dve', TT(cim, cim, den, mult), r=S, w=S)
        op('pool', lambda e: e.memset(Oc[:, :, 0:1], 1.0), r=[], w=['Oc'])
        op('pool', lambda e: e.memset(Os[:, :, 0:1], 0.0), r=[], w=['Os'])
        op('dve', CP(Oc[:, :, 1], cs_), r=S, w=['Oc'])
        op('dve', CP(Os[:, :, 1], sn), r=S, w=['Os'])
        OO = ['Oc', 'Os']
        k_ = 1
        while k_ < 128:
            bc = lambda t_, k_=k_: t_[:, :, k_:k_ + 1].to_broadcast([128, 8, k_])
            hi = slice(k_ + 1, 2 * k_ + 1) if 2 * k_ + 1 <= 128 else slice(k_ + 1, 128)
            n_ = hi.stop - hi.start
            lo = slice(1, 1 + n_)
            bcn = lambda t_, k_=k_, n_=n_: t_[:, :, k_:k_ + 1].to_broadcast([128, 8, n_])
            a1, a2 = A[0][:, :, 0:n_], A[1][:, :, 0:n_]
            op('dve', TT(a1, Oc[:, :, lo], bcn(Oc), mult), r=OO, w=['A0'])
            op('dve', TT(a2, Os[:, :, lo], bcn(Os), mult), r=OO, w=['A1'])
            op('dve', TT(Oc[:, :, hi], a1, a2, sub), r=['A0', 'A1'] + OO, w=['Oc'])
            op('dve', TT(a1, Oc[:, :, lo], bcn(Os), mult), r=OO, w=['A0'])
            op('dve', TT(a2, Os[:, :, lo], bcn(Oc), mult), r=OO, w=['A1'])
            op('dve', TT(Os[:, :, hi], a1, a2, add), r=['A0', 'A1'] + OO, w=['Os'])
            k_ *= 2
        bc8 = lambda t_: t_.unsqueeze(2).to_broadcast([128, 8, 128])
        op('dve', TT(A[0][:], Oc[:], bc8(cre), mult), r=OO + S, w=['A0'])
        op('dve', TT(A[1][:], Os[:], bc8(cim), mult), r=OO + S, w=['A1'])
        op('dve', TT(Tc[:], A[0][:], A[1][:], add), r=['A0', 'A1'], w=['Tc'])
        op('dve', TT(A[0][:], Oc[:], bc8(cim), mult), r=OO + S, w=['A0'])
        op('dve', TT(A[1][:], Os[:], bc8(cre), mult), r=OO + S, w=['A1'])
        op('dve', TT(Ts[:], A[0][:], A[1][:], sub), r=['A0', 'A1'], w=['Ts'])
        op('dve', CP(MAGz[:], bc8(mag)), r=S, w=['MAGz'])
        op('pool', lambda e: e.memset(MAGz[:, :, 0:1], 0.0), r=[], w=['MAGz'])
        mec, mes = T(15), T(16)
        op('dve', TT(t0, Oc[:, :, 127], cs_, mult), r=OO + S, w=S)
        op('dve', TT(t1, Os[:, :, 127], sn, mult), r=OO + S, w=S)
        op('dve', TT(mec, t0, t1, sub), r=S, w=S)
        op('dve', TT(t0, Oc[:, :, 127], sn, mult), r=OO + S, w=S)
        op('dve', TT(t1, Os[:, :, 127], cs_, mult), r=OO + S, w=S)
        op('dve', TT(mes, t0, t1, add), r=S, w=S)
        op('dve', TT(mec, mec, mag, mult), r=S, w=S)
        op('dve', TT(mes, mes, mag, mult), r=S, w=S)

    def branch_out(obank, obn, br):
        op('act', ACT(sq[:, 0:256], obank, AF.Square), r=[obn], w=['A3'])
        ssq = sm[:, 8 + 4 * br: 12 + 4 * br]
        nm = ['sm_b%d' % br]
        op('dve', lambda e: e.tensor_reduce(out=ssq, in_=sq[:, 0:256].rearrange("p (h d) -> p h d", h=4), axis=AX.X, op=add), r=['A3'], w=nm)
        rsqrt_small(ssq, nm, 1.0 / 64, 1e-6)
        hv = lambda a_: a_.rearrange("p (h d) -> p h d", h=4)
        op('dve', TT(hv(sq[:, 0:256]), hv(obank), ssq.unsqueeze(2).to_broadcast([128, 4, 64]), mult), r=[obn, 'A3'] + nm, w=['A3'])
        op('dve', TT(yb[:, br * 256:(br + 1) * 256], sq[:, 0:256], ngg[:, br * 256:(br + 1) * 256], mult),
           r=['A3', 'ngg', 'hT'], w=['yb%d' % br])

    def early(l, n, dst, b):
        R.dma('sp', dst[n * 128:(n + 1) * 128, :], xt[b][:], key='xo', r=['xt'], w=['dst%d_%d' % (l, n)])

    def tile(l, n, src, dst):
        b = n % 2
        R.dma('sp', xt[b][:], src[n * 128:(n + 1) * 128, :], key='xt', r=(['dst%d_%d' % (l - 1, n)] if l > 0 else []), w=['xt'])
        R.dma('sp', ropeC[b][:], d_ropec[n], key='rc', w=['rc'])
        R.dma('sp', ropeS[b][:], d_ropes[n], key='rs', w=['rs'])
        X = 'xt'
        op('act', ACT(A[3][:].rearrange('p a b -> p (a b)'), xt[b][:], AF.Square, accum_out=sm[:, 0:1]), r=[X], w=['A3', 'sm0'])
        rsqrt_small(sm[:, 0:1], ['sm0'], 1.0 / D, 1e-6)
        op('act', ACT(xs[:], xt[b][:], AF.Copy, scale=sm[:, 0:1]), r=[X, 'sm0'], w=['xs', 'yb0', 'yb1', 'yb2', 'yb3'])
        bk, bn = bank()
        bkb = bk.bitcast(BF16)
        for c in range(8):
            op('pe', TR(bkb[:, c * 128:(c + 1) * 128], xs[:, c * 128:(c + 1) * 128], identb), r=['xs', 'cb'], w=[bn])
        op('dve', CP(hT[:].rearrange("p a b -> p (a b)"), bkb), r=[bn], w=['hT'])
        for i, (c0, c1) in enumerate(BANKS):
            bk, bn = bank()
            for c in range(8):
                op('pe', MM(bk[:, 0:c1 - c0], hT[:, c, :], Wb[:, c, c0:c1], c == 0, c == 7), r=['hT', 'Wb'], w=[bn])
            if i % 2 == 0:
                op('act', ACT(p[:, c0:c1], bk[:, 0:c1 - c0], AF.Copy), r=[bn], w=['p%d' % i])
            else:
                op('dve', CP(p[:, c0:c1], bk[:, 0:c1 - c0]), r=[bn], w=['p%d' % i])
        if upto < 1:
            return early(l, n, dst, b)
        g0 = COL['rg'][0]
        op('act', ACT(ngg[:], p[:, g0:g0 + D], AF.Silu), r=['p5', 'p6'], w=['ngg'])
        c0 = COL['dq'][0]
        for k in range(4):
            op('pool', TT(pk[b][:, k, :], p[:, c0:c0 + 768], cw[:, k, :], mult), r=['p2', 'p3', 'cw'], w=['pk%d' % b])
        bq, bqn = bank()
        bv, bvn = bank()
        for (bk, bn, o0, w_) in ((bq, bqn, 0, 512), (bv, bvn, 512, 256)):
            taps = [(Sh[3 - k], pk[b][:, k, o0:o0 + w_], 'pk%d' % b) for k in range(4)]
            taps += [(ShP[3 - k], pk[1 - b][:, k, o0:o0 + w_], 'pk%d' % (1 - b)) for k in range(3)]
            for i, (lt, rh, nm) in enumerate(taps):
                op('pe', MM(bk[:, 0:w_], lt, rh, i == 0, i == len(taps) - 1), r=['cb', nm], w=[bn])
        op('act', ACT(cs[:, 0:512], bq, AF.Silu), r=[bqn], w=['A2'])
        op('act', ACT(cs[:, 512:768], bv[:, 0:256], AF.Silu), r=[bvn], w=['A2'])
        if upto < 2:
            return early(l, n, dst, b)
        RC, RS = ropeC[b], ropeS[b]
        A3f = A[3][:].rearrange('p a b -> p (a b)')
        m1, m2 = A3f[:, 0:512], A3f[:, 512:1024]
        op('dve', TT(m1, p[:, 0:512], RC[:], mult), r=['p0', 'rc'], w=['A3'])
        pv = p[:, 0:512].rearrange("p (a t) -> p a t", t=2)
        m2v = m2.rearrange("p (a t) -> p a t", t=2)
        sv = RS[:].rearrange("p (a t) -> p a t", t=2)
        op('dve', TT(m2v[:, :, 0], pv[:, :, 1], sv[:, :, 0], mult), r=['p0', 'rs'], w=['A3'])
        op('dve', TT(m2v[:, :, 1], pv[:, :, 0], sv[:, :, 1], mult), r=['p0', 'rs'], w=['A3'])
        op('dve', TT(qk[:], m1, m2, add), r=['A3'], w=['qk'])
        op('act', ACT(vb[:], p[:, 512:1024], AF.Copy), r=['p1'], w=['vb'])
        bk, bn = bank()
        bkb = bk.bitcast(BF16)
        for j in range(8):
            op('pe', TR(bkb[0:64, j * 128:(j + 1) * 128], qk[:, j * 64:(j + 1) * 64], identb), r=['qk', 'cb'], w=[bn])
        op('dve', CP(qkT[:].rearrange("p a b -> p (a b)"), bkb[0:64, :]), r=[bn], w=['dT0'])
        bk, bn = bank()
        for h in range(4):
            op('pe', MM(bk[:, h * 128:(h + 1) * 128], qkT[:, 4 + h, :], qkT[:, h, :]), r=['dT0'], w=[bn])
        op('dve', TT(sT[:], bk, Ui4, mult), r=[bn, 'cb'], w=['sT'])
        bo, bon = bank()
        for h in range(4):
            op('pe', MM(bo[:, h * 64:(h + 1) * 64], sT[:, h * 128:(h + 1) * 128], vb[:, h * 64:(h + 1) * 64], True, False), r=['sT', 'vb'], w=[bon])
            op('pe', MM(bo[:, h * 64:(h + 1) * 64], qkT[:, h, :], Rrb[:, h * 64:(h + 1) * 64], False, True), r=['dT0', 'Rrb'], w=[bon])
        bk, bn = bank()
        for h in range(4):
            op('pe', MM(bk[0:64, h * 64:(h + 1) * 64], qk[:, 256 + h * 64:256 + (h + 1) * 64], vb[:, h * 64:(h + 1) * 64]), r=['qk', 'vb'], w=[bn])
        op('dve', TT(tmpR[:], Rr32[:], bk[0:64, 0:256], add), r=['Rr32', bn], w=['tmpR'])
        op('pool', TT(Rr32[:], tmpR[:], GC, mult), r=['tmpR', 'cf'], w=['Rr32'])
        op('act', ACT(Rrb[:], Rr32[:], AF.Copy), r=['Rr32'], w=['Rrb'])
        branch_out(bo[:, 0:256], bon, 0)
        if upto < 3:
            return early(l, n, dst, b)
        op('act', ACT(sq[:], cs[:, 0:512], AF.Square), r=['A2'], w=['A3'])
        rn = sm[:, 24:32]
        op('dve', lambda e: e.tensor_reduce(out=rn, in_=sq[:].rearrange("p (h d) -> p h d", h=8), axis=AX.X, op=add), r=['A3'], w=['rn'])
        rsqrt_small(rn, ['rn'], 1.0, 1e-6)
        h64 = lambda a_: a_.rearrange("p (h d) -> p h d", h=4)
        bc64 = lambda a_: a_.unsqueeze(2).to_broadcast([128, 4, 64])
        op('dve', TS(rn[:, 0:4], rn[:, 0:4], 0.125, None, mult), r=['rn'], w=['rn'])
        op('dve', TT(h64(qn[:]), h64(cs[:, 0:256]), bc64(rn[:, 0:4]), mult), r=['A2', 'rn'], w=['qn'])
        op('dve', TT(h64(kn[:]), h64(cs[:, 256:512]), bc64(rn[:, 4:8]), mult), r=['A2', 'rn'], w=['kn'])
        beta, gz, gg_, gc, eg, egl, egll, beg = (sm[:, 32:36], sm[:, 36:40], sm[:, 40:44], sm[:, 44:52], sm[:, 52:56],
                                                  sm[:, 56:60], sm[:, 60:64], sm[:, 4:8])
        DS = ['dsm']
        cb_, ca_ = COL['dbeta'][0], COL['da'][0]
        sigmoid_lnexp(beta, p[:, cb_:cb_ + 4], 1.0, ['p4'], DS)
        op('dve', TT(gz, p[:, ca_:ca_ + 4], dnp[:, 4:8], add), r=['p4', 'dnp'], w=DS)
        op('act', ACT(gz, gz, AF.Exp), r=DS, w=DS)
        op('act', ACT(gz, gz, AF.Ln, bias=1.0), r=DS, w=DS)
        op('dve', TT(gg_, gz, dnp[:, 0:4], mult), r=DS + ['dnp'], w=DS)
        if upto < 3.2:
            return early(l, n, dst, b)
        bg, bgn = bank()
        op('pe', MM(bg[:, 0:4], triU, gg_), r=['cf'] + DS, w=[bgn])
        op('pe', MM(bg[:, 4:8], ones, gg_), r=['cf'] + DS, w=[bgn])
        op('pe', MM(bg[0:4, 128:256], gg_, triU), r=['cf'] + DS, w=[bgn])
        op('dve', CP(gc, bg[:, 0:8]), r=[bgn], w=DS)
        op('dve', CP(gct[:], bg[0:4, 128:256]), r=[bgn], w=['gct'])
        op('act', ACT(eg, gc[:, 0:4], AF.Exp), r=DS, w=DS)
        op('dve', TT(egl, gc[:, 4:8], gc[:, 0:4], sub), r=DS, w=DS)
        op('act', ACT(egl, egl, AF.Exp), r=DS, w=DS)
        op('act', ACT(egll, gc[:, 4:8], AF.Exp), r=DS, w=DS)
        op('dve', TT(beg, beta, eg, mult), r=DS, w=DS)
        if upto < 3.3:
            return early(l, n, dst, b)
        bR, bRn = bank()
        for h in range(4):
            op('pe', MM(bR[:, h * 128:(h + 1) * 128], Esel[:, h * 128:(h + 1) * 128], gct[:]), r=['cf', 'gct'], w=[bRn])
        h128 = lambda a_: a_.rearrange("p (h c) -> p h c", h=4)
        op('dve', TT(h128(tmpU), h128(bR), gc[:, 0:4].unsqueeze(2).to_broadcast([128, 4, 128]), sub), r=[bRn] + DS, w=['A0'])
        op('dve', TS(tmpL, tmpU, 0.0, None, ALU.max), r=['A0'], w=['A0'])
        op('dve', TS(tmpU, tmpU, 0.0, None, ALU.min), r=['A0'], w=['A0'])
        if upto < 3.4:
            return early(l, n, dst, b)
        op('act', ACT(tmpL[:], tmpL[:], AF.Exp, scale=-1.0), r=['A0'], w=['A0'])
        op('act', ACT(tmpU[:], tmpU[:], AF.Exp), r=['A0'], w=['A0'])
        op('pool', TT(DLs[:], tmpL[:], negLs4, mult), r=['A0', 'cb'], w=['A0'])
        op('pool', TT(DUi[:], tmpU[:], Ui4, mult), r=['A0', 'cb'], w=['A1'])
        op('pool', TT(DUs[:], tmpU[:], negUs4, mult), r=['A0', 'cb'], w=['A0'])
        if l == 0 and n == 0:
            dump('cs', cs, ['A2']); dump('kn', kn[:], ['kn']); dump('qn', qn[:], ['qn']); dump('sm', sm[:, 24:64], DS + ['rn'])
            dump('DLs', DLs, ['A0']); dump('DUs', DUs, ['A0']); dump('DUi', DUi, ['A1'])
        op('dve', TT(h64(kbq[:, 0:256]), h64(kn[:]), bc64(beta), mult), r=['kn'] + DS, w=['kbq'])
        op('dve', TT(h64(kbq[:, 256:512]), h64(qn[:]), bc64(eg), mult), r=['qn'] + DS, w=['kbq'])
        if upto < 3.5:
            return early(l, n, dst, b)
        srcs = [(kn, 0, 'kn'), (kbq, 0, 'kbq'), (qn, 0, 'qn'), (kbq, 256, 'kbq')]
        for half in range(2):
            bk, bn = bank()
            bkb = bk.bitcast(BF16)
            for jj in range(8):
                j = half * 8 + jj
                t_, off, nm = srcs[j // 4]
                h = j % 4
                op('pe', TR(bkb[0:64, jj * 128:(jj + 1) * 128], t_[:, off + h * 64:off + (h + 1) * 64], identb), r=[nm, 'cb'], w=[bn])
            op('dve' if half == 0 else 'act', CP(dT[:, half * 8:(half + 1) * 8, :].rearrange("p a b -> p (a b)"), bkb[0:64, :]) if half == 0 else
               ACT(dT[:, half * 8:(half + 1) * 8, :].rearrange("p a b -> p (a b)"), bkb[0:64, :], AF.Copy), r=[bn], w=['dT%d' % half])
        bA, bAn = bank()
        bAT, bATn = bank()
        bat, batn = bank()
        for h in range(4):
            hs = slice(h * 128, (h + 1) * 128)
            op('pe', MM(bA[:, hs], dT[:, 4 + h, :], dT[:, h, :]), r=['dT0'], w=[bAn])
            op('pe', MM(bAT[:, hs], dT[:, h, :], dT[:, 4 + h, :]), r=['dT0'], w=[bATn])
            op('pe', MM(bat[:, hs], dT[:, h, :], dT[:, 8 + h, :]), r=['dT0', 'dT1'], w=[batn])
        op('dve', TT(Pm[0][:], bA, DLs[:], mult), r=[bAn, 'A0'], w=['Pm0'])
        op('dve', TT(PTm[0][:], bAT, DUs[:], mult), r=[bATn, 'A0'], w=['PTm0'])
        op('dve', TT(attnT[:], bat, DUi[:], mult), r=[batn, 'A1'], w=['attnT'])
        op('dve', TT(X32[:, :, 0:64], h64(cs[:, 512:768]), bc64(beta), mult), r=['A2'] + DS, w=['A1'])
        op('dve', TT(X32[:, :, 64:128], h64(kn[:]), bc64(beg), mult), r=['kn'] + DS, w=['A1'])
        if l == 0 and n == 0:
            dump('N0', Pm[0], ['Pm0']); dump('NT0', PTm[0], ['PTm0']); dump('attnT', attnT[:], ['attnT']); dump('X0', X32, ['A1'])
        X32f = X32[:].rearrange("p a b -> p (a b)")
        Xbf = Xb[:].rearrange("p a b -> p (a b)")
        op('act', ACT(Xbf, X32f, AF.Copy), r=['A1'], w=['Xb'])
        if upto < 3.6:
            return early(l, n, dst, b)
        Lm, LmT, Yb, W1b = sT[:], qk[:], kbq[:], xs[:, 512:1024]
        Tm, TTm = Pm[1], PTm[1]
        v4 = lambda a_: a_.rearrange("p (h c) -> p h c", h=4)
        bm = lambda i: HM[i].unsqueeze(1).to_broadcast([128, 4, 128])
        idb4 = identb.unsqueeze(1).to_broadcast([128, 4, 128])
        op('pool', TT(v4(Tm), v4(Pm[0]), bm(0), mult), r=['Pm0', 'cb'], w=['Pm1'])
        op('pool', TT(v4(Tm), v4(Tm), idb4, add), r=['Pm1', 'cb'], w=['Pm1'])
        op('pool', TT(v4(TTm), v4(PTm[0]), bm(7), mult), r=['PTm0', 'cb'], w=['PTm1'])
        op('pool', TT(v4(TTm), v4(TTm), idb4, add), r=['PTm1', 'cb'], w=['PTm1'])
        for lev in range(1, 7):
            op('pool', TT(v4(Lm), v4(Pm[0]), bm(lev), mult), r=['Pm0', 'cb'], w=['sT'])
            op('pool', TT(v4(LmT), v4(PTm[0]), bm(7 + lev), mult), r=['PTm0', 'cb'], w=['qk'])
            bY, bYn = bank()
            bW, bWn = bank()
            for h in range(4):
                hs = slice(h * 128, (h + 1) * 128)
                op('pe', MM(bY[:, hs], LmT[:, hs], Tm[:, hs]), r=['qk', 'Pm1'], w=[bYn])
                op('pe', MM(bW[:, hs], Lm[:, hs], TTm[:, hs]), r=['sT', 'PTm1'], w=[bWn])
            op('act', ACT(Yb, bY, AF.Copy), r=[bYn], w=['kbq'])
            op('dve', CP(W1b, bW), r=[bWn], w=['yb2', 'yb3'])
            bZ, bZn = bank()
            bW2, bW2n = bank()
            for h in range(4):
                hs = slice(h * 128, (h + 1) * 128)
                op('pe', MM(bZ[:, hs], TTm[:, hs], Yb[:, hs]), r=['kbq', 'PTm1'], w=[bZn])
                op('pe', MM(bW2[:, hs], Tm[:, hs], W1b[:, hs]), r=['yb2', 'yb3', 'Pm1'], w=[bW2n])
            op('dve', TT(Tm, Tm, bZ, add), r=['Pm1', bZn], w=['Pm1'])
            op('dve', TT(TTm, TTm, bW2, add), r=['PTm1', bW2n], w=['PTm1'])
        if upto < 3.7:
            return early(l, n, dst, b)
        bX, bXn = bank()
        for h in range(4):
            hs = slice(h * 128, (h + 1) * 128)
            op('pe', MM(bX[:, hs], TTm[:, hs], Xb[:, h, :]), r=['PTm1', 'Xb'], w=[bXn])
        op('dve', CP(X32f, bX), r=[bXn], w=['A1'])
        op('act', ACT(Xbf, bX, AF.Copy), r=[bXn], w=['Xb'])
        if l == 0 and n == 0:
            dump('X7', X32, ['A1'])
        bk, bn = bank()
        bkb = bk.bitcast(BF16)
        for h in range(4):
            op('pe', TR(bkb[0:64, h * 128:(h + 1) * 128], Xb[:, h, 64:128], identb), r=['Xb', 'cb'], w=[bn])
        op('dve', CP(WT[:].rearrange("p a b -> p (a b)"), bkb[0:64, 0:512]), r=[bn], w=['WT'])
        if upto < 3.8:
            return early(l, n, dst, b)
        bw, bwn = bank()
        for h in range(4):
            hs = slice(h * 64, (h + 1) * 64)
            op('pe', MM(bw[:, hs], WT[:, h, :], Sdb[:, hs]), r=['WT', 'Sdb'], w=[bwn])
        op('dve', TT(vnew[:].rearrange("p (h d) -> p h d", h=4), X32[:, :, 0:64], bw[:, 0:256].rearrange("p (h d) -> p h d", h=4), sub),
           r=['A1', bwn], w=['vnew'])
        if upto < 3.85:
            return early(l, n, dst, b)
        bo, bon = bank()
        for h in range(4):
            hs = slice(h * 64, (h + 1) * 64)
            op('pe', MM(bo[:, hs], dT[:, 12 + h, :], Sdb[:, hs], True, False), r=['dT1', 'Sdb'], w=[bon])
            op('pe', MM(bo[:, hs], attnT[:, h * 128:(h + 1) * 128], vnew[:, hs], False, True), r=['attnT', 'vnew'], w=[bon])
        if upto < 3.9:
            return early(l, n, dst, b)
        op('dve', TT(h64(kg[:]), h64(kn[:]), bc64(egl), mult), r=['kn'] + DS, w=['kg'])
        bk, bn = bank()
        for h in range(4):
            hs = slice(h * 64, (h + 1) * 64)
            op('pe', MM(bk[0:64, hs], kg[:, hs], vnew[:, hs]), r=['kg', 'vnew'], w=[bn])
        for h in range(4):
            hs = slice(h * 64, (h + 1) * 64)
            op('dve', STT(Sd32[:, hs], Sd32[:, hs], egll[0:64, h:h + 1], bk[0:64, hs], mult, add), r=['Sd32', bn] + DS, w=['Sd32'])
        if upto < 3.95:
            return early(l, n, dst, b)
        op('dve', CP(Sdb[:], Sd32[:]), r=['Sd32'], w=['Sdb'])
        if l == 0 and n == 0:
            dump('vnew', vnew[:], ['vnew'])
            op('dve', CP(Gb[:, 0:256], bo[:, 0:256]), r=[bon], w=['G'])
            dump('odn', Gb[:, 0:256], ['G'])
        branch_out(bo[:, 0:256], bon, 1)
        if l == 0 and n == 0:
            dump('ssqdn', sm[:, 12:16], ['sm_b1']); dump('ngg', ngg[:], ['ngg'])
        if upto < 4:
            return early(l, n, dst, b)
        cu = COL['su'][0]
        op('act', ACT(ub[:], p[:, cu:cu + 256], AF.Copy), r=['p3'], w=['ub'])
        bk, bn = bank()
        bkb = bk.bitcast(BF16)
        for c in range(2):
            op('pe', TR(bkb[:, c * 128:(c + 1) * 128], ub[:, c * 128:(c + 1) * 128], identb), r=['ub', 'cb'], w=[bn])
        op('dve', CP(uT[:].rearrange("p a b -> p (a b)"), bkb[:, 0:256]), r=[bn], w=['uT'])
        xr, xrn = dbank()
        xi, xin = dbank()
        for s_ in range(8):
            op('pe', MM(xr[:, s_ * 128:(s_ + 1) * 128], BT[:, 0, s_, :], uT[:, s_ // 4, :]), r=['BT', 'uT'], w=[xrn[s_ // 4]])
            op('pe', MM(xi[:, s_ * 128:(s_ + 1) * 128], BT[:, 1, s_, :], uT[:, s_ // 4, :]), r=['BT', 'uT'], w=[xin[s_ // 4]])
        Af = [a[:].rearrange("p a b -> p (a b)") for a in A]
        fl = lambda t_: t_[:].rearrange("p a b -> p (a b)")
        op('dve', TT(Af[0], xr[:], fl(Tc), mult), r=xrn + ['Tc'], w=['A0'])
        op('dve', TT(Af[1], xi[:], fl(Ts), mult), r=xin + ['Ts'], w=['A1'])
        op('dve', TT(Af[0], Af[0], Af[1], sub), r=['A0', 'A1'], w=['A0'])
        op('dve', TT(Af[1], xr[:], fl(Ts), mult), r=xrn + ['Ts'], w=['A1'])
        op('dve', TT(Af[2], xi[:], fl(Tc), mult), r=xin + ['Tc'], w=['A2'])
        op('dve', TT(Af[1], Af[1], Af[2], add), r=['A1', 'A2'], w=['A1'])
        op('dve', TT(A[0][:, :, 0], A[0][:, :, 0], mc[:, 0, :], add), r=['A0', 'mc'], w=['A0'])
        op('dve', TT(A[1][:, :, 0], A[1][:, :, 0], mc[:, 1, :], add), r=['A1', 'mc'], w=['A1'])
        op('dve', lambda e: e.tensor_tensor_scan(out=Af[2], data0=fl(MAGz), data1=Af[0], initial=0.0, op0=mult, op1=add), r=['MAGz', 'A0'], w=['A2'])
        op('dve', lambda e: e.tensor_tensor_scan(out=Af[3], data0=fl(MAGz), data1=Af[1], initial=0.0, op0=mult, op1=add), r=['MAGz', 'A1'], w=['A3'])
        T = lambda i: s5t[:, i, :]
        mec, mes, t0, t1 = T(15), T(16), T(17), T(18)
        S2 = ['s5t2']
        op('dve', TT(t0, A[2][:, :, 127], mec, mult), r=['A2', 's5t'], w=S2)
        op('dve', TT(t1, A[3][:, :, 127], mes, mult), r=['A3', 's5t'], w=S2)
        op('dve', TT(mc[:, 0, :], t0, t1, sub), r=S2, w=['mc'])
        op('dve', TT(t0, A[2][:, :, 127], mes, mult), r=['A2', 's5t'], w=S2)
        op('dve', TT(t1, A[3][:, :, 127], mec, mult), r=['A3', 's5t'], w=S2)
        op('dve', TT(mc[:, 1, :], t0, t1, add), r=S2, w=['mc'])
        op('pool', TT(Af[0], Af[2], fl(Oc), mult), r=['A2', 'Oc'], w=['A0'])
        op('pool', TT(Af[1], Af[3], fl(Os), mult), r=['A3', 'Os'], w=['A1'])
        op('pool', TT(fl(sre), Af[0], Af[1], sub), r=['A0', 'A1'], w=['Pm0', 'PTm0'])
        pt, ptn = dbank()
        op('dve', TT(pt[:], Af[2], fl(Os), mult), r=['A2', 'Os'], w=ptn)
        op('dve', TT(Af[3], Af[3], fl(Oc), mult), r=['A3', 'Oc'], w=['A3'])
        op('dve', TT(fl(sim), Af[3], pt[:], add), r=['A3'] + ptn, w=['Pm1', 'PTm1'])
        by, byn = bank()
        for s_ in range(8):
            cs_ = slice(s_ * 32, (s_ + 1) * 32)
            op('pe', MM(by[:, cs_], sre[:, s_, :], CT[:, 0, s_, :], True, False), r=['Pm0', 'PTm0', 'CT'], w=[byn])
            op('pe', MM(by[:, cs_], sim[:, s_, :], CT[:, 1, s_, :], False, False), r=['Pm1', 'PTm1', 'CT'], w=[byn])
            op('pe', MM(by[:, cs_], uT[:, s_ // 4, :], Dg[:, s_, :], False, True), r=['uT', 'Dg'], w=[byn])
        y_ = by[:, 0:256]
        op('act', ACT(t256[:], y_, AF.Square), r=[byn], w=['G'])
        op('dve', TS(t256[:], t256[:], 0.044715, 1.0, mult, add), r=['G'], w=['G'])
        op('dve', TT(t256[:], t256[:], y_, mult), r=['G', byn], w=['G'])
        op('act', ACT(t256[:], t256[:], AF.Sigmoid, scale=2.0 * 0.7978845608028654), r=['G'], w=['G'])
        op('dve', TT(yg[:], t256[:], y_, mult), r=['G', byn], w=['G'])
        op('act', ACT(ygb[:], yg[:], AF.Copy), r=['G'], w=['ygb'])
        bk, bn = bank()
        bkb = bk.bitcast(BF16)
        for c in range(2):
            op('pe', TR(bkb[:, c * 128:(c + 1) * 128], ygb[:, c * 128:(c + 1) * 128], identb), r=['ygb', 'cb'], w=[bn])
        op('dve', CP(ygT[:].rearrange("p a b -> p (a b)"), bkb[:, 0:256]), r=[bn], w=['ygT'])
        bgl, bgln = bank()
        op('pe', MM(bgl[:, 0:256], ygT[:, 0, :], Wglu[:, 0, :], True, False), r=['ygT', 'Wglu'], w=[bgln])
        op('pe', MM(bgl[:, 0:256], ygT[:, 1, :], Wglu[:, 1, :], False, False), r=['ygT', 'Wglu'], w=[bgln])
        op('pe', MM(bgl[:, 0:256], onesb[:], bglu[:], False, True), r=['onesb', 'bglu'], w=[bgln])
        op('act', ACT(u256[:], bgl[:, 0:256], AF.Sigmoid), r=[bgln], w=['G'])
        op('dve', TT(u256[:], u256[:], yg[:], mult), r=['G', 'G'], w=['G'])
        op('dve', TT(yb[:, 512:768], u256[:], ngg[:, 512:768], mult), r=['G', 'ngg', 'hT'], w=['yb2'])
        if upto < 5:
            return early(l, n, dst, b)
        cgc, cgq, cgk = COL['gcode'][0], COL['gq'][0], COL['gk'][0]
        op('act', ACT(gcb[:], p[:, cgc:cgc + 16], AF.Copy), r=['p4'], w=['gcb'])
        bk, bn = bank()
        bkb = bk.bitcast(BF16)
        op('pe', TR(bkb[0:16, 0:128], gcb[:], identb), r=['gcb', 'cb'], w=[bn])
        op('dve', CP(gcT[:], bkb[0:16, 0:128]), r=[bn], w=['gcT'])
        bz, bzn = bank()
        op('pe', MM(bz[:, 0:128], gcT[:], wgk[:], True, False), r=['gcT', 'wgk'], w=[bzn])
        op('pe', MM(bz[:, 0:128], onesb[:], bgk[:], False, True), r=['onesb', 'bgk'], w=[bzn])
        gkk, cum, clb, ec, enc, ecl = [g[:] for g in g128]
        op('act', ACT(gkk, bz[:, 0:128], AF.Exp, scale=-1.0), r=[bzn], w=['G'])
        op('act', ACT(gkk, gkk, AF.Ln, bias=1.0), r=['G'], w=['G'])
        op('dve', TS(gkk, gkk, -1.0 / 16, None, mult), r=['G'], w=['G'])
        bc_, bcn_ = bank()
        op('pe', MM(bc_[:, 0:128], triU, gkk), r=['cf', 'G'], w=[bcn_])
        op('pe', MM(bc_[:, 128:256], ones, gkk), r=['cf', 'G'], w=[bcn_])
        for h in range(4):
            op('pe', MM(bc_[0:32, 256 + h:257 + h], g128[0][:, h * 32:(h + 1) * 32], ones[:, 0:1]), r=['cf', 'G'], w=[bcn_])
        op('dve', CP(cum, bc_[:, 0:128]), r=[bcn_], w=['G'])
        op('dve', TT(clb, bc_[:, 128:256], cum, sub), r=[bcn_, 'G'], w=['G'])
        op('act', ACT(ec, cum, AF.Exp), r=['G'], w=['G'])
        op('act', ACT(enc, cum, AF.Exp, scale=-1.0), r=['G'], w=['G'])
        op('act', ACT(ecl, clb, AF.Exp), r=['G'], w=['G'])
        ecl32 = sm[0:32, 64:68]
        op('act', ACT(ecl32, bc_[0:32, 256:260], AF.Exp), r=[bcn_], w=['ecl32'])
        op('dve', STT(gq3[:, 0:128], p[:, cgq:cgq + 128], 32.0 ** -0.5, ec, mult, mult), r=['p4', 'G'], w=['gq3'])
        op('dve', TT(gq3[:, 128:256], p[:, cgk:cgk + 128], enc, mult), r=['p4', 'G'], w=['gq3'])
        op('dve', TT(gq3[:, 256:384], p[:, cgk:cgk + 128], ecl, mult), r=['p4', 'G'], w=['gq3'])
        bk, bn = bank()
        bkb = bk.bitcast(BF16)
        for j in range(8):
            op('pe', TR(bkb[0:32, j * 128:(j + 1) * 128], gq3[:, j * 32:(j + 1) * 32], identb), r=['gq3', 'cb'], w=[bn])
        op('dve', CP(gT[:].rearrange("p a b -> p (a b)"), bkb[0:32, :]), r=[bn], w=['dT0'])
        bk, bn = bank()
        for h in range(4):
            op('pe', MM(bk[:, h * 128:(h + 1) * 128], gT[:, 4 + h, :], gT[:, h, :]), r=['dT0'], w=[bn])
        op('dve', TT(gsT[:], bk, Ui4, mult), r=[bn, 'cb'], w=['sT'])
        bo, bon = bank()
        for h in range(4):
            hs = slice(h * 64, (h + 1) * 64)
            gv_ = vb[:, 256 + h * 64:256 + (h + 1) * 64]
            op('pe', MM(bo[:, hs], gsT[:, h * 128:(h + 1) * 128], gv_, True, False), r=['sT', 'vb'], w=[bon])
            op('pe', MM(bo[:, hs], gT[:, h, :], Gsb[:, hs], False, True), r=['dT0', 'Gsb'], w=[bon])
        bk, bn = bank()
        for h in range(4):
            hs = slice(h * 64, (h + 1) * 64)
            op('pe', MM(bk[0:32, hs], gq3[:, 256 + h * 32:256 + (h + 1) * 32], vb[:, 256 + h * 64:256 + (h + 1) * 64]), r=['gq3', 'vb'], w=[bn])
        for h in range(4):
            hs = slice(h * 64, (h + 1) * 64)
            op('dve', STT(Gs32[:, hs], Gs32[:, hs], ecl32[:, h:h + 1], bk[0:32, hs], mult, add), r=['Gs32', bn, 'ecl32'], w=['Gs32'])
        op('act', ACT(Gsb[:], Gs32[:], AF.Copy), r=['Gs32'], w=['Gsb'])
        branch_out(bo[:, 0:256], bon, 3)
        if upto < 6:
            return early(l, n, dst, b)
        bk, bn = bank()
        bkb = bk.bitcast(BF16)
        YB = ['yb0', 'yb1', 'yb2', 'yb3']
        if dbg and l == NL - 1:
            R.dma('sp', d_dbg[n * 128:(n + 1) * 128, :], yb[:], key='dbg', r=YB, w=['dbgout%d' % n])
        for c in range(8):
            op('pe', TR(bkb[:, c * 128:(c + 1) * 128], yb[:, c * 128:(c + 1) * 128], identb), r=YB + ['cb'], w=[bn])
        op('dve', CP(yT[:].rearrange("p a b -> p (a b)"), bkb), r=[bn], w=['hT'])
        po, pon = dbank()
        for half in range(2):
            for c in range(8):
                op('pe', MM(po[:, half * 512:(half + 1) * 512], yT[:, c, :], Wo[:, c, half * 512:(half + 1) * 512], c == 0, c == 7), r=['hT', 'Wo'], w=[pon[half]])
        op('act', ACT(A[3][:].rearrange('p a b -> p (a b)'), po[:], AF.Square, accum_out=sm[:, 1:2]), r=pon, w=['A3', 'sm1'])
        rsqrt_small(sm[:, 1:2], ['sm1'], 1.0 / D, 1e-6)
        op('dve', STT(Af[0], po[:], sm[:, 1:2], Gpost[:], mult, mult), r=pon + ['sm1', 'Gpost'], w=['A0'])
        op('pool', TT(Af[0], Af[0], xt[b][:], add), r=['A0', X], w=['A0'])
        R.dma('sp', dst[n * 128:(n + 1) * 128, :], Af[0], key='xo', r=['A0'], w=['dst%d_%d' % (l, n)])

    for l in range(NL):
        if do_setup:
            setup(l)
        for n in range(NT):
            tile(l, n, xs_dram[l], xs_dram[l + 1])
    print('SBUF bytes/partition', R.sb_bytes)
    R.finish(final_reads=['dst%d_%d' % (NL - 1, n) for n in range(max(0, NT - 2), NT)] + (['dbgout%d' % n for n in range(NT)] + dumps if dbg else []))
    return nc


def _bf(a):
    return np.ascontiguousarray(a).astype(ml_dtypes.bfloat16)


def make_consts(NT):
    idn = np.eye(128, dtype=np.float32)
    j = np.arange(128)[:, None]
    t = np.arange(128)[None, :]
    Sh = [(j == t - s).astype(np.float32) for s in range(4)]
    ShP = [(j == 128 + t - s).astype(np.float32) for s in range(1, 4)]
    Ui = (j <= t).astype(np.float32)
    negLs = -(t < j).astype(np.float32)
    negUs = -(t > j).astype(np.float32)
    hm = []
    ii_, jj_ = np.arange(128)[:, None], np.arange(128)[None, :]
    for lev in range(7):
        b_ = 1 << lev
        hm.append(((ii_ // (2 * b_) == jj_ // (2 * b_)) & (ii_ % (2 * b_) >= b_) & (jj_ % (2 * b_) < b_)).astype(np.float32))
    hm = hm + [m_.T.copy() for m_ in hm]
    cb = np.concatenate([idn] + Sh + ShP + [np.tile(Ui, (1, 4)), np.tile(negLs, (1, 4)), np.tile(negUs, (1, 4))] + hm, axis=1)
    Esel = np.zeros((128, 512), np.float32)
    for h in range(4):
        Esel[h, h * 128:(h + 1) * 128] = 1.0
    GCt = np.zeros((128, 256), np.float32)
    for h in range(4):
        GCt[:, h * 64:(h + 1) * 64] = np.float32(GAMMA[h]) ** 128
    cf = np.concatenate([idn, Ui, np.ones((128, 128), np.float32), Esel, GCt], axis=1).astype(np.float32)
    pos = np.arange(NT * 128, dtype=np.float64)
    inv = 10000.0 ** (-np.arange(0, 64, 2, dtype=np.float64) / 64)
    ang = pos[:, None] * inv[None, :]
    cos, sin = np.cos(ang), np.sin(ang)
    ii = (np.arange(NT * 128) % 128).astype(np.float64)
    Cq = np.zeros((NT * 128, 4, 32, 2)); Sq = np.zeros_like(Cq); Ck = np.zeros_like(Cq); Sk = np.zeros_like(Cq)
    for h in range(4):
        dq = (GAMMA[h] ** (ii + 1.0)) * (64 ** -0.5)
        dk = GAMMA[h] ** (-(ii + 1.0))
        for (Ct, St, dd) in ((Cq, Sq, dq), (Ck, Sk, dk)):
            Ct[:, h, :, 0] = cos * dd[:, None]
            Ct[:, h, :, 1] = cos * dd[:, None]
            St[:, h, :, 0] = -sin * dd[:, None]
            St[:, h, :, 1] = sin * dd[:, None]
    ropec = np.concatenate([Cq.reshape(-1, 256), Ck.reshape(-1, 256)], axis=1).reshape(NT, 128, 512).astype(np.float32)
    ropes = np.concatenate([Sq.reshape(-1, 256), Sk.reshape(-1, 256)], axis=1).reshape(NT, 128, 512).astype(np.float32)
    return dict(cb=_bf(cb), cf=cf, ropec=ropec, ropes=ropes)


def make_params(inp, layers):
    f = lambda k: np.asarray(inp[k], dtype=np.float32)
    L = list(layers)
    NL = len(L)
    w_in = f('w_in')[L][:, :, PERM]
    w_out = f('w_out')[L]
    gpre = f('norm_pre')[L].reshape(NL, 8, 128).transpose(0, 2, 1)
    gpost = f('norm_post')[L].reshape(NL, 1, D)
    ng = np.concatenate([np.tile(f('ret_norm')[L], (1, 4)), np.tile(f('dn_norm')[L], (1, 4)),
                         np.ones((NL, 256), np.float32), np.tile(f('gla_norm')[L], (1, 4))], axis=1).reshape(NL, 8, 128).transpose(0, 2, 1)
    cw = f('dn_conv')[L].reshape(NL, 1, 4 * 768)
    dnp = np.concatenate([f('dn_a_log')[L], f('dn_dt_bias')[L]], axis=1).reshape(NL, 1, 8)

    def st(a):
        return a.reshape(NL, 8, 2, 64).transpose(0, 2, 3, 1).reshape(NL, 128, 8)
    ldt = np.repeat(f('s5_log_dt')[L][:, :, None], 64, axis=2)
    s5p = np.concatenate([st(f('s5_lam_re')[L]), st(f('s5_lam_im')[L]), st(ldt)], axis=2)
    bt = np.zeros((NL, 2, 128, 8, 128), np.float32)
    ct = np.zeros((NL, 2, 128, 8, 32), np.float32)
    dg = np.zeros((NL, 128, 8, 32), np.float32)
    bre, bim, cre, cim, dd = f('s5_b_re')[L], f('s5_b_im')[L], f('s5_c_re')[L], f('s5_c_im')[L], f('s5_d')[L]
    for g in range(16):
        s_, gg = g // 2, g % 2
        r0 = 32 * (s_ % 4) + gg * 16
        for k_, (bb, cc) in enumerate(((bre, cre), (bim, cim))):
            bt[:, k_, r0:r0 + 16, s_, gg * 64:(gg + 1) * 64] = bb[:, g].transpose(0, 2, 1)
            ct[:, k_, gg * 64:(gg + 1) * 64, s_, gg * 16:(gg + 1) * 16] = cc[:, g].transpose(0, 2, 1)
        for h in range(16):
            dg[:, r0 + h, s_, gg * 16 + h] = dd[:, g, h]
    bt = bt.transpose(0, 2, 1, 3, 4).reshape(NL, 128, 2048)
    ct = ct.transpose(0, 2, 1, 3, 4).reshape(NL, 128, 512)
    dg = dg.reshape(NL, 128, 256)
    wglu = f('s5_w_glu')[L].reshape(NL, 2, 128, 256).transpose(0, 2, 1, 3).reshape(NL, 128, 512)
    bglu = f('s5_b_glu')[L].reshape(NL, 1, 256)
    wgk = f('gla_w_gk')[L]
    bgk = f('gla_b_gk')[L].reshape(NL, 1, 128)
    c = np.ascontiguousarray
    return dict(w_in=c(w_in), w_out=c(w_out), gpre=c(gpre), gpost=c(gpost), ng=c(ng), cw=c(cw), dnp=c(dnp), s5p=c(s5p),
                s5bt=c(bt), s5ct=c(ct), s5dg=c(dg), wglu=c(wglu), bglu=c(bglu), wgk=c(wgk), bgk=c(bgk))


_PROG = {}


def kernel(**inputs):
    x = np.asarray(inputs['x'], dtype=np.float32)
    B, L, _ = x.shape
    NT = L // 128
    NLAY = np.asarray(inputs['w_in']).shape[0]
    key = (NT, NLAY)
    if key not in _PROG:
        _PROG[key] = (build_program(NT, NLAY), make_consts(NT))
    nc, consts = _PROG[key]
    prm = make_params(inputs, range(NLAY))
    in_maps = []
    for b in range(B):
        m = dict(consts)
        m.update(prm)
        m['x'] = np.ascontiguousarray(x[b])
        in_maps.append(m)
    res = run_bass_kernel_spmd(nc, in_maps, core_ids=list(range(B)))
    return np.stack([np.asarray(res.results[b]['out'], dtype=np.float32) for b in range(B)], axis=0)
```
